# Optimizing a Trainium2 kernel written in Bass

```python
import jax
import jax.numpy as jnp
from jax import lax
import numpy as np


D_MODEL = 2048
BATCH = 4
SEQ = 2048
DEPTH = 1

PLE_DIM = 256
MIX_W = D_MODEL
SSD_W = MIX_W // 2
ATTN_W = MIX_W - SSD_W
SSD_HEADDIM = 64
SSD_HEADS = SSD_W // SSD_HEADDIM
SSD_GROUPS = 2
SSD_STATE = 128
SSD_CHUNK = 128
CONV_W = 5
XBC_W = SSD_W + 2 * SSD_GROUPS * SSD_STATE
DIFF_HEAD_DIM = 64
DIFF_HEADS = ATTN_W // (2 * DIFF_HEAD_DIM)
ROT_DIM = DIFF_HEAD_DIM // 4
ROPE_THETA = 500000.0
Q_BLOCK = 128
IN_W = SSD_W + XBC_W + 2 * SSD_HEADS + 3 * ATTN_W
MOE_GROUPS = 8
EXPERTS_PER_GROUP = 8
N_EXPERTS = MOE_GROUPS * EXPERTS_PER_GROUP
TOP_K = 2
EXPERT_FF = D_MODEL // 4
ROW_BLOCK = 128
EPS = 1e-6

kernel_name = 'hymba_ssd_diffattn_hmoe_ple_layer'


def rms_norm(x, g):
    xf = x.astype(jnp.float32)
    y = xf * lax.rsqrt(jnp.mean(xf * xf, axis=-1, keepdims=True) + EPS)
    return (y * g.astype(jnp.float32)).astype(x.dtype)


def ssd_scan(x, dt, a, bm, cm):
    b, s, h, p = x.shape
    g, n = bm.shape[2], bm.shape[3]
    r = h // g
    c = s // SSD_CHUNK
    l = SSD_CHUNK
    xd = (x.astype(jnp.float32) * dt[..., None]).reshape(b, c, l, g, r, p)
    da = (dt * a).reshape(b, c, l, g, r).transpose(0, 3, 4, 1, 2)
    bc = bm.astype(jnp.float32).reshape(b, c, l, g, n)
    cc = cm.astype(jnp.float32).reshape(b, c, l, g, n)
    a_cs = jnp.cumsum(da, axis=-1)
    seg = a_cs[..., :, None] - a_cs[..., None, :]
    lower = jnp.tril(jnp.ones((l, l), dtype=bool))
    decay_in = jnp.exp(jnp.where(lower, seg, -jnp.inf))
    cb = jnp.einsum('bclgn,bcsgn->bcgls', cc, bc)
    y_diag = jnp.einsum('bcgls,bgrcls,bcsgrp->bclgrp', cb, decay_in, xd)
    decay_to_end = jnp.exp(a_cs[..., -1:] - a_cs)
    chunk_states = jnp.einsum('bclgn,bgrcl,bclgrp->bcgrpn', bc, decay_to_end, xd)
    chunk_decay = jnp.exp(a_cs[..., -1])

    def step(state, inp):
        st, dec = inp
        return state * dec[..., None, None] + st, state

    _, states_in = lax.scan(step, jnp.zeros_like(chunk_states[:, 0]),
                            (jnp.moveaxis(chunk_states, 1, 0), jnp.moveaxis(chunk_decay, 3, 0)))
    states_in = jnp.moveaxis(states_in, 0, 1)
    y_off = jnp.einsum('bclgn,bcgrpn,bgrcl->bclgrp', cc, states_in, jnp.exp(a_cs))
    return (y_diag + y_off).reshape(b, s, h, p).astype(x.dtype)


def ssd_mixer(z, xbc, dt_f_raw, dt_b_raw, conv_w, conv_b, dt_bias_f, dt_bias_b,
              a_log_f, a_log_b, d_skip, norm_g):
    b, s, _ = z.shape
    pad = CONV_W // 2
    xbc = lax.conv_general_dilated(xbc, conv_w[:, None, :].astype(xbc.dtype), (1,), [(pad, pad)],
                                   dimension_numbers=('NWC', 'WIO', 'NWC'),
                                   feature_group_count=XBC_W)
    xbc = jax.nn.silu(xbc + conv_b)
    xs, bm, cm = jnp.split(xbc, [SSD_W, SSD_W + SSD_GROUPS * SSD_STATE], axis=-1)
    xs = xs.reshape(b, s, SSD_HEADS, SSD_HEADDIM)
    bm = bm.reshape(b, s, SSD_GROUPS, SSD_STATE)
    cm = cm.reshape(b, s, SSD_GROUPS, SSD_STATE)
    dt_f = jax.nn.softplus((dt_f_raw + dt_bias_f).astype(jnp.float32))
    dt_b = jax.nn.softplus((dt_b_raw + dt_bias_b).astype(jnp.float32))
    a_f = -jnp.exp(a_log_f.astype(jnp.float32))
    a_b = -jnp.exp(a_log_b.astype(jnp.float32))
    y_f = ssd_scan(xs, dt_f, a_f, bm, cm)
    y_b = jnp.flip(ssd_scan(jnp.flip(xs, 1), jnp.flip(dt_b, 1), a_b,
                            jnp.flip(bm, 1), jnp.flip(cm, 1)), 1)
    y = (y_f + y_b + xs * d_skip[:, None]).reshape(b, s, SSD_W)
    return rms_norm(y * jax.nn.silu(z), norm_g)


def rope_partial(t, cos, sin):
    half = ROT_DIM // 2
    c = cos[:, :, None, None, :].astype(t.dtype)
    sn = sin[:, :, None, None, :].astype(t.dtype)
    t1 = t[..., :half]
    t2 = t[..., half:ROT_DIM]
    return jnp.concatenate([t1 * c - t2 * sn, t2 * c + t1 * sn, t[..., ROT_DIM:]], axis=-1)


def diff_attention(q, k, v, cos, sin, lam, lam_init, subln_g):
    b, s = q.shape[0], q.shape[1]
    q = rope_partial(q.reshape(b, s, DIFF_HEADS, 2, DIFF_HEAD_DIM), cos, sin) * (DIFF_HEAD_DIM ** -0.5)
    k = rope_partial(k.reshape(b, s, DIFF_HEADS, 2, DIFF_HEAD_DIM), cos, sin)
    v = v.reshape(b, s, DIFF_HEADS, 2 * DIFF_HEAD_DIM)
    nq = s // Q_BLOCK
    q_blocks = jnp.moveaxis(q.reshape(b, nq, Q_BLOCK, DIFF_HEADS, 2, DIFF_HEAD_DIM), 1, 0)

    def attend(qb):
        scores = jnp.einsum('bqhcd,bkhcd->bhcqk', qb, k).astype(jnp.float32)
        probs = jax.nn.softmax(scores, axis=-1)
        w = probs[:, :, 0] - lam * probs[:, :, 1]
        return jnp.einsum('bhqk,bkhe->bqhe', w.astype(v.dtype), v)

    o = lax.map(attend, q_blocks)
    o = jnp.moveaxis(o, 0, 1).reshape(b, s, DIFF_HEADS, 2 * DIFF_HEAD_DIM)
    o = rms_norm(o, subln_g) * (1.0 - lam_init)
    return o.reshape(b, s, ATTN_W)


def hier_moe(h, w_rg, b_rg, w_re, b_re, w_gate, w_up, w_down):
    bsz, s, d = h.shape
    t = bsz * s
    hf = h.reshape(t, d)
    g_logits = (hf @ w_rg + b_rg).astype(jnp.float32)
    g_prob = jax.nn.softmax(g_logits, axis=-1)
    g_sel = jnp.argmax(g_logits, axis=-1)
    p_g = jnp.take_along_axis(g_prob, g_sel[:, None], axis=1)[:, 0]
    e_logits = (hf @ w_re + b_re).astype(jnp.float32).reshape(t, MOE_GROUPS, EXPERTS_PER_GROUP)
    e_logits = jnp.take_along_axis(e_logits, g_sel[:, None, None], axis=1)[:, 0]
    top_w, top_i = lax.top_k(jax.nn.softmax(e_logits, axis=-1), TOP_K)
    top_w = top_w / jnp.sum(top_w, axis=-1, keepdims=True)
    gate = p_g[:, None] * top_w
    expert_id = (g_sel[:, None] * EXPERTS_PER_GROUP + top_i).astype(jnp.int32)
    e_flat = expert_id.reshape(-1)
    tok_flat = jnp.repeat(jnp.arange(t, dtype=jnp.int32), TOP_K)
    w_flat = gate.reshape(-1)
    order = jnp.argsort(e_flat)
    e_sorted = e_flat[order]
    counts = jnp.bincount(e_flat, length=N_EXPERTS).astype(jnp.int32)
    padded = ((counts + ROW_BLOCK - 1) // ROW_BLOCK) * ROW_BLOCK
    ends_pad = jnp.cumsum(padded)
    starts_pad = ends_pad - padded
    starts = jnp.cumsum(counts) - counts
    dest = starts_pad[e_sorted] + (jnp.arange(t * TOP_K, dtype=jnp.int32) - starts[e_sorted])
    n_rows = ((t * TOP_K + N_EXPERTS * (ROW_BLOCK - 1)) + ROW_BLOCK - 1) // ROW_BLOCK * ROW_BLOCK
    n_blocks = n_rows // ROW_BLOCK
    row_tok = jnp.full((n_rows,), t, dtype=jnp.int32).at[dest].set(tok_flat[order])
    row_w = jnp.zeros((n_rows,), jnp.float32).at[dest].set(w_flat[order])
    block_expert = jnp.minimum(
        jnp.searchsorted(ends_pad, jnp.arange(n_blocks, dtype=jnp.int32) * ROW_BLOCK, side='right'),
        N_EXPERTS - 1)
    x_rows = jnp.concatenate([hf, jnp.zeros((1, d), hf.dtype)], axis=0)[row_tok]
    x_rows = x_rows.reshape(n_blocks, ROW_BLOCK, d)

    def expert_block(args):
        xb, e = args
        return (jax.nn.silu(xb @ w_gate[e]) * (xb @ w_up[e])) @ w_down[e]

    y_rows = lax.map(expert_block, (x_rows, block_expert)).reshape(n_rows, d)
    y = jax.ops.segment_sum(y_rows * row_w[:, None].astype(y_rows.dtype), row_tok,
                            num_segments=t + 1)[:t]
    return y.reshape(bsz, s, d)


def setup_inputs(seed: int = 0) -> dict:
    key = jax.random.key(seed)
    ks = list(jax.random.split(key, 40))
    f32 = jnp.float32

    def nrm(k, shape, scale):
        return jax.random.normal(k, shape, f32) * scale

    def gain(k, shape):
        return 1.0 + 0.01 * jax.random.normal(k, shape, f32)

    x = jax.random.normal(ks[0], (BATCH, SEQ, D_MODEL), f32)
    p = jax.random.normal(ks[1], (DEPTH, BATCH, SEQ, PLE_DIM), f32)
    offsets = jax.random.randint(ks[2], (BATCH, 1), 0, 1024, dtype=jnp.int32)
    positions = offsets + jnp.arange(SEQ, dtype=jnp.int32)[None, :]
    dt0 = jnp.exp(jax.random.uniform(ks[7], (DEPTH, SSD_HEADS), f32, np.log(1e-3), np.log(1e-1)))
    dt1 = jnp.exp(jax.random.uniform(ks[8], (DEPTH, SSD_HEADS), f32, np.log(1e-3), np.log(1e-1)))
    inv_softplus = lambda y: y + jnp.log(-jnp.expm1(-y))
    return {
        'x': x,
        'p': p,
        'positions': positions,
        'norm_mix_g': gain(ks[3], (DEPTH, D_MODEL)),
        'w_in': nrm(ks[4], (DEPTH, D_MODEL, IN_W), D_MODEL ** -0.5),
        'conv_w': nrm(ks[5], (DEPTH, CONV_W, XBC_W), CONV_W ** -0.5),
        'conv_b': nrm(ks[6], (DEPTH, XBC_W), 0.02),
        'dt_bias_f': inv_softplus(dt0),
        'dt_bias_b': inv_softplus(dt1),
        'a_log_f': jnp.log(jax.random.uniform(ks[9], (DEPTH, SSD_HEADS), f32, 1.0, 16.0)),
        'a_log_b': jnp.log(jax.random.uniform(ks[10], (DEPTH, SSD_HEADS), f32, 1.0, 16.0)),
        'd_skip': gain(ks[11], (DEPTH, SSD_HEADS)),
        'ssd_norm_g': gain(ks[12], (DEPTH, SSD_W)),
        'lam_q1': nrm(ks[13], (DEPTH, DIFF_HEAD_DIM), 0.1),
        'lam_k1': nrm(ks[14], (DEPTH, DIFF_HEAD_DIM), 0.1),
        'lam_q2': nrm(ks[15], (DEPTH, DIFF_HEAD_DIM), 0.1),
        'lam_k2': nrm(ks[16], (DEPTH, DIFF_HEAD_DIM), 0.1),
        'subln_g': gain(ks[17], (DEPTH, 2 * DIFF_HEAD_DIM)),
        'w_out': nrm(ks[18], (DEPTH, MIX_W, D_MODEL), MIX_W ** -0.5),
        'norm_ffn_g': gain(ks[19], (DEPTH, D_MODEL)),
        'w_route_group': nrm(ks[20], (DEPTH, D_MODEL, MOE_GROUPS), D_MODEL ** -0.5),
        'b_route_group': nrm(ks[21], (DEPTH, MOE_GROUPS), 0.01),
        'w_route_expert': nrm(ks[22], (DEPTH, D_MODEL, N_EXPERTS), D_MODEL ** -0.5),
        'b_route_expert': nrm(ks[23], (DEPTH, N_EXPERTS), 0.01),
        'w_exp_gate': nrm(ks[24], (DEPTH, N_EXPERTS, D_MODEL, EXPERT_FF), D_MODEL ** -0.5),
        'w_exp_up': nrm(ks[25], (DEPTH, N_EXPERTS, D_MODEL, EXPERT_FF), D_MODEL ** -0.5),
        'w_exp_down': nrm(ks[26], (DEPTH, N_EXPERTS, EXPERT_FF, D_MODEL), EXPERT_FF ** -0.5),
        'w_ple_proj': nrm(ks[27], (DEPTH, PLE_DIM, D_MODEL), PLE_DIM ** -0.5),
        'ple_norm_g': gain(ks[28], (DEPTH, D_MODEL)),
        'w_ple_gate': nrm(ks[29], (DEPTH, D_MODEL, D_MODEL), D_MODEL ** -0.5),
        'b_ple_gate': nrm(ks[30], (DEPTH, D_MODEL), 0.02),
        'final_norm_g': gain(ks[31], (D_MODEL,)),
    }


def reference(x, p, positions, norm_mix_g, w_in, conv_w, conv_b, dt_bias_f, dt_bias_b,
              a_log_f, a_log_b, d_skip, ssd_norm_g, lam_q1, lam_k1, lam_q2, lam_k2, subln_g,
              w_out, norm_ffn_g, w_route_group, b_route_group, w_route_expert, b_route_expert,
              w_exp_gate, w_exp_up, w_exp_down, w_ple_proj, ple_norm_g, w_ple_gate, b_ple_gate,
              final_norm_g):
    inv_freq = ROPE_THETA ** (-jnp.arange(0, ROT_DIM, 2, dtype=jnp.float32) / ROT_DIM)
    angles = positions.astype(jnp.float32)[..., None] * inv_freq
    cos, sin = jnp.cos(angles), jnp.sin(angles)
    sizes = [SSD_W, XBC_W, SSD_HEADS, SSD_HEADS, ATTN_W, ATTN_W, ATTN_W]
    split_at = np.cumsum(sizes)[:-1].tolist()
    h = x
    for i in range(DEPTH):
        lam_init = 0.8 - 0.6 * float(np.exp(-0.3 * i))
        n = rms_norm(h, norm_mix_g[i])
        proj = n @ w_in[i]
        z, xbc, dt_f, dt_b, q, k, v = jnp.split(proj, split_at, axis=-1)
        y_ssd = ssd_mixer(z, xbc, dt_f, dt_b, conv_w[i], conv_b[i], dt_bias_f[i], dt_bias_b[i],
                          a_log_f[i], a_log_b[i], d_skip[i], ssd_norm_g[i])
        lam = (jnp.exp(jnp.sum(lam_q1[i] * lam_k1[i]).astype(jnp.float32))
               - jnp.exp(jnp.sum(lam_q2[i] * lam_k2[i]).astype(jnp.float32)) + lam_init)
        y_att = diff_attention(q, k, v, cos, sin, lam, lam_init, subln_g[i])
        h = h + jnp.concatenate([y_ssd, y_att], axis=-1) @ w_out[i]
        h = h + hier_moe(rms_norm(h, norm_ffn_g[i]), w_route_group[i], b_route_group[i],
                         w_route_expert[i], b_route_expert[i], w_exp_gate[i], w_exp_up[i],
                         w_exp_down[i])
        ple = rms_norm(p[i] @ w_ple_proj[i], ple_norm_g[i])
        h = h + jax.nn.sigmoid(h @ w_ple_gate[i] + b_ple_gate[i]) * ple
    return rms_norm(h, final_norm_g)
```

```python
import numpy as np
import concourse.bass as bass
import concourse.mybir as mybir
from concourse.bass_utils import run_bass_kernel_spmd

F32 = mybir.dt.float32
BF16 = mybir.dt.bfloat16
I32 = mybir.dt.int32
AF = mybir.ActivationFunctionType
ALU = mybir.AluOpType
AX = mybir.AxisListType

SBUF_LO = 16512
SBUF_HI = 229344


class _Op:
    __slots__ = ("eng", "fn", "dma", "deps", "flag", "ordinal", "sem", "semval", "idx", "extra_waits")

    def __init__(self, eng, fn, dma):
        self.eng = eng
        self.fn = fn
        self.dma = dma
        self.deps = set()
        self.flag = False
        self.ordinal = -1
        self.sem = None
        self.semval = 0
        self.idx = -1
        self.extra_waits = None


def _box(ap):
    t = ap.tensor
    name = t.name
    pairs = [(int(s), int(c)) for s, c in ap.ap]
    off = int(ap.offset)
    cls = type(t).__name__
    if cls.startswith("DRam"):
        lo = off
        hi = off
        for s, c in pairs:
            if s >= 0:
                hi += s * (c - 1)
            else:
                lo += s * (c - 1)
        return (name, 0, 1, lo, hi)
    shape = [int(v) for v in t.shape]
    rowlen = 1
    for v in shape[1:]:
        rowlen *= v
    esz = 4 if t.dtype in (F32, I32) else 2
    p0 = off // rowlen
    lo = off % rowlen
    ps, pc = pairs[0]
    p1 = p0 + (pc - 1) * (ps // rowlen) + 1
    hi = lo
    for s, c in pairs[1:]:
        hi += abs(s) * (c - 1)
    return (name, p0, p1, lo * esz, (hi + 1) * esz - 1)


def _overlap(a, b):
    return a[1] < b[2] and b[1] < a[2] and a[3] <= b[4] and b[3] <= a[4]


def _covers(a, b):
    return a[1] <= b[1] and a[2] >= b[2] and a[3] <= b[3] and a[4] >= b[4]


class Prog:
    ENGS = ("pe", "act", "dve", "pool", "sp")
    NDMA = 8

    def __init__(self, nc):
        self.nc = nc
        self.ops = []
        self.live = {}
        self.per_eng = {e: [] for e in self.ENGS}
        self.dma_count = {e: 0 for e in self.ENGS}
        self.dma_hist = {e: [] for e in self.ENGS}
        self.sb_ptr = SBUF_LO
        self.sb_names = 0
        self.psum = []

    def sb(self, name, shape, dtype):
        esz = 4 if dtype in (F32, I32) else 2
        n = 1
        for v in shape[1:]:
            n *= v
        nbytes = (n * esz + 31) // 32 * 32
        off = self.sb_ptr
        if off + nbytes > getattr(self, "sb_hi", SBUF_HI):
            raise RuntimeError(f"SBUF overflow allocating {name}: {off}+{nbytes}")
        self.sb_ptr += nbytes
        self.sb_names += 1
        return self.nc.alloc_sbuf_tensor_at(f"{name}_{self.sb_names}", list(shape), dtype, offset=off)

    def sb_top(self, name, shape, dtype):
        esz = 4 if dtype in (F32, I32) else 2
        n = 1
        for v in shape[1:]:
            n *= v
        nbytes = (n * esz + 31) // 32 * 32
        self.sb_hi = getattr(self, "sb_hi", SBUF_HI) - nbytes
        self.sb_names += 1
        return self.nc.alloc_sbuf_tensor_at(f"{name}_{self.sb_names}", list(shape), dtype, offset=self.sb_hi)

    def sb_at(self, name, shape, dtype, offset):
        self.sb_names += 1
        return self.nc.alloc_sbuf_tensor_at(f"{name}_{self.sb_names}", list(shape), dtype, offset=offset)

    def sb_mark(self):
        return self.sb_ptr

    def sb_release(self, mark):
        self.barrier()
        self.sb_ptr = mark

    def add(self, eng, fn, reads=(), writes=(), dma=False):
        op = _Op(eng, fn, dma)
        op.idx = len(self.ops)
        self.ops.append(op)
        self.per_eng[eng].append(op)
        for ap in reads:
            b = _box(ap)
            lst = self.live.setdefault(b[0], [])
            for ent in lst:
                if ent[2] and _overlap(ent[0], b):
                    op.deps.add(ent[1])
            if not dma:
                for ent in lst:
                    if (not ent[2]) and ent[0] == b and ent[1].eng == eng and not ent[1].dma:
                        ent[1] = op
                        break
                else:
                    lst.append([b, op, False])
            else:
                lst.append([b, op, False])
        for ap in writes:
            b = _box(ap)
            lst = self.live.setdefault(b[0], [])
            keep = []
            for ent in lst:
                if _overlap(ent[0], b):
                    if ent[1] is not op:
                        op.deps.add(ent[1])
                    if _covers(b, ent[0]):
                        continue
                keep.append(ent)
            keep.append([b, op, True])
            self.live[b[0]] = keep
        if dma:
            j = self.dma_count[eng]
            self.dma_count[eng] = j + 1
            op.sem = ("dma", eng, j % self.NDMA)
            op.semval = 16 * (j // self.NDMA + 1)
            hist = self.dma_hist[eng]
            if j >= self.NDMA:
                op.deps.add(hist[j - self.NDMA])
            hist.append(op)
        return op

    def barrier(self):
        last = {e: (self.per_eng[e][-1] if self.per_eng[e] else None) for e in self.ENGS}
        dmas = []
        for e in self.ENGS:
            dmas.extend(self.dma_hist[e][-self.NDMA:])
        for e in self.ENGS:
            op = _Op(e, None, False)
            op.idx = len(self.ops)
            self.ops.append(op)
            self.per_eng[e].append(op)
            for e2 in self.ENGS:
                if last[e2] is not None and not last[e2].dma:
                    op.deps.add(last[e2])
                elif last[e2] is not None:
                    for o in reversed(self.per_eng[e2][:-1]):
                        if not o.dma:
                            op.deps.add(o)
                            break
            for d in dmas:
                op.deps.add(d)
        self.live = {}

    def emit(self):
        nc = self.nc
        for op in self.ops:
            for d in op.deps:
                if d.dma:
                    continue
                if d.eng == "pe" and op.eng == "pe" and not op.dma and op.fn is not None:
                    continue
                d.flag = True
        counts = {e: 0 for e in self.ENGS}
        for e in self.ENGS:
            for op in self.per_eng[e]:
                if op.dma:
                    continue
                if op.flag:
                    if op.fn is None:
                        op.flag = False
                        continue
                    counts[e] += 1
                    op.ordinal = counts[e]
        import contextlib
        with contextlib.ExitStack() as st:
            sems = {}
            for e in self.ENGS:
                if counts[e] > 0:
                    sems[("eng", e)] = st.enter_context(nc.semaphore(f"c_{e}"))
                if self.dma_count[e] > 0:
                    for k in range(min(self.NDMA, self.dma_count[e])):
                        sems[("dma", e, k)] = st.enter_context(nc.semaphore(f"d_{e}{k}"))
            block = st.enter_context(nc.Block())
            engobj = {}

            def run_engine(e, eng):
                waited = {}
                for op in self.per_eng[e]:
                    need = {}
                    for d in op.deps:
                        if d.dma:
                            key, val = d.sem, d.semval
                        else:
                            if d.eng == "pe" and e == "pe" and not op.dma and op.fn is not None:
                                continue
                            if d.ordinal < 0:
                                continue
                            key, val = ("eng", d.eng), d.ordinal
                        if need.get(key, 0) < val:
                            need[key] = val
                    for key, val in need.items():
                        if waited.get(key, 0) >= val:
                            continue
                        waited[key] = val
                        eng.wait_ge(sems[key], val)
                    if op.fn is None:
                        continue
                    ins = op.fn(eng)
                    if op.dma:
                        ins.then_inc(sems[op.sem], 16)
                    elif op.flag:
                        ins.then_inc(sems[("eng", e)], 1)

            @block.tensor
            def _(eng):
                run_engine("pe", eng)

            @block.scalar
            def _(eng):
                run_engine("act", eng)

            @block.vector
            def _(eng):
                run_engine("dve", eng)

            @block.gpsimd
            def _(eng):
                run_engine("pool", eng)

            @block.sync
            def _(eng):
                run_engine("sp", eng)

    def dma(self, q, out, in_, **kw):
        return self.add(q, lambda eng: eng.dma_start(out=out, in_=in_, **kw), reads=[in_], writes=[out], dma=True)

    def mm(self, out, lhsT, rhs, start=True, stop=True, **kw):
        rd = [lhsT, rhs]
        return self.add("pe", lambda eng: eng.matmul(out, lhsT, rhs, start=start, stop=stop, **kw),
                        reads=rd, writes=[out])

    def tr(self, out, in_, ident):
        return self.add("pe", lambda eng: eng.transpose(out, in_, ident), reads=[in_, ident], writes=[out])

    def act(self, out, in_, func, bias=None, scale=None, accum_out=None, eng="act"):
        kw = {}
        rd = [in_]
        wr = [out]
        if bias is not None:
            kw["bias"] = bias
            if not isinstance(bias, (int, float)):
                rd.append(bias)
        if scale is not None:
            kw["scale"] = scale
            if not isinstance(scale, (int, float)):
                rd.append(scale)
        if accum_out is not None:
            kw["accum_out"] = accum_out
            wr.append(accum_out)
        return self.add(eng, lambda e: e.activation(out, in_, func, **kw), reads=rd, writes=wr)

    def tt(self, out, in0, in1, op, eng="dve"):
        return self.add(eng, lambda e: e.tensor_tensor(out, in0, in1, op), reads=[in0, in1], writes=[out])

    def ts(self, out, in0, s1, s2, op0, op1=None, eng="dve", accum_out=None):
        rd = [in0]
        if not isinstance(s1, (int, float)):
            rd.append(s1)
        if s2 is not None and not isinstance(s2, (int, float)):
            rd.append(s2)
        wr = [out]
        kw = {}
        if accum_out is not None:
            kw["accum_out"] = accum_out
            wr.append(accum_out)
        if op1 is None:
            return self.add(eng, lambda e: e.tensor_scalar(out, in0, s1, None, op0, **kw), reads=rd, writes=wr)
        return self.add(eng, lambda e: e.tensor_scalar(out, in0, s1, s2, op0, op1, **kw), reads=rd, writes=wr)

    def stt(self, out, in0, scalar, in1, op0, op1, eng="dve", accum_out=None):
        rd = [in0, in1]
        if not isinstance(scalar, (int, float)):
            rd.append(scalar)
        wr = [out]
        kw = {}
        if accum_out is not None:
            kw["accum_out"] = accum_out
            wr.append(accum_out)
        return self.add(eng, lambda e: e.scalar_tensor_tensor(out, in0, scalar, in1, op0, op1, **kw),
                        reads=rd, writes=wr)

    def copy(self, out, in_, eng="dve"):
        if eng == "act":
            return self.add("act", lambda e: e.copy(out, in_), reads=[in_], writes=[out])
        return self.add(eng, lambda e: e.tensor_copy(out, in_), reads=[in_], writes=[out])

    def reduce(self, out, in_, op, axis=AX.X, eng="dve"):
        return self.add(eng, lambda e: e.tensor_reduce(out, in_, axis, op), reads=[in_], writes=[out])

    def memset(self, ap, val, eng="dve"):
        return self.add(eng, lambda e: e.memset(ap, val), reads=[], writes=[ap])


D = 2048
SEQ = 2048
OWN = 1024
NT_ALL = 16
NT_OWN = 8
IN_W = 5664
EPS = 1e-6
C_Z = 0
C_XBC = 1024
C_DT = 2560
C_Q = 2592
C_K = 3616
C_V = 4640
NEG = -1.0e30

K_IDENT = 0
K_TRIU = 128
K_TRIL = 256
K_ONES = 384
K_NMU = 512
K_NML = 640
K_SLT = 768
K_DELTA = 896
K_INVF = 912
K_IOTA = 920
NCONST = 984


def host_consts():
    c = np.zeros((128, NCONST), np.float32)
    s = np.arange(128)[:, None]
    l = np.arange(128)[None, :]
    c[:, K_IDENT:K_IDENT + 128] = (s == l)
    c[:, K_TRIU:K_TRIU + 128] = (s <= l)
    c[:, K_TRIL:K_TRIL + 128] = (s >= l)
    c[:, K_ONES:K_ONES + 128] = 1.0
    c[:, K_NMU:K_NMU + 128] = np.where(s <= l, 0.0, NEG)
    c[:, K_NML:K_NML + 128] = np.where(s >= l, 0.0, NEG)
    c[:, K_SLT:K_SLT + 128] = (s < l)
    c[:16, K_DELTA:K_DELTA + 16] = np.eye(16)
    inv_freq = (500000.0 ** (-np.arange(0, 16, 2, dtype=np.float32) / 16)).astype(np.float32)
    c[:, K_INVF:K_INVF + 8] = inv_freq[None, :]
    c[:, K_IOTA:K_IOTA + 64] = np.arange(64, dtype=np.float32)[None, :]
    return c


class Ctx:
    pass


def build_program(nc, stages=99, dbg=False):
    P = Prog(nc)
    nc.allow_low_precision("bf16 matmul operands, fp32 accumulation")
    okind = "ExternalOutput" if dbg else "Internal"

    def din(name, shape, dt=F32):
        return nc.dram_tensor(name, list(shape), dt, kind="ExternalInput").ap()

    def dscr(name, shape, dt):
        return nc.dram_tensor(name, list(shape), dt, kind=okind).ap()

    x_d = din("x", [SEQ, D])
    pos_d = din("pos", [128, NT_ALL], I32)
    consts_d = din("consts", [128, NCONST])
    g_mix_d = din("g_mix", [1, D])
    w_in_d = din("w_in", [D, IN_W])
    convw_d = din("convw", [128, 12, 5])
    convb_d = din("convb", [128, 12])
    dtb_d = din("dtb", [1, 32])
    alog_d = din("alog", [1, 32])
    out_d = nc.dram_tensor("out", [OWN, D], F32, kind="ExternalOutput").ap()

    z_d = dscr("z_s", [OWN, 1024], F32)
    q_d = dscr("q_s", [OWN, 1024], BF16)
    k_d = dscr("k_s", [SEQ, 1024], BF16)
    v_d = dscr("v_s", [SEQ, 1024], BF16)
    xsb_d = dscr("xsb_s", [SEQ, 1280], BF16)
    bc_d = dscr("bc_s", [512, SEQ], BF16)
    dt_d = dscr("dt_s", [SEQ, 64], F32)

    consts = P.sb("consts", [128, NCONST], F32)
    ident_bf = P.sb("ident_bf", [128, 128], BF16)
    dtda = P.sb("dtda", [128, NT_ALL, 64], F32)
    junk = P.sb("junk", [128, D], BF16)
    ss = [P.sb(f"ss{i}", [128, 2], F32) for i in range(2)]
    eps_t = P.sb("eps_t", [128, 1], F32)
    P.memset(eps_t[:], EPS)
    u_sb = P.sb_top("u_sb", [128, NT_OWN, D], BF16)
    U_OFF = P.sb_hi
    ps = [nc.alloc_psum_tensor(f"ps{i}", [128, 512], F32) for i in range(8)]
    psb = [ps[6][:].bitcast(BF16), ps[7][:].bitcast(BF16)]

    P.dma("sp", consts[:], consts_d)
    P.copy(ident_bf[:], consts[:, K_IDENT:K_IDENT + 128])
    ident_f = consts[:, K_IDENT:K_IDENT + 128]

    evac_rr = [0]

    def evac(out, in_):
        evac_rr[0] += 1
        if evac_rr[0] % 2:
            return P.copy(out, in_, eng="act")
        return P.copy(out, in_, eng="dve")

    mark0 = P.sb_mark()
    nT = P.sb("nT", [128, 16, SEQ], BF16)
    mark1 = P.sb_mark()
    g_bc = P.sb("g_bc", [128, D], F32)
    xt = [P.sb(f"xt{i}", [128, D], F32) for i in range(2)]
    nb = [P.sb(f"nb{i}", [128, D], BF16) for i in range(2)]

    P.dma("sp", g_bc[:], g_mix_d.to_broadcast([128, D]))


    def rmsnorm_tile(src, dst_bf, gb, ssb, width):
        P.stt(junk[:, 0:width], src, 1.0, src, ALU.mult, ALU.mult, accum_out=ssb[:, 0:1])
        P.act(ssb[:, 1:2], ssb[:, 0:1], AF.Sqrt, bias=eps_t[:, 0:1], scale=1.0 / width)
        P.add("dve", lambda e, o=ssb[:, 1:2]: e.reciprocal(o, o), reads=[ssb[:, 1:2]], writes=[ssb[:, 1:2]])
        P.stt(dst_bf, src, ssb[:, 1:2], gb, ALU.mult, ALU.mult)

    for i in range(NT_ALL):
        xti = xt[i % 2]
        P.dma("sp", xti[:], x_d[i * 128:(i + 1) * 128, :])
        rmsnorm_tile(xti[:], nb[i % 2][:], g_bc[:], ss[i % 2], D)
        for grp in range(4):
            pt = psb[grp % 2]
            for j in range(4):
                kc = grp * 4 + j
                P.tr(pt[:, j * 128:(j + 1) * 128], nb[i % 2][:, kc * 128:(kc + 1) * 128], ident_bf[:])
            evac(nT[:, grp * 4:(grp + 1) * 4, i * 128:(i + 1) * 128],
                 pt[:, 0:512].rearrange("p (j t) -> p j t", j=4))
    P.sb_release(mark1)
    if stages <= 1:
        P.emit()
        return P

    wb = [P.sb(f"wb{i}", [128, 16, 512], BF16) for i in range(2)]
    w_in_v = w_in_d.rearrange("(kc p) n -> p kc n", p=128)
    wb_i = [0]

    def load_wblock(c0, ncols):
        b = wb[wb_i[0] % 2]
        wb_i[0] += 1
        for q4 in range(4):
            P.dma("pool", b[:, q4 * 4:(q4 + 1) * 4, 0:ncols], w_in_v[:, q4 * 4:(q4 + 1) * 4, c0:c0 + ncols])
        return b

    cs_sin = P.sb("sin", [128, NT_ALL, 8], F32)
    cs_cos = P.sb("cos", [128, NT_ALL, 8], F32)
    pos_i = P.sb("pos_i", [128, NT_ALL], I32)
    pos_sb = P.sb("pos", [128, NT_ALL], F32)
    ang = P.sb("ang", [128, NT_ALL, 8], F32)
    P.dma("sp", pos_i[:], pos_d)
    P.copy(pos_sb[:], pos_i[:])
    TWO_PI = float(2 * np.pi)
    P.tt(ang[:], pos_sb[:].unsqueeze(2).to_broadcast([128, NT_ALL, 8]),
         consts[:, K_INVF:K_INVF + 8].unsqueeze(1).to_broadcast([128, NT_ALL, 8]), ALU.mult)
    angi = P.sb("angi", [128, NT_ALL, 8], I32)
    angk = P.sb("angk", [128, NT_ALL, 8], F32)
    angm = P.sb("angm", [128, NT_ALL, 8], F32)
    PI = float(np.pi)

    def sin_of(dst, shift):
        P.ts(angk[:], ang[:], shift, 1.0 / TWO_PI, ALU.add, ALU.mult)
        P.copy(angi[:], angk[:])
        P.copy(angk[:], angi[:])
        P.ts(angm[:], ang[:], shift, None, ALU.add)
        P.stt(angm[:], angk[:], -TWO_PI, angm[:], ALU.mult, ALU.add)
        P.ts(angk[:], angm[:], PI, None, ALU.is_gt)
        P.stt(angm[:], angk[:], -TWO_PI, angm[:], ALU.mult, ALU.add)
        P.ts(angm[:], angm[:], -PI, PI, ALU.max, ALU.min)
        P.act(dst, angm[:], AF.Sin)

    sin_of(cs_sin[:], 0.0)
    sin_of(cs_cos[:], PI / 2)

    dtb_bc = P.sb("dtb_bc", [128, 32], F32)
    a_bc = P.sb("a_bc", [128, 32], F32)
    P.dma("sp", dtb_bc[:], dtb_d.to_broadcast([128, 32]))
    P.dma("sp", a_bc[:], alog_d.to_broadcast([128, 32]))
    P.act(a_bc[:], a_bc[:], AF.Exp)
    P.ts(a_bc[:], a_bc[:], -1.0, None, ALU.mult)

    stf = [P.sb(f"stf{i}", [128, 512], F32) for i in range(2)]
    stb = [P.sb(f"stb{i}", [128, 512], BF16) for i in range(2)]
    rtmp = P.sb("rtmp", [128, 4, 4, 8], F32)
    cnt = [0]

    def tokmajor_segment(c0, ncols_total, ntiles, kind, dst):
        for b0 in range(0, ncols_total, 512):
            ncols = min(512, ncols_total - b0)
            w = load_wblock(c0 + b0, ncols)
            for i in range(ntiles):
                cnt[0] += 1
                pst = ps[cnt[0] % 4]
                for kc in range(16):
                    P.mm(pst[:, 0:ncols], nT[:, kc, i * 128:(i + 1) * 128], w[:, kc, 0:ncols],
                         start=(kc == 0), stop=(kc == 15))
                sf = stf[cnt[0] % 2]
                sbf = stb[cnt[0] % 2]
                rows = slice(i * 128, (i + 1) * 128)
                if kind == "z":
                    evac(sf[:, 0:ncols], pst[:, 0:ncols])
                    P.dma("sp", dst[rows, b0:b0 + ncols], sf[:, 0:ncols])
                elif kind == "v":
                    evac(sbf[:, 0:ncols], pst[:, 0:ncols])
                    P.dma("sp", dst[rows, b0:b0 + ncols], sbf[:, 0:ncols])
                elif kind == "qk":
                    evac(sf[:, 0:ncols], pst[:, 0:ncols])
                    v4 = sf[:, 0:512].rearrange("p (g d) -> p g d", d=64)
                    t1 = v4[:, :, 0:8]
                    t2 = v4[:, :, 8:16]
                    cb = cs_cos[:, i, :].unsqueeze(1).to_broadcast([128, 8, 8])
                    sb_ = cs_sin[:, i, :].unsqueeze(1).to_broadcast([128, 8, 8])
                    r = rtmp[:].rearrange("p a b d -> p (a b) d")
                    P.tt(r[:, 0:8, :], t1, cb, ALU.mult)
                    P.tt(r[:, 8:16, :], t2, sb_, ALU.mult)
                    r2 = rtmp2[:].rearrange("p a b d -> p (a b) d")
                    P.tt(r2[:, 0:8, :], t2, cb, ALU.mult)
                    P.tt(r2[:, 8:16, :], t1, sb_, ALU.mult)
                    P.tt(t1, r[:, 0:8, :], r[:, 8:16, :], ALU.subtract)
                    P.tt(t2, r2[:, 0:8, :], r2[:, 8:16, :], ALU.add)
                    P.copy(sbf[:, 0:ncols], sf[:, 0:ncols], eng="pool")
                    P.dma("sp", dst[rows, b0:b0 + ncols], sbf[:, 0:ncols])
                elif kind == "dt":
                    d = dtda[:, i, :]
                    P.tt(d[:, 0:32], pst[:, 0:32], dtb_bc[:], ALU.add)
                    P.act(d[:, 0:32], d[:, 0:32], AF.Exp)
                    P.act(d[:, 0:32], d[:, 0:32], AF.Ln, bias=1.0)
                    P.tt(d[:, 32:64], d[:, 0:32], a_bc[:], ALU.mult)
                    if dbg:
                        P.dma("sp", dt_d[rows, :], d)

    rtmp2 = P.sb("rtmp2", [128, 4, 4, 8], F32)

    tokmajor_segment(C_DT, 32, NT_ALL, "dt", None)
    tokmajor_segment(C_Z, 1024, NT_OWN, "z", z_d)
    tokmajor_segment(C_Q, 1024, NT_OWN, "qk", q_d)
    tokmajor_segment(C_K, 1024, NT_ALL, "qk", k_d)
    tokmajor_segment(C_V, 1024, NT_ALL, "v", v_d)

    cw = P.sb("cw", [128, 12, 5], F32)
    cbias = P.sb("cbias", [128, 12], F32)
    P.dma("sp", cw[:], convw_d)
    P.dma("sp", cbias[:], convb_d)
    xcm = [P.sb(f"xcm{i}", [128, SEQ + 4], F32) for i in range(2)]
    for b_ in xcm:
        P.memset(b_[:, 0:2], 0.0)
        P.memset(b_[:, SEQ + 2:SEQ + 4], 0.0)
    cacc = P.sb("cacc", [128, SEQ], F32)
    so = [P.sb(f"so{i}", [128, SEQ], BF16) for i in range(2)]
    tm = [P.sb(f"tm{i}", [128, NT_ALL, 128], BF16) for i in range(2)]
    xsb_v = xsb_d.rearrange("(tt p) c -> p tt c", p=128)
    for blk in range(3):
        w = load_wblock(C_XBC + blk * 512, 512)
        for cc in range(4):
            ch = blk * 4 + cc
            xb = xcm[ch % 2]
            for tb in range(4):
                cnt[0] += 1
                pst = ps[cnt[0] % 4]
                for kc in range(16):
                    P.mm(pst[:], w[:, kc, cc * 128:(cc + 1) * 128], nT[:, kc, tb * 512:(tb + 1) * 512],
                         start=(kc == 0), stop=(kc == 15))
                evac(xb[:, 2 + tb * 512:2 + (tb + 1) * 512], pst[:])
            P.ts(cacc[:], xb[:, 0:SEQ], cw[:, ch, 0:1], None, ALU.mult)
            for k in range(1, 5):
                P.stt(cacc[:], xb[:, k:k + SEQ], cw[:, ch, k:k + 1], cacc[:], ALU.mult, ALU.add)
            s_ = so[ch % 2]
            P.act(s_[:], cacc[:], AF.Silu, bias=cbias[:, ch:ch + 1])
            if ch >= 8:
                P.dma("sp", bc_d[(ch - 8) * 128:(ch - 7) * 128, :], s_[:])
            if ch < 10:
                t_ = tm[ch % 2]
                for grp in range(4):
                    pt = psb[grp % 2]
                    for j in range(4):
                        tt_ = grp * 4 + j
                        P.tr(pt[:, j * 128:(j + 1) * 128], s_[:, tt_ * 128:(tt_ + 1) * 128], ident_bf[:])
                    evac(t_[:, grp * 4:(grp + 1) * 4, :], pt[:, 0:512].rearrange("p (j t) -> p j t", j=4))
                for hh in range(2):
                    P.dma("sp", xsb_v[:, hh * 8:(hh + 1) * 8, ch * 128:(ch + 1) * 128], t_[:, hh * 8:(hh + 1) * 8, :])
    P.sb_release(mark0)
    if stages <= 2:
        P.emit()
        return P

    def load_cast_block(dst, src_v, c0, ncols, nkc):
        step = 4
        for q4 in range(0, nkc, step):
            P.dma("pool", dst[:, q4:q4 + step, 0:ncols], src_v[:, q4:q4 + step, c0:c0 + ncols])

    dskip_d = din("dskip", [1, 16])
    ssdg_d = din("ssd_g", [1, 1024])
    m3 = P.sb_mark()
    yacc = P.sb("yacc", [128, NT_OWN, 1024], F32)
    Sin = P.sb("Sin", [128, 16, 64], F32)
    Sin_bf = P.sb("Sin_bf", [128, 16, 64], BF16)
    dskip_bc = P.sb("dskip_bc", [128, 16], F32)
    ssdg_bc = P.sb("ssdg_bc", [128, 1024], F32)
    P.dma("sp", dskip_bc[:], dskip_d.to_broadcast([128, 16]))
    P.dma("sp", ssdg_bc[:], ssdg_d.to_broadcast([128, 1024]))
    negm4 = [P.sb(f"negm4_{d_}", [128, 4, 128], F32) for d_ in range(2)]
    P.copy(negm4[0][:], consts[:, K_NMU:K_NMU + 128].unsqueeze(1).to_broadcast([128, 4, 128]))
    P.copy(negm4[1][:], consts[:, K_NML:K_NML + 128].unsqueeze(1).to_broadcast([128, 4, 128]))
    tri = [consts[:, K_TRIU:K_TRIU + 128], consts[:, K_TRIL:K_TRIL + 128]]
    ones_f = consts[:, K_ONES:K_ONES + 128]
    xsb_t = [P.sb(f"xsbt{i}", [128, 1280], BF16) for i in range(2)]
    bcm_t = [P.sb(f"bcm{i}", [128, 4, 128], BF16) for i in range(2)]
    cst = P.sb("cst", [128, 32], F32)
    wst = P.sb("wst", [128, 16], F32)
    dec = P.sb("dec", [128, 16], F32)
    ecs = P.sb("ecs", [128, 16], F32)
    xd = P.sb("xd", [128, 16, 64], BF16)
    xdw = P.sb("xdw", [128, 16, 64], BF16)
    cbT = P.sb("cbT", [128, 2, 128], F32)
    datri = P.sb("datri", [128, 16, 128], F32)
    arg = P.sb("arg", [128, 16, 128], F32)
    Mbf = P.sb("Mbf", [128, 16, 128], BF16)
    ytmp = P.sb("ytmp", [128, 16, 64], F32)
    bc_v = bc_d.rearrange("(j n) t -> n j t", n=128)
    P.memset(Sin[:], 0.0)
    P.memset(Sin_bf[:], 0.0)
    it3 = [0]

    def ssd_chunk(c, d_, out):
        it3[0] += 1
        xt_ = xsb_t[it3[0] % 2]
        bt = bcm_t[it3[0] % 2]
        P.dma("sp", xt_[:], xsb_d[c * 128:(c + 1) * 128, :])
        if out:
            P.dma("sp", bt[:], bc_v[:, :, c * 128:(c + 1) * 128])
        dt_ = dtda[:, c, d_ * 16:(d_ + 1) * 16]
        da = dtda[:, c, 32 + d_ * 16:32 + (d_ + 1) * 16]
        A = ps[4]
        P.mm(A[:, 0:16], tri[d_], da)
        P.mm(A[:, 16:32], ones_f, da)
        P.copy(cst[:], A[:, 0:32])
        P.tt(wst[:], cst[:, 16:32], cst[:, 0:16], ALU.subtract)
        P.act(wst[:], wst[:], AF.Exp)
        P.act(dec[:], cst[:, 16:32], AF.Exp)
        xs3 = xt_[:, 0:1024].rearrange("p (h e) -> p h e", e=64)
        P.tt(xd[:], xs3, dt_.unsqueeze(2).to_broadcast([128, 16, 64]), ALU.mult)
        P.tt(xdw[:], xd[:], wst[:].unsqueeze(2).to_broadcast([128, 16, 64]), ALU.mult)
        if out:
            for g in range(2):
                P.mm(A[:, 256 + g * 128:256 + (g + 1) * 128], bt[:, g, :], bt[:, 2 + g, :])
            P.copy(cbT[:], A[:, 256:512].rearrange("p (g l) -> p g l", g=2))
            P.tt(datri[:], tri[d_].unsqueeze(1).to_broadcast([128, 16, 128]),
                 da.unsqueeze(2).to_broadcast([128, 16, 128]), ALU.mult)
            for j in range(4):
                R = ps[j]
                P.mm(R[:], ones_f, datri[:, 4 * j:4 * j + 4, :].rearrange("p h l -> p (h l)"), start=True, stop=False)
                P.mm(R[:], ident_f, negm4[d_][:].rearrange("p h l -> p (h l)"), start=False, stop=True)
                P.tt(arg[:, 4 * j:4 * j + 4, :], R[:].rearrange("p (h l) -> p h l", h=4),
                     cst[:, 4 * j:4 * j + 4].unsqueeze(2).to_broadcast([128, 4, 128]), ALU.subtract)
            P.act(arg[:], arg[:], AF.Exp)
            for g in range(2):
                P.tt(Mbf[:, g * 8:(g + 1) * 8, :], arg[:, g * 8:(g + 1) * 8, :],
                     cbT[:, g, :].unsqueeze(1).to_broadcast([128, 8, 128]), ALU.mult)
            for h in range(16):
                Y = ps[5 + h // 8]
                P.mm(Y[:, (h % 8) * 64:(h % 8 + 1) * 64], Mbf[:, h, :], xd[:, h, :])
            for g in range(2):
                P.mm(ps[g][:], bt[:, 2 + g, :], Sin_bf[:, g * 8:(g + 1) * 8, :].rearrange("p h e -> p (h e)"))
            P.act(ecs[:], cst[:, 0:16], AF.Exp)
            for g in range(2):
                hs = slice(g * 8, (g + 1) * 8)
                ysl = yacc[:, c, g * 512:(g + 1) * 512].rearrange("p (h e) -> p h e", e=64)
                P.tt(ytmp[:, hs, :], ps[g][:].rearrange("p (h e) -> p h e", e=64),
                     ecs[:, hs].unsqueeze(2).to_broadcast([128, 8, 64]), ALU.mult)
                P.tt(ytmp[:, hs, :], ytmp[:, hs, :], ps[5 + g][:].rearrange("p (h e) -> p h e", e=64), ALU.add)
                if d_ == 0:
                    P.tt(ysl, xs3[:, hs, :], dskip_bc[:, hs].unsqueeze(2).to_broadcast([128, 8, 64]), ALU.mult)
                P.tt(ysl, ysl, ytmp[:, hs, :], ALU.add)
        for g in range(2):
            P.mm(ps[2 + g][:], xt_[:, 1024 + g * 128:1024 + (g + 1) * 128],
                 xdw[:, g * 8:(g + 1) * 8, :].rearrange("p h e -> p (h e)"))
        P.tt(Sin[:], Sin[:], dec[:].unsqueeze(2).to_broadcast([128, 16, 64]), ALU.mult)
        for g in range(2):
            sl = Sin[:, g * 8:(g + 1) * 8, :]
            P.tt(sl, sl, ps[2 + g][:].rearrange("p (h e) -> p h e", e=64), ALU.add)
        P.copy(Sin_bf[:], Sin[:], eng="act")

    for c in range(NT_OWN):
        ssd_chunk(c, 0, True)
    P.memset(Sin[:], 0.0)
    P.memset(Sin_bf[:], 0.0)
    for c in range(NT_ALL - 1, NT_OWN - 1, -1):
        ssd_chunk(c, 1, False)
    for c in range(NT_OWN - 1, -1, -1):
        ssd_chunk(c, 1, True)
    ztile = [P.sb(f"zt{i}", [128, 1024], F32) for i in range(2)]
    gy = P.sb("gy", [128, 1024], F32)
    for c in range(NT_OWN):
        zt = ztile[c % 2]
        P.dma("sp", zt[:], z_d[c * 128:(c + 1) * 128, :])
        P.act(zt[:], zt[:], AF.Silu)
        P.tt(gy[:], yacc[:, c, :], zt[:], ALU.mult)
        rmsnorm_tile(gy[:], u_sb[:, c, 0:1024], ssdg_bc[:], ss[0], 1024)
    P.sb_release(m3)
    if stages <= 3:
        if dbg:
            u_dbg = dscr("u_s", [OWN, D], BF16)
            P.dma("sp", u_dbg.rearrange("(t p) c -> p t c", p=128), u_sb[:])
            P.barrier()
        P.emit()
        return P

    LAM_INIT = 0.8 - 0.6 * float(np.exp(-0.3 * 0))
    lamv_d = din("lamv", [1, 256])
    subg_d = din("subln_g", [1, 128])
    lam_sb = P.sb("lam_sb", [128, 256], F32)
    ls = P.sb("ls", [128, 4], F32)
    nlam = P.sb("nlam", [128, 1], F32)
    gsub_bc = P.sb("gsub_bc", [128, 128], F32)
    P.dma("sp", lam_sb[:], lamv_d.to_broadcast([128, 256]))
    P.dma("sp", gsub_bc[:], subg_d.to_broadcast([128, 128]))
    P.ts(gsub_bc[:], gsub_bc[:], 1.0 - LAM_INIT, None, ALU.mult)
    P.stt(junk[:, 0:64], lam_sb[:, 0:64], 1.0, lam_sb[:, 64:128], ALU.mult, ALU.mult, accum_out=ls[:, 0:1])
    P.stt(junk[:, 0:64], lam_sb[:, 128:192], 1.0, lam_sb[:, 192:256], ALU.mult, ALU.mult, accum_out=ls[:, 1:2])
    P.act(ls[:, 0:2], ls[:, 0:2], AF.Exp)
    P.tt(ls[:, 2:3], ls[:, 0:1], ls[:, 1:2], ALU.subtract)
    P.ts(nlam[:], ls[:, 2:3], LAM_INIT, -1.0, ALU.add, ALU.mult)
    ktm = P.sb("ktm", [128, NT_ALL, 128], BF16)
    vtm = [P.sb(f"vtm{i}", [128, NT_ALL, 128], BF16) for i in range(2)]
    qtm = P.sb("qtm", [128, NT_OWN, 128], BF16)
    kT = [P.sb(f"kT{i}", [128, SEQ], BF16) for i in range(2)]
    qT = [P.sb(f"qT{i}", [128, OWN], BF16) for i in range(2)]
    Pc = [P.sb(f"Pc{i}", [128, SEQ], F32) for i in range(2)]
    wbf = P.sb("wbf", [128, SEQ], BF16)
    wT = P.sb("wT", [128, NT_ALL, 128], BF16)
    osub = P.sb("osub", [128, 128], F32)
    mx4 = P.sb("mx4", [128, 4], F32)
    mx = P.sb("mx", [128, 2], F32)
    sm4 = P.sb("sm4", [128, 8], F32)
    sm2 = P.sb("sm2", [128, 4], F32)
    k_v = k_d.rearrange("(t p) c -> p t c", p=128)
    v_v = v_d.rearrange("(t p) c -> p t c", p=128)
    q_v = q_d.rearrange("(t p) c -> p t c", p=128)
    for h in range(8):
        cs_ = slice(h * 128, (h + 1) * 128)
        P.dma("sp", ktm[:], k_v[:, :, cs_])
        P.dma("sp", vtm[h % 2][:], v_v[:, :, cs_])
        P.dma("sp", qtm[:], q_v[:, :, cs_])
        kT_ = kT[h % 2]
        qT_ = qT[h % 2]
        for grp in range(4):
            pt = psb[grp % 2]
            for j in range(4):
                P.tr(pt[:, j * 128:(j + 1) * 128], ktm[:, grp * 4 + j, :], ident_bf[:])
            evac(kT_[:, grp * 512:(grp + 1) * 512], pt[:, 0:512])
        for grp in range(2):
            pt = psb[grp % 2]
            for j in range(4):
                P.tr(pt[:, j * 128:(j + 1) * 128], qtm[:, grp * 4 + j, :], ident_bf[:])
            evac(qT_[:, grp * 512:(grp + 1) * 512], pt[:, 0:512])
        for qt in range(NT_OWN):
            for comp in range(2):
                pr = slice(comp * 64, (comp + 1) * 64)
                for kb in range(4):
                    P.mm(ps[kb][:], qT_[pr, qt * 128:(qt + 1) * 128], kT_[pr, kb * 512:(kb + 1) * 512])
                for kb in range(4):
                    P.reduce(mx4[:, kb:kb + 1], ps[kb][:], ALU.max)
                P.reduce(mx[:, 0:1], mx4[:], ALU.max)
                P.ts(mx[:, 1:2], mx[:, 0:1], -0.125, None, ALU.mult)
                for kb in range(4):
                    P.act(Pc[comp][:, kb * 512:(kb + 1) * 512], ps[kb][:], AF.Exp, bias=mx[:, 1:2], scale=0.125,
                          accum_out=sm4[:, comp * 4 + kb:comp * 4 + kb + 1])
            P.reduce(sm2[:, 0:2], sm4[:].rearrange("p (c k) -> p c k", c=2), ALU.add)
            P.add("dve", lambda e, o=sm2[:, 0:2]: e.reciprocal(o, o), reads=[sm2[:, 0:2]], writes=[sm2[:, 0:2]])
            P.tt(sm2[:, 2:3], sm2[:, 1:2], nlam[:], ALU.mult)
            P.ts(Pc[1][:], Pc[1][:], sm2[:, 2:3], None, ALU.mult, eng="pool")
            P.stt(wbf[:], Pc[0][:], sm2[:, 0:1], Pc[1][:], ALU.mult, ALU.add)
            for grp in range(4):
                pt = psb[grp % 2]
                for j in range(4):
                    kt = grp * 4 + j
                    P.tr(pt[:, j * 128:(j + 1) * 128], wbf[:, kt * 128:(kt + 1) * 128], ident_bf[:])
                evac(wT[:, grp * 4:(grp + 1) * 4, :], pt[:, 0:512].rearrange("p (j t) -> p j t", j=4))
            for kt in range(NT_ALL):
                P.mm(ps[4][:, 0:128], wT[:, kt, :], vtm[h % 2][:, kt, :], start=(kt == 0), stop=(kt == NT_ALL - 1))
            evac(osub[:], ps[4][:, 0:128])
            rmsnorm_tile(osub[:], u_sb[:, qt, 1024 + h * 128:1024 + (h + 1) * 128], gsub_bc[:], ss[1], 128)
    P.sb_release(m3)
    if stages <= 4:
        if dbg:
            u_dbg = dscr("u_s", [OWN, D], BF16)
            P.dma("sp", u_dbg.rearrange("(t p) c -> p t c", p=128), u_sb[:])
            P.barrier()
        P.emit()
        return P

    w_out_d = din("w_out", [D, D])
    h1 = P.sb_top("h1", [128, NT_OWN, D], F32)
    m5 = P.sb_mark()
    uT = P.sb("uT", [128, 16, OWN], BF16)
    wb2 = [P.sb(f"wb2_{i}", [128, 16, 512], BF16) for i in range(2)]
    xo = [P.sb(f"xo{i}", [128, 512], F32) for i in range(2)]
    for i in range(NT_OWN):
        for grp in range(4):
            pt = psb[grp % 2]
            for j in range(4):
                kc = grp * 4 + j
                P.tr(pt[:, j * 128:(j + 1) * 128], u_sb[:, i, kc * 128:(kc + 1) * 128], ident_bf[:])
            evac(uT[:, grp * 4:(grp + 1) * 4, i * 128:(i + 1) * 128], pt[:, 0:512].rearrange("p (j t) -> p j t", j=4))
    wo_v = w_out_d.rearrange("(kc p) n -> p kc n", p=128)
    for cb in range(4):
        w = wb2[cb % 2]
        load_cast_block(w, wo_v, cb * 512, 512, 16)
        for i in range(NT_OWN):
            cnt[0] += 1
            pst = ps[cnt[0] % 4]
            xo_ = xo[cnt[0] % 2]
            P.dma("sp", xo_[:], x_d[i * 128:(i + 1) * 128, cb * 512:(cb + 1) * 512])
            for kc in range(16):
                P.mm(pst[:], uT[:, kc, i * 128:(i + 1) * 128], w[:, kc, :], start=(kc == 0), stop=(kc == 15))
            P.tt(h1[:, i, cb * 512:(cb + 1) * 512], pst[:], xo_[:], ALU.add)
    P.sb_release(m5)
    if stages <= 5:
        if dbg:
            h_dbg = dscr("h1_s", [OWN, D], F32)
            P.dma("sp", h_dbg.rearrange("(t p) c -> p t c", p=128), h1[:])
            P.barrier()
        P.emit()
        return P

    NE = 64
    CAP = 128
    gffn_d = din("g_ffn", [1, D])
    wr_d = din("w_route", [D, 72])
    br_d = din("b_route", [1, 72])
    weg_d = din("w_exp_gate", [NE, D, 512])
    weu_d = din("w_exp_up", [NE, D, 512])
    wed_d = din("w_exp_down", [NE, 512, D])
    n2_d = dscr("n2_s", [OWN + 128, D], BF16)
    rowtok_d = dscr("rowtok_s", [NE * CAP, 1], I32)
    yrows_d = dscr("yrows_s", [NE * CAP, D], F32)

    m6 = P.sb_mark()
    gates = P.sb("gates", [128, NT_OWN, 2], F32)
    dest_f = P.sb("dest_f", [128, NT_OWN, 2], F32)
    dest_i = P.sb("dest_i", [128, NT_OWN, 2], I32)
    dst2_f = P.sb("dst2_f", [128, NT_OWN, 2], F32)
    dst2_i = P.sb("dst2_i", [128, NT_OWN, 2], I32)
    rowtok = P.sb("rowtok", [128, NE], I32)
    m6r = P.sb_mark()
    gffn_bc = P.sb("gffn_bc", [128, D], F32)
    P.dma("sp", gffn_bc[:], gffn_d.to_broadcast([128, D]))
    wr_sb = P.sb("wr_sb", [128, 16, 72], F32)
    P.dma("sp", wr_sb[:], wr_d.rearrange("(kc p) n -> p kc n", p=128))
    br_bc = P.sb("br_bc", [128, 72], F32)
    P.dma("sp", br_bc[:], br_d.to_broadcast([128, 72]))
    n2f = [P.sb(f"n2f{i}", [128, D], F32) for i in range(2)]
    n2b = [P.sb(f"n2b{i}", [128, D], BF16) for i in range(2)]
    n2T = P.sb("n2T", [128, 16, 128], F32)
    A1 = P.sb("A1", [128, NT_OWN, NE], F32)
    A2 = P.sb("A2", [128, NT_OWN, NE], F32)
    Aall = P.sb("Aall", [128, NT_OWN, NE], BF16)
    lg = P.sb("lg", [128, 72], F32)
    rt = P.sb("rt", [128, 16], F32)
    Gm = P.sb("Gm", [128, 8], F32)
    tmp88 = P.sb("tmp88", [128, 8, 8], F32)
    esel = P.sb("esel", [128, 8], F32)
    e2 = P.sb("e2", [128, 8], F32)
    mk1 = P.sb("mk1", [128, 8], F32)
    mk2 = P.sb("mk2", [128, 8], F32)
    zrow = P.sb("zrow", [128, D], BF16)
    P.memset(zrow[:], 0.0)
    P.dma("sp", n2_d[OWN:OWN + 128, :], zrow[:])
    rinit = P.sb("rinit", [128, NE], I32)
    P.memset(rinit[:], OWN)
    P.dma("sp", rowtok_d.rearrange("(r e) o -> r (e o)", e=NE), rinit[:])
    ones_bf = P.sb("ones_bf", [128, 128], BF16)
    slt_bf = P.sb("slt_bf", [128, 128], BF16)
    P.copy(ones_bf[:], consts[:, K_ONES:K_ONES + 128])
    P.copy(slt_bf[:], consts[:, K_SLT:K_SLT + 128])
    iota_e = consts[:, K_IOTA:K_IOTA + 64]

    for i in range(NT_OWN):
        nf = n2f[i % 2]
        nbb = n2b[i % 2]
        P.stt(junk[:], h1[:, i, :], 1.0, h1[:, i, :], ALU.mult, ALU.mult, accum_out=ss[0][:, 0:1])
        P.act(ss[0][:, 1:2], ss[0][:, 0:1], AF.Sqrt, bias=eps_t[:, 0:1], scale=1.0 / D)
        P.add("dve", lambda e, o=ss[0][:, 1:2]: e.reciprocal(o, o), reads=[ss[0][:, 1:2]], writes=[ss[0][:, 1:2]])
        P.stt(nf[:], h1[:, i, :], ss[0][:, 1:2], gffn_bc[:], ALU.mult, ALU.mult)
        P.copy(nbb[:], nf[:], eng="pool")
        P.dma("sp", n2_d[i * 128:(i + 1) * 128, :], nbb[:])
        for grp in range(4):
            pt = ps[grp % 2]
            for j in range(4):
                kc = grp * 4 + j
                P.tr(pt[:, j * 128:(j + 1) * 128], nf[:, kc * 128:(kc + 1) * 128], ident_f)
            evac(n2T[:, grp * 4:(grp + 1) * 4, :], pt[:].rearrange("p (j t) -> p j t", j=4))
        for kc in range(16):
            P.mm(ps[2][:, 0:72], n2T[:, kc, :], wr_sb[:, kc, :], start=(kc == 0), stop=(kc == 15))
        P.tt(lg[:], ps[2][:, 0:72], br_bc[:], ALU.add)
        P.reduce(rt[:, 0:1], lg[:, 0:8], ALU.max)
        P.ts(Gm[:], lg[:, 0:8], rt[:, 0:1], None, ALU.is_equal)
        P.ts(rt[:, 1:2], rt[:, 0:1], -1.0, None, ALU.mult)
        P.act(tmp88[:, 0, :], lg[:, 0:8], AF.Exp, bias=rt[:, 1:2], accum_out=rt[:, 2:3])
        P.add("dve", lambda e, o=rt[:, 2:3]: e.reciprocal(o, o), reads=[rt[:, 2:3]], writes=[rt[:, 2:3]])
        P.tt(tmp88[:], lg[:, 8:72].rearrange("p (g e) -> p g e", g=8), Gm[:].unsqueeze(2).to_broadcast([128, 8, 8]), ALU.mult)
        P.reduce(esel[:], tmp88[:].rearrange("p g e -> p e g"), ALU.add)
        P.reduce(rt[:, 3:4], esel[:], ALU.max)
        P.ts(mk1[:], esel[:], rt[:, 3:4], None, ALU.is_equal)
        P.stt(e2[:], mk1[:], NEG, esel[:], ALU.mult, ALU.add)
        P.reduce(rt[:, 4:5], e2[:], ALU.max)
        P.ts(mk2[:], e2[:], rt[:, 4:5], None, ALU.is_equal)
        P.tt(rt[:, 5:6], rt[:, 4:5], rt[:, 3:4], ALU.subtract)
        P.act(rt[:, 5:6], rt[:, 5:6], AF.Exp)
        P.ts(rt[:, 5:6], rt[:, 5:6], 1.0, None, ALU.add)
        P.add("dve", lambda e, o=rt[:, 5:6]: e.reciprocal(o, o), reads=[rt[:, 5:6]], writes=[rt[:, 5:6]])
        P.ts(rt[:, 6:7], rt[:, 5:6], -1.0, 1.0, ALU.mult, ALU.add)
        P.tt(gates[:, i, 0:1], rt[:, 5:6], rt[:, 2:3], ALU.mult)
        P.tt(gates[:, i, 1:2], rt[:, 6:7], rt[:, 2:3], ALU.mult)
        P.tt(A1[:, i, :].rearrange("p (g e) -> p g e", g=8), Gm[:].unsqueeze(2).to_broadcast([128, 8, 8]),
             mk1[:].unsqueeze(1).to_broadcast([128, 8, 8]), ALU.mult)
        P.tt(A2[:, i, :].rearrange("p (g e) -> p g e", g=8), Gm[:].unsqueeze(2).to_broadcast([128, 8, 8]),
             mk2[:].unsqueeze(1).to_broadcast([128, 8, 8]), ALU.mult)
        P.tt(Aall[:, i, :], A1[:, i, :], A2[:, i, :], ALU.add)

    tokidx = P.sb("tokidx", [128, NT_OWN], I32)
    tokf = P.sb("tokf", [128, NT_OWN], F32)
    rk = P.sb("rk", [128, NE], F32)
    ovf = P.sb("ovf", [128, NE], F32)
    for i in range(NT_OWN):
        P.ts(tokf[:, i:i + 1], consts[:, K_IOTA:K_IOTA + 1], 1.0, float(i * 128), ALU.mult, ALU.add)
    pidx = P.sb("pidx", [128, 1], F32)
    P.reduce(pidx[:], consts[:, K_SLT:K_SLT + 128].rearrange("p l -> p l"), ALU.add)
    P.ts(pidx[:], pidx[:], -1.0, 127.0, ALU.mult, ALU.add)
    for i in range(NT_OWN):
        P.tt(tokf[:, i:i + 1], tokf[:, i:i + 1], pidx[:], ALU.add)
    P.copy(tokidx[:], tokf[:])
    for i in range(NT_OWN):
        R = ps[3]
        for j in range(i):
            P.mm(R[:, 0:NE], ones_bf[:], Aall[:, j, :], start=(j == 0), stop=False)
        P.mm(R[:, 0:NE], slt_bf[:], Aall[:, i, :], start=(i == 0), stop=True)
        P.ts(ovf[:], R[:, 0:NE], float(CAP), 1.0e6, ALU.is_ge, ALU.mult)
        P.stt(rk[:], iota_e, float(CAP), R[:, 0:NE], ALU.mult, ALU.add)
        P.tt(rk[:], rk[:], ovf[:], ALU.add)
        P.tt(ovf[:], rk[:], A1[:, i, :], ALU.mult)
        P.reduce(dest_f[:, i, 0:1], ovf[:], ALU.add)
        P.tt(ovf[:], rk[:], A2[:, i, :], ALU.mult)
        P.reduce(dest_f[:, i, 1:2], ovf[:], ALU.add)
        P.ts(ovf[:], R[:, 0:NE], float(CAP), 1.0e6, ALU.is_ge, ALU.mult)
        P.stt(rk[:], R[:, 0:NE], float(NE), iota_e, ALU.mult, ALU.add)
        P.tt(rk[:], rk[:], ovf[:], ALU.add)
        P.tt(ovf[:], rk[:], A1[:, i, :], ALU.mult)
        P.reduce(dst2_f[:, i, 0:1], ovf[:], ALU.add)
        P.tt(ovf[:], rk[:], A2[:, i, :], ALU.mult)
        P.reduce(dst2_f[:, i, 1:2], ovf[:], ALU.add)
    P.copy(dest_i[:], dest_f[:])
    P.copy(dst2_i[:], dst2_f[:])
    for i in range(NT_OWN):
        for k_ in range(2):
            P.add("pool", lambda e, i=i, k_=k_: e.indirect_dma_start(
                out=rowtok_d, out_offset=bass.IndirectOffsetOnAxis(ap=dst2_i[:, i, k_:k_ + 1], axis=0),
                in_=tokidx[:, i:i + 1], in_offset=None, bounds_check=NE * CAP - 1, oob_is_err=False),
                reads=[dst2_i[:, i, k_:k_ + 1], tokidx[:, i:i + 1]], writes=[rowtok_d], dma=True)
    P.dma("sp", rowtok[:], rowtok_d.rearrange("(r e) o -> r (e o)", e=NE))
    P.sb_release(m6r)
    m6b = P.sb_mark()
    wg = [P.sb(f"wg{i}", [128, 16, 512], BF16) for i in range(2)]
    wu = [P.sb(f"wu{i}", [128, 16, 512], BF16) for i in range(2)]
    wd = [P.sb_at(f"wd{i}", [128, 4, D], BF16, U_OFF + i * 16384) for i in range(2)]
    xe = [P.sb(f"xe{i}", [128, D], BF16) for i in range(2)]
    xeT = P.sb("xeT", [128, 16, 128], BF16)
    hg = P.sb("hg", [128, 512], F32)
    hact = P.sb("hact", [128, 512], BF16)
    actT = P.sb("actT", [128, 4, 128], BF16)
    ye = [P.sb(f"ye{i}", [128, D], F32) for i in range(2)]
    for e_ in range(NE):
        b_ = e_ % 2
        load_cast_block(wg[b_], weg_d[e_].rearrange("(kc p) f -> p kc f", p=128), 0, 512, 16)
        load_cast_block(wu[b_], weu_d[e_].rearrange("(kc p) f -> p kc f", p=128), 0, 512, 16)
        wdv = wed_d[e_].rearrange("(kc p) n -> p kc n", p=128)
        for kc in range(4):
            P.dma("pool", wd[b_][:, kc:kc + 1, :], wdv[:, kc:kc + 1, :])
        P.add("pool", lambda e, e_=e_, b_=b_: e.indirect_dma_start(
            out=xe[b_][:], out_offset=None, in_=n2_d,
            in_offset=bass.IndirectOffsetOnAxis(ap=rowtok[:, e_:e_ + 1], axis=0)),
            reads=[n2_d, rowtok[:, e_:e_ + 1]], writes=[xe[b_][:]], dma=True)
        for grp in range(4):
            pt = psb[grp % 2]
            for j in range(4):
                kc = grp * 4 + j
                P.tr(pt[:, j * 128:(j + 1) * 128], xe[b_][:, kc * 128:(kc + 1) * 128], ident_bf[:])
            evac(xeT[:, grp * 4:(grp + 1) * 4, :], pt[:, 0:512].rearrange("p (j t) -> p j t", j=4))
        for kc in range(16):
            P.mm(ps[0][:], xeT[:, kc, :], wg[b_][:, kc, :], start=(kc == 0), stop=(kc == 15))
        for kc in range(16):
            P.mm(ps[1][:], xeT[:, kc, :], wu[b_][:, kc, :], start=(kc == 0), stop=(kc == 15))
        P.act(hg[:], ps[0][:], AF.Silu)
        P.tt(hact[:], hg[:], ps[1][:], ALU.mult)
        pt = psb[0]
        for kc in range(4):
            P.tr(pt[:, kc * 128:(kc + 1) * 128], hact[:, kc * 128:(kc + 1) * 128], ident_bf[:])
        evac(actT[:], pt[:, 0:512].rearrange("p (j t) -> p j t", j=4))
        for cb in range(4):
            Yb = ps[2 + cb]
            for kc in range(4):
                P.mm(Yb[:], actT[:, kc, :], wd[b_][:, kc, cb * 512:(cb + 1) * 512], start=(kc == 0), stop=(kc == 3))
            evac(ye[b_][:, cb * 512:(cb + 1) * 512], Yb[:])
        P.dma("sp", yrows_d[e_ * CAP:(e_ + 1) * CAP, :], ye[b_][:])
    P.sb_release(m6b)
    yg = [P.sb(f"yg{i}", [128, D], F32) for i in range(2)]
    for i in range(NT_OWN):
        for k_ in range(2):
            y_ = yg[k_]
            P.memset(y_[:], 0.0, eng="pool")
            P.add("pool", lambda e, i=i, k_=k_, y_=y_: e.indirect_dma_start(
                out=y_[:], out_offset=None, in_=yrows_d,
                in_offset=bass.IndirectOffsetOnAxis(ap=dest_i[:, i, k_:k_ + 1], axis=0),
                bounds_check=NE * CAP - 1, oob_is_err=False),
                reads=[yrows_d, dest_i[:, i, k_:k_ + 1]], writes=[y_[:]], dma=True)
            P.stt(h1[:, i, :], y_[:], gates[:, i, k_:k_ + 1], h1[:, i, :], ALU.mult, ALU.add)
    P.sb_release(m6)
    if stages <= 6:
        if dbg:
            h_dbg = dscr("h2_s", [OWN, D], F32)
            P.dma("sp", h_dbg.rearrange("(t p) c -> p t c", p=128), h1[:])
            P.barrier()
        P.emit()
        return P

    p_d = din("p", [OWN, 256])
    wpp_d = din("w_ple_proj", [256, D])
    pleg_d = din("ple_g", [1, D])
    wpg_d = din("w_ple_gate", [D, D])
    bpg_d = din("b_ple_gate", [1, D])
    fing_d = din("final_g", [1, D])
    wpg = P.sb("wpg", [128, 16, D], BF16)
    wpg_v = wpg_d.rearrange("(kc p) n -> p kc n", p=128)
    for cb in range(4):
        for q4 in range(0, 16, 4):
            P.dma("pool", wpg[:, q4:q4 + 4, cb * 512:(cb + 1) * 512], wpg_v[:, q4:q4 + 4, cb * 512:(cb + 1) * 512])
    wpp = P.sb("wpp", [128, 2, D], BF16)
    P.dma("pool", wpp[:], wpp_d.rearrange("(kc p) n -> p kc n", p=128))
    pleg_bc = P.sb_at("pleg_bc", [128, D], F32, U_OFF)
    bpg_bc = P.sb_at("bpg_bc", [128, D], F32, U_OFF + 8192)
    fing_bc = P.sb_at("fing_bc", [128, D], F32, U_OFF + 16384)
    P.dma("sp", pleg_bc[:], pleg_d.to_broadcast([128, D]))
    P.dma("sp", bpg_bc[:], bpg_d.to_broadcast([128, D]))
    P.dma("sp", fing_bc[:], fing_d.to_broadcast([128, D]))
    hb = P.sb("hb", [128, D], BF16)
    hT = P.sb("hT", [128, 16, 128], BF16)
    pt_f = P.sb("pt_f", [128, 256], F32)
    pt_b = P.sb("pt_b", [128, 256], BF16)
    pT = P.sb("pT", [128, 2, 128], BF16)
    gate_sb = P.sb("gate_sb", [128, D], F32)
    ple_raw = P.sb("ple_raw", [128, D], F32)
    ple_n = ple_raw
    osb = [gate_sb, gate_sb]
    for i in range(NT_OWN):
        hrow = h1[:, i, :]
        P.copy(hb[:], hrow, eng="pool")
        for grp in range(4):
            pt = psb[grp % 2]
            for j in range(4):
                kc = grp * 4 + j
                P.tr(pt[:, j * 128:(j + 1) * 128], hb[:, kc * 128:(kc + 1) * 128], ident_bf[:])
            evac(hT[:, grp * 4:(grp + 1) * 4, :], pt[:, 0:512].rearrange("p (j t) -> p j t", j=4))
        P.dma("sp", pt_f[:], p_d[i * 128:(i + 1) * 128, :])
        P.copy(pt_b[:], pt_f[:])
        ptp = psb[0]
        for kc in range(2):
            P.tr(ptp[:, kc * 128:(kc + 1) * 128], pt_b[:, kc * 128:(kc + 1) * 128], ident_bf[:])
        evac(pT[:], ptp[:, 0:256].rearrange("p (j t) -> p j t", j=2))
        for cb in range(4):
            cs_ = slice(cb * 512, (cb + 1) * 512)
            G = ps[cb % 2]
            for kc in range(16):
                P.mm(G[:], hT[:, kc, :], wpg[:, kc, cs_], start=(kc == 0), stop=(kc == 15))
            P.tt(gate_sb[:, cs_], G[:], bpg_bc[:, cs_], ALU.add)
            Lp = ps[2 + cb % 2]
            for kc in range(2):
                P.mm(Lp[:], pT[:, kc, :], wpp[:, kc, cs_], start=(kc == 0), stop=(kc == 1))
            evac(ple_raw[:, cs_], Lp[:])
        P.act(gate_sb[:], gate_sb[:], AF.Sigmoid)
        P.stt(junk[:], ple_raw[:], 1.0, ple_raw[:], ALU.mult, ALU.mult, accum_out=ss[0][:, 0:1])
        P.act(ss[0][:, 1:2], ss[0][:, 0:1], AF.Sqrt, bias=eps_t[:, 0:1], scale=1.0 / D)
        P.add("dve", lambda e, o=ss[0][:, 1:2]: e.reciprocal(o, o), reads=[ss[0][:, 1:2]], writes=[ss[0][:, 1:2]])
        P.stt(ple_n[:], ple_raw[:], ss[0][:, 1:2], pleg_bc[:], ALU.mult, ALU.mult)
        P.tt(ple_n[:], ple_n[:], gate_sb[:], ALU.mult)
        P.tt(hrow, hrow, ple_n[:], ALU.add)
        o_ = osb[i % 2]
        P.stt(junk[:], hrow, 1.0, hrow, ALU.mult, ALU.mult, accum_out=ss[1][:, 0:1])
        P.act(ss[1][:, 1:2], ss[1][:, 0:1], AF.Sqrt, bias=eps_t[:, 0:1], scale=1.0 / D)
        P.add("dve", lambda e, o=ss[1][:, 1:2]: e.reciprocal(o, o), reads=[ss[1][:, 1:2]], writes=[ss[1][:, 1:2]])
        P.stt(o_[:], hrow, ss[1][:, 1:2], fing_bc[:], ALU.mult, ALU.mult)
        P.dma("sp", out_d[i * 128:(i + 1) * 128, :], o_[:])
    P.barrier()
    P.emit()
    return P


def prep_core_inputs(inp, c, shared):
    b, half = c // 2, c % 2
    flip = half == 1
    m = {}
    xb = inp["x"][b]
    m["x"] = np.ascontiguousarray(xb[::-1] if flip else xb)
    pos = inp["positions"][b]
    pos = pos[::-1] if flip else pos
    m["pos"] = np.ascontiguousarray(pos.reshape(NT_ALL, 128).T).astype(np.int32)
    m["consts"] = shared["consts"]
    m["g_mix"] = np.ascontiguousarray(inp["norm_mix_g"][0][None, :])
    m["w_in"] = shared["w_in_flip"] if flip else shared["w_in"]
    cw = inp["conv_w"][0]
    if flip:
        cw = cw[::-1]
    m["convw"] = np.ascontiguousarray(cw.T.reshape(12, 128, 5).transpose(1, 0, 2))
    m["convb"] = np.ascontiguousarray(inp["conv_b"][0].reshape(12, 128).T)
    f, r = ("dt_bias_b", "dt_bias_f") if flip else ("dt_bias_f", "dt_bias_b")
    m["dtb"] = np.concatenate([inp[f][0], inp[r][0]])[None, :].astype(np.float32)
    f, r = ("a_log_b", "a_log_f") if flip else ("a_log_f", "a_log_b")
    m["alog"] = np.concatenate([inp[f][0], inp[r][0]])[None, :].astype(np.float32)
    return m


def prep_shared(inp):
    sh = {}
    sh["consts"] = host_consts()
    w = np.ascontiguousarray(inp["w_in"][0])
    sh["w_in"] = w
    wf = w.copy()
    wf[:, C_DT:C_DT + 16] = w[:, C_DT + 16:C_DT + 32]
    wf[:, C_DT + 16:C_DT + 32] = w[:, C_DT:C_DT + 16]
    sh["w_in_flip"] = wf
    return sh


def prep_core_inputs_full(inp, c, shared):
    m = prep_core_inputs(inp, c, shared)
    b, half = c // 2, c % 2
    flip = half == 1
    m["dskip"] = np.ascontiguousarray(inp["d_skip"][0][None, :])
    m["ssd_g"] = np.ascontiguousarray(inp["ssd_norm_g"][0][None, :])
    m["lamv"] = shared["lamv"]
    m["subln_g"] = np.ascontiguousarray(inp["subln_g"][0][None, :])
    m["w_out"] = shared["w_out"]
    m["g_ffn"] = np.ascontiguousarray(inp["norm_ffn_g"][0][None, :])
    m["w_route"] = shared["w_route"]
    m["b_route"] = shared["b_route"]
    m["w_exp_gate"] = shared["w_exp_gate"]
    m["w_exp_up"] = shared["w_exp_up"]
    m["w_exp_down"] = shared["w_exp_down"]
    pb = inp["p"][0, b]
    pb = pb[::-1] if flip else pb
    m["p"] = np.ascontiguousarray(pb[:OWN])
    m["w_ple_proj"] = shared["w_ple_proj"]
    m["ple_g"] = np.ascontiguousarray(inp["ple_norm_g"][0][None, :])
    m["w_ple_gate"] = shared["w_ple_gate"]
    m["b_ple_gate"] = np.ascontiguousarray(inp["b_ple_gate"][0][None, :])
    m["final_g"] = np.ascontiguousarray(inp["final_norm_g"][None, :])
    return m


def prep_shared_full(inp):
    sh = prep_shared(inp)
    sh["lamv"] = np.concatenate([inp["lam_q1"][0], inp["lam_k1"][0], inp["lam_q2"][0], inp["lam_k2"][0]])[None, :].astype(np.float32)
    sh["w_out"] = np.ascontiguousarray(inp["w_out"][0])
    sh["w_route"] = np.ascontiguousarray(np.concatenate([inp["w_route_group"][0], inp["w_route_expert"][0]], axis=1))
    sh["b_route"] = np.concatenate([inp["b_route_group"][0], inp["b_route_expert"][0]])[None, :].astype(np.float32)
    sh["w_exp_gate"] = np.ascontiguousarray(inp["w_exp_gate"][0])
    sh["w_exp_up"] = np.ascontiguousarray(inp["w_exp_up"][0])
    sh["w_exp_down"] = np.ascontiguousarray(inp["w_exp_down"][0])
    sh["w_ple_proj"] = np.ascontiguousarray(inp["w_ple_proj"][0])
    sh["w_ple_gate"] = np.ascontiguousarray(inp["w_ple_gate"][0])
    return sh


def kernel(**inputs):
    inp = {k: np.asarray(v) for k, v in inputs.items()}
    nc = bass.Bass("TRN2", target_bir_lowering=False)
    build_program(nc, stages=99, dbg=False)
    sh = prep_shared_full(inp)
    maps = [prep_core_inputs_full(inp, c, sh) for c in range(8)]
    res = run_bass_kernel_spmd(nc, maps, core_ids=list(range(8)))
    out = np.zeros((4, SEQ, D), np.float32)
    for c in range(8):
        b, half = c // 2, c % 2
        o = np.asarray(res.results[c]["out"])
        if half == 0:
            out[b, 0:OWN] = o
        else:
            out[b, OWN:SEQ] = o[::-1]
    return out
```

```python
import numpy as np
import concourse.bass as bass
import concourse.mybir as mybir
from concourse.bass_utils import run_bass_kernel_spmd

F32 = mybir.dt.float32
BF16 = mybir.dt.bfloat16
I32 = mybir.dt.int32
AF = mybir.ActivationFunctionType
ALU = mybir.AluOpType
AX = mybir.AxisListType

SBUF_LO = 16512
SBUF_HI = 229344


class _Op:
    __slots__ = ("eng", "fn", "dma", "deps", "flag", "ordinal", "sem", "semval", "idx", "extra_waits")

    def __init__(self, eng, fn, dma):
        self.eng = eng
        self.fn = fn
        self.dma = dma
        self.deps = set()
        self.flag = False
        self.ordinal = -1
        self.sem = None
        self.semval = 0
        self.idx = -1
        self.extra_waits = None


def _box(ap):
    t = ap.tensor
    name = t.name
    pairs = [(int(s), int(c)) for s, c in ap.ap]
    off = int(ap.offset)
    cls = type(t).__name__
    if cls.startswith("DRam"):
        lo = off
        hi = off
        for s, c in pairs:
            if s >= 0:
                hi += s * (c - 1)
            else:
                lo += s * (c - 1)
        return (name, 0, 1, lo, hi)
    shape = [int(v) for v in t.shape]
    rowlen = 1
    for v in shape[1:]:
        rowlen *= v
    esz = 4 if t.dtype in (F32, I32) else 2
    p0 = off // rowlen
    lo = off % rowlen
    ps, pc = pairs[0]
    p1 = p0 + (pc - 1) * (ps // rowlen) + 1
    hi = lo
    for s, c in pairs[1:]:
        hi += abs(s) * (c - 1)
    return (name, p0, p1, lo * esz, (hi + 1) * esz - 1)


def _overlap(a, b):
    return a[1] < b[2] and b[1] < a[2] and a[3] <= b[4] and b[3] <= a[4]


def _covers(a, b):
    return a[1] <= b[1] and a[2] >= b[2] and a[3] <= b[3] and a[4] >= b[4]


class Prog:
    ENGS = ("pe", "act", "dve", "pool", "sp")
    NDMA = 8

    def __init__(self, nc):
        self.nc = nc
        self.ops = []
        self.live = {}
        self.per_eng = {e: [] for e in self.ENGS}
        self.dma_count = {e: 0 for e in self.ENGS}
        self.dma_hist = {e: [] for e in self.ENGS}
        self.sb_ptr = SBUF_LO
        self.sb_names = 0
        self.psum = []

    def sb(self, name, shape, dtype):
        esz = 4 if dtype in (F32, I32) else 2
        n = 1
        for v in shape[1:]:
            n *= v
        nbytes = (n * esz + 31) // 32 * 32
        off = self.sb_ptr
        if off + nbytes > getattr(self, "sb_hi", SBUF_HI):
            raise RuntimeError(f"SBUF overflow allocating {name}: {off}+{nbytes}")
        self.sb_ptr += nbytes
        self.sb_names += 1
        return self.nc.alloc_sbuf_tensor_at(f"{name}_{self.sb_names}", list(shape), dtype, offset=off)

    def sb_top(self, name, shape, dtype):
        esz = 4 if dtype in (F32, I32) else 2
        n = 1
        for v in shape[1:]:
            n *= v
        nbytes = (n * esz + 31) // 32 * 32
        self.sb_hi = getattr(self, "sb_hi", SBUF_HI) - nbytes
        self.sb_names += 1
        return self.nc.alloc_sbuf_tensor_at(f"{name}_{self.sb_names}", list(shape), dtype, offset=self.sb_hi)

    def sb_at(self, name, shape, dtype, offset):
        self.sb_names += 1
        return self.nc.alloc_sbuf_tensor_at(f"{name}_{self.sb_names}", list(shape), dtype, offset=offset)

    def sb_mark(self):
        return self.sb_ptr

    def sb_release(self, mark):
        self.barrier()
        self.sb_ptr = mark

    def add(self, eng, fn, reads=(), writes=(), dma=False):
        op = _Op(eng, fn, dma)
        op.idx = len(self.ops)
        self.ops.append(op)
        self.per_eng[eng].append(op)
        for ap in reads:
            b = _box(ap)
            lst = self.live.setdefault(b[0], [])
            for ent in lst:
                if ent[2] and _overlap(ent[0], b):
                    op.deps.add(ent[1])
            if not dma:
                for ent in lst:
                    if (not ent[2]) and ent[0] == b and ent[1].eng == eng and not ent[1].dma:
                        ent[1] = op
                        break
                else:
                    lst.append([b, op, False])
            else:
                lst.append([b, op, False])
        for ap in writes:
            b = _box(ap)
            lst = self.live.setdefault(b[0], [])
            keep = []
            for ent in lst:
                if _overlap(ent[0], b):
                    if ent[1] is not op:
                        op.deps.add(ent[1])
                    if _covers(b, ent[0]):
                        continue
                keep.append(ent)
            keep.append([b, op, True])
            self.live[b[0]] = keep
        if dma:
            j = self.dma_count[eng]
            self.dma_count[eng] = j + 1
            op.sem = ("dma", eng, j % self.NDMA)
            op.semval = 16 * (j // self.NDMA + 1)
            hist = self.dma_hist[eng]
            if j >= self.NDMA:
                op.deps.add(hist[j - self.NDMA])
            hist.append(op)
        return op

    def barrier(self):
        last = {e: (self.per_eng[e][-1] if self.per_eng[e] else None) for e in self.ENGS}
        dmas = []
        for e in self.ENGS:
            dmas.extend(self.dma_hist[e][-self.NDMA:])
        for e in self.ENGS:
            op = _Op(e, None, False)
            op.idx = len(self.ops)
            self.ops.append(op)
            self.per_eng[e].append(op)
            for e2 in self.ENGS:
                if last[e2] is not None and not last[e2].dma:
                    op.deps.add(last[e2])
                elif last[e2] is not None:
                    for o in reversed(self.per_eng[e2][:-1]):
                        if not o.dma:
                            op.deps.add(o)
                            break
            for d in dmas:
                op.deps.add(d)
        self.live = {}

    def emit(self):
        nc = self.nc
        for op in self.ops:
            for d in op.deps:
                if d.dma:
                    continue
                if d.eng == "pe" and op.eng == "pe" and not op.dma and op.fn is not None:
                    continue
                d.flag = True
        counts = {e: 0 for e in self.ENGS}
        for e in self.ENGS:
            for op in self.per_eng[e]:
                if op.dma:
                    continue
                if op.flag:
                    if op.fn is None:
                        op.flag = False
                        continue
                    counts[e] += 1
                    op.ordinal = counts[e]
        import contextlib
        with contextlib.ExitStack() as st:
            sems = {}
            for e in self.ENGS:
                if counts[e] > 0:
                    sems[("eng", e)] = st.enter_context(nc.semaphore(f"c_{e}"))
                if self.dma_count[e] > 0:
                    for k in range(min(self.NDMA, self.dma_count[e])):
                        sems[("dma", e, k)] = st.enter_context(nc.semaphore(f"d_{e}{k}"))
            block = st.enter_context(nc.Block())
            engobj = {}

            def run_engine(e, eng):
                waited = {}
                for op in self.per_eng[e]:
                    need = {}
                    for d in op.deps:
                        if d.dma:
                            key, val = d.sem, d.semval
                        else:
                            if d.eng == "pe" and e == "pe" and not op.dma and op.fn is not None:
                                continue
                            if d.ordinal < 0:
                                continue
                            key, val = ("eng", d.eng), d.ordinal
                        if need.get(key, 0) < val:
                            need[key] = val
                    for key, val in need.items():
                        if waited.get(key, 0) >= val:
                            continue
                        waited[key] = val
                        eng.wait_ge(sems[key], val)
                    if op.fn is None:
                        continue
                    ins = op.fn(eng)
                    if op.dma:
                        ins.then_inc(sems[op.sem], 16)
                    elif op.flag:
                        ins.then_inc(sems[("eng", e)], 1)

            @block.tensor
            def _(eng):
                run_engine("pe", eng)

            @block.scalar
            def _(eng):
                run_engine("act", eng)

            @block.vector
            def _(eng):
                run_engine("dve", eng)

            @block.gpsimd
            def _(eng):
                run_engine("pool", eng)

            @block.sync
            def _(eng):
                run_engine("sp", eng)

    def dma(self, q, out, in_, **kw):
        return self.add(q, lambda eng: eng.dma_start(out=out, in_=in_, **kw), reads=[in_], writes=[out], dma=True)

    def mm(self, out, lhsT, rhs, start=True, stop=True, **kw):
        rd = [lhsT, rhs]
        return self.add("pe", lambda eng: eng.matmul(out, lhsT, rhs, start=start, stop=stop, **kw),
                        reads=rd, writes=[out])

    def tr(self, out, in_, ident):
        return self.add("pe", lambda eng: eng.transpose(out, in_, ident), reads=[in_, ident], writes=[out])

    def act(self, out, in_, func, bias=None, scale=None, accum_out=None, eng="act"):
        kw = {}
        rd = [in_]
        wr = [out]
        if bias is not None:
            kw["bias"] = bias
            if not isinstance(bias, (int, float)):
                rd.append(bias)
        if scale is not None:
            kw["scale"] = scale
            if not isinstance(scale, (int, float)):
                rd.append(scale)
        if accum_out is not None:
            kw["accum_out"] = accum_out
            wr.append(accum_out)
        return self.add(eng, lambda e: e.activation(out, in_, func, **kw), reads=rd, writes=wr)

    def tt(self, out, in0, in1, op, eng="dve"):
        return self.add(eng, lambda e: e.tensor_tensor(out, in0, in1, op), reads=[in0, in1], writes=[out])

    def ts(self, out, in0, s1, s2, op0, op1=None, eng="dve", accum_out=None):
        rd = [in0]
        if not isinstance(s1, (int, float)):
            rd.append(s1)
        if s2 is not None and not isinstance(s2, (int, float)):
            rd.append(s2)
        wr = [out]
        kw = {}
        if accum_out is not None:
            kw["accum_out"] = accum_out
            wr.append(accum_out)
        if op1 is None:
            return self.add(eng, lambda e: e.tensor_scalar(out, in0, s1, None, op0, **kw), reads=rd, writes=wr)
        return self.add(eng, lambda e: e.tensor_scalar(out, in0, s1, s2, op0, op1, **kw), reads=rd, writes=wr)

    def stt(self, out, in0, scalar, in1, op0, op1, eng="dve", accum_out=None):
        rd = [in0, in1]
        if not isinstance(scalar, (int, float)):
            rd.append(scalar)
        wr = [out]
        kw = {}
        if accum_out is not None:
            kw["accum_out"] = accum_out
            wr.append(accum_out)
        return self.add(eng, lambda e: e.scalar_tensor_tensor(out, in0, scalar, in1, op0, op1, **kw),
                        reads=rd, writes=wr)

    def copy(self, out, in_, eng="dve"):
        if eng == "act":
            return self.add("act", lambda e: e.copy(out, in_), reads=[in_], writes=[out])
        return self.add(eng, lambda e: e.tensor_copy(out, in_), reads=[in_], writes=[out])

    def reduce(self, out, in_, op, axis=AX.X, eng="dve"):
        return self.add(eng, lambda e: e.tensor_reduce(out, in_, axis, op), reads=[in_], writes=[out])

    def memset(self, ap, val, eng="dve"):
        return self.add(eng, lambda e: e.memset(ap, val), reads=[], writes=[ap])


D = 2048
SEQ = 2048
OWN = 1024
NT_ALL = 16
NT_OWN = 8
IN_W = 5664
EPS = 1e-6
C_Z = 0
C_XBC = 1024
C_DT = 2560
C_Q = 2592
C_K = 3616
C_V = 4640
NEG = -1.0e30

K_IDENT = 0
K_TRIU = 128
K_TRIL = 256
K_ONES = 384
K_NMU = 512
K_NML = 640
K_SLT = 768
K_DELTA = 896
K_INVF = 912
K_IOTA = 920
NCONST = 984


def host_consts():
    c = np.zeros((128, NCONST), np.float32)
    s = np.arange(128)[:, None]
    l = np.arange(128)[None, :]
    c[:, K_IDENT:K_IDENT + 128] = (s == l)
    c[:, K_TRIU:K_TRIU + 128] = (s <= l)
    c[:, K_TRIL:K_TRIL + 128] = (s >= l)
    c[:, K_ONES:K_ONES + 128] = 1.0
    c[:, K_NMU:K_NMU + 128] = np.where(s <= l, 0.0, NEG)
    c[:, K_NML:K_NML + 128] = np.where(s >= l, 0.0, NEG)
    c[:, K_SLT:K_SLT + 128] = (s < l)
    c[:16, K_DELTA:K_DELTA + 16] = np.eye(16)
    inv_freq = (500000.0 ** (-np.arange(0, 16, 2, dtype=np.float32) / 16)).astype(np.float32)
    c[:, K_INVF:K_INVF + 8] = inv_freq[None, :]
    c[:, K_IOTA:K_IOTA + 64] = np.arange(64, dtype=np.float32)[None, :]
    return c


class Ctx:
    pass


def build_program(nc, stages=99, dbg=False):
    P = Prog(nc)
    nc.allow_low_precision("bf16 matmul operands, fp32 accumulation")
    okind = "ExternalOutput" if dbg else "Internal"

    def din(name, shape, dt=F32):
        return nc.dram_tensor(name, list(shape), dt, kind="ExternalInput").ap()

    def dscr(name, shape, dt):
        return nc.dram_tensor(name, list(shape), dt, kind=okind).ap()

    x_d = din("x", [SEQ, D])
    pos_d = din("pos", [128, NT_ALL], I32)
    consts_d = din("consts", [128, NCONST])
    g_mix_d = din("g_mix", [1, D])
    w_in_d = din("w_in", [D, IN_W])
    convw_d = din("convw", [128, 12, 5])
    convb_d = din("convb", [128, 12])
    dtb_d = din("dtb", [1, 32])
    alog_d = din("alog", [1, 32])
    out_d = nc.dram_tensor("out", [OWN, D], F32, kind="ExternalOutput").ap()

    z_d = dscr("z_s", [OWN, 1024], F32)
    q_d = dscr("q_s", [OWN, 1024], BF16)
    k_d = dscr("k_s", [SEQ, 1024], BF16)
    v_d = dscr("v_s", [SEQ, 1024], BF16)
    xsb_d = dscr("xsb_s", [SEQ, 1280], BF16)
    bc_d = dscr("bc_s", [512, SEQ], BF16)
    dt_d = dscr("dt_s", [SEQ, 64], F32)

    consts = P.sb("consts", [128, NCONST], F32)
    ident_bf = P.sb("ident_bf", [128, 128], BF16)
    dtda = P.sb("dtda", [128, NT_ALL, 64], F32)
    junk = P.sb("junk", [128, D], BF16)
    ss = [P.sb(f"ss{i}", [128, 2], F32) for i in range(2)]
    eps_t = P.sb("eps_t", [128, 1], F32)
    P.memset(eps_t[:], EPS)
    u_sb = P.sb_top("u_sb", [128, NT_OWN, D], BF16)
    U_OFF = P.sb_hi
    ps = [nc.alloc_psum_tensor(f"ps{i}", [128, 512], F32) for i in range(8)]
    psb = [ps[6][:].bitcast(BF16), ps[7][:].bitcast(BF16)]

    P.dma("sp", consts[:], consts_d)
    P.copy(ident_bf[:], consts[:, K_IDENT:K_IDENT + 128])
    ident_f = consts[:, K_IDENT:K_IDENT + 128]

    evac_rr = [0]

    def evac(out, in_):
        evac_rr[0] += 1
        if evac_rr[0] % 2:
            return P.copy(out, in_, eng="act")
        return P.copy(out, in_, eng="dve")

    mark0 = P.sb_mark()
    nT = P.sb("nT", [128, 16, SEQ], BF16)
    mark1 = P.sb_mark()
    g_bc = P.sb("g_bc", [128, D], F32)
    xt = [P.sb(f"xt{i}", [128, D], F32) for i in range(2)]
    nb = [P.sb(f"nb{i}", [128, D], BF16) for i in range(2)]

    P.dma("sp", g_bc[:], g_mix_d.to_broadcast([128, D]))


    def rmsnorm_tile(src, dst_bf, gb, ssb, width):
        P.stt(junk[:, 0:width], src, 1.0, src, ALU.mult, ALU.mult, accum_out=ssb[:, 0:1])
        P.act(ssb[:, 1:2], ssb[:, 0:1], AF.Sqrt, bias=eps_t[:, 0:1], scale=1.0 / width)
        P.add("dve", lambda e, o=ssb[:, 1:2]: e.reciprocal(o, o), reads=[ssb[:, 1:2]], writes=[ssb[:, 1:2]])
        P.stt(dst_bf, src, ssb[:, 1:2], gb, ALU.mult, ALU.mult)

    for i in range(NT_ALL):
        xti = xt[i % 2]
        P.dma("sp", xti[:], x_d[i * 128:(i + 1) * 128, :])
        rmsnorm_tile(xti[:], nb[i % 2][:], g_bc[:], ss[i % 2], D)
        for grp in range(4):
            pt = psb[grp % 2]
            for j in range(4):
                kc = grp * 4 + j
                P.tr(pt[:, j * 128:(j + 1) * 128], nb[i % 2][:, kc * 128:(kc + 1) * 128], ident_bf[:])
            evac(nT[:, grp * 4:(grp + 1) * 4, i * 128:(i + 1) * 128],
                 pt[:, 0:512].rearrange("p (j t) -> p j t", j=4))
    P.sb_release(mark1)
    if stages <= 1:
        P.emit()
        return P

    wb = [P.sb(f"wb{i}", [128, 16, 512], BF16) for i in range(2)]
    w_in_v = w_in_d.rearrange("(kc p) n -> p kc n", p=128)
    wb_i = [0]

    def load_wblock(c0, ncols):
        b = wb[wb_i[0] % 2]
        wb_i[0] += 1
        for q4 in range(4):
            P.dma("pool", b[:, q4 * 4:(q4 + 1) * 4, 0:ncols], w_in_v[:, q4 * 4:(q4 + 1) * 4, c0:c0 + ncols])
        return b

    cs_sin = P.sb("sin", [128, NT_ALL, 8], F32)
    cs_cos = P.sb("cos", [128, NT_ALL, 8], F32)
    pos_i = P.sb("pos_i", [128, NT_ALL], I32)
    pos_sb = P.sb("pos", [128, NT_ALL], F32)
    ang = P.sb("ang", [128, NT_ALL, 8], F32)
    P.dma("sp", pos_i[:], pos_d)
    P.copy(pos_sb[:], pos_i[:])
    TWO_PI = float(2 * np.pi)
    P.tt(ang[:], pos_sb[:].unsqueeze(2).to_broadcast([128, NT_ALL, 8]),
         consts[:, K_INVF:K_INVF + 8].unsqueeze(1).to_broadcast([128, NT_ALL, 8]), ALU.mult)
    angi = P.sb("angi", [128, NT_ALL, 8], I32)
    angk = P.sb("angk", [128, NT_ALL, 8], F32)
    angm = P.sb("angm", [128, NT_ALL, 8], F32)
    PI = float(np.pi)

    def sin_of(dst, shift):
        P.ts(angk[:], ang[:], shift, 1.0 / TWO_PI, ALU.add, ALU.mult)
        P.copy(angi[:], angk[:])
        P.copy(angk[:], angi[:])
        P.ts(angm[:], ang[:], shift, None, ALU.add)
        P.stt(angm[:], angk[:], -TWO_PI, angm[:], ALU.mult, ALU.add)
        P.ts(angk[:], angm[:], PI, None, ALU.is_gt)
        P.stt(angm[:], angk[:], -TWO_PI, angm[:], ALU.mult, ALU.add)
        P.ts(angm[:], angm[:], -PI, PI, ALU.max, ALU.min)
        P.act(dst, angm[:], AF.Sin)

    sin_of(cs_sin[:], 0.0)
    sin_of(cs_cos[:], PI / 2)

    dtb_bc = P.sb("dtb_bc", [128, 32], F32)
    a_bc = P.sb("a_bc", [128, 32], F32)
    P.dma("sp", dtb_bc[:], dtb_d.to_broadcast([128, 32]))
    P.dma("sp", a_bc[:], alog_d.to_broadcast([128, 32]))
    P.act(a_bc[:], a_bc[:], AF.Exp)
    P.ts(a_bc[:], a_bc[:], -1.0, None, ALU.mult)

    stf = [P.sb(f"stf{i}", [128, 512], F32) for i in range(2)]
    stb = [P.sb(f"stb{i}", [128, 512], BF16) for i in range(2)]
    rtmp = P.sb("rtmp", [128, 4, 4, 8], F32)
    cnt = [0]

    def tokmajor_segment(c0, ncols_total, ntiles, kind, dst):
        for b0 in range(0, ncols_total, 512):
            ncols = min(512, ncols_total - b0)
            w = load_wblock(c0 + b0, ncols)
            for i in range(ntiles):
                cnt[0] += 1
                pst = ps[cnt[0] % 4]
                for kc in range(16):
                    P.mm(pst[:, 0:ncols], nT[:, kc, i * 128:(i + 1) * 128], w[:, kc, 0:ncols],
                         start=(kc == 0), stop=(kc == 15))
                sf = stf[cnt[0] % 2]
                sbf = stb[cnt[0] % 2]
                rows = slice(i * 128, (i + 1) * 128)
                if kind == "z":
                    evac(sf[:, 0:ncols], pst[:, 0:ncols])
                    P.dma("sp", dst[rows, b0:b0 + ncols], sf[:, 0:ncols])
                elif kind == "v":
                    evac(sbf[:, 0:ncols], pst[:, 0:ncols])
                    P.dma("sp", dst[rows, b0:b0 + ncols], sbf[:, 0:ncols])
                elif kind == "qk":
                    evac(sf[:, 0:ncols], pst[:, 0:ncols])
                    v4 = sf[:, 0:512].rearrange("p (g d) -> p g d", d=64)
                    t1 = v4[:, :, 0:8]
                    t2 = v4[:, :, 8:16]
                    cb = cs_cos[:, i, :].unsqueeze(1).to_broadcast([128, 8, 8])
                    sb_ = cs_sin[:, i, :].unsqueeze(1).to_broadcast([128, 8, 8])
                    r = rtmp[:].rearrange("p a b d -> p (a b) d")
                    P.tt(r[:, 0:8, :], t1, cb, ALU.mult)
                    P.tt(r[:, 8:16, :], t2, sb_, ALU.mult)
                    r2 = rtmp2[:].rearrange("p a b d -> p (a b) d")
                    P.tt(r2[:, 0:8, :], t2, cb, ALU.mult)
                    P.tt(r2[:, 8:16, :], t1, sb_, ALU.mult)
                    P.tt(t1, r[:, 0:8, :], r[:, 8:16, :], ALU.subtract)
                    P.tt(t2, r2[:, 0:8, :], r2[:, 8:16, :], ALU.add)
                    P.copy(sbf[:, 0:ncols], sf[:, 0:ncols], eng="pool")
                    P.dma("sp", dst[rows, b0:b0 + ncols], sbf[:, 0:ncols])
                elif kind == "dt":
                    d = dtda[:, i, :]
                    P.tt(d[:, 0:32], pst[:, 0:32], dtb_bc[:], ALU.add)
                    P.act(d[:, 0:32], d[:, 0:32], AF.Exp)
                    P.act(d[:, 0:32], d[:, 0:32], AF.Ln, bias=1.0)
                    P.tt(d[:, 32:64], d[:, 0:32], a_bc[:], ALU.mult)
                    if dbg:
                        P.dma("sp", dt_d[rows, :], d)

    rtmp2 = P.sb("rtmp2", [128, 4, 4, 8], F32)

    tokmajor_segment(C_DT, 32, NT_ALL, "dt", None)
    tokmajor_segment(C_Z, 1024, NT_OWN, "z", z_d)
    tokmajor_segment(C_Q, 1024, NT_OWN, "qk", q_d)
    tokmajor_segment(C_K, 1024, NT_ALL, "qk", k_d)
    tokmajor_segment(C_V, 1024, NT_ALL, "v", v_d)

    cw = P.sb("cw", [128, 12, 5], F32)
    cbias = P.sb("cbias", [128, 12], F32)
    P.dma("sp", cw[:], convw_d)
    P.dma("sp", cbias[:], convb_d)
    xcm = [P.sb(f"xcm{i}", [128, SEQ + 4], F32) for i in range(2)]
    for b_ in xcm:
        P.memset(b_[:, 0:2], 0.0)
        P.memset(b_[:, SEQ + 2:SEQ + 4], 0.0)
    cacc = P.sb("cacc", [128, SEQ], F32)
    so = [P.sb(f"so{i}", [128, SEQ], BF16) for i in range(2)]
    tm = [P.sb(f"tm{i}", [128, NT_ALL, 128], BF16) for i in range(2)]
    xsb_v = xsb_d.rearrange("(tt p) c -> p tt c", p=128)
    for blk in range(3):
        w = load_wblock(C_XBC + blk * 512, 512)
        for cc in range(4):
            ch = blk * 4 + cc
            xb = xcm[ch % 2]
            for tb in range(4):
                cnt[0] += 1
                pst = ps[cnt[0] % 4]
                for kc in range(16):
                    P.mm(pst[:], w[:, kc, cc * 128:(cc + 1) * 128], nT[:, kc, tb * 512:(tb + 1) * 512],
                         start=(kc == 0), stop=(kc == 15))
                evac(xb[:, 2 + tb * 512:2 + (tb + 1) * 512], pst[:])
            P.ts(cacc[:], xb[:, 0:SEQ], cw[:, ch, 0:1], None, ALU.mult)
            for k in range(1, 5):
                P.stt(cacc[:], xb[:, k:k + SEQ], cw[:, ch, k:k + 1], cacc[:], ALU.mult, ALU.add)
            s_ = so[ch % 2]
            P.act(s_[:], cacc[:], AF.Silu, bias=cbias[:, ch:ch + 1])
            if ch >= 8:
                P.dma("sp", bc_d[(ch - 8) * 128:(ch - 7) * 128, :], s_[:])
            if ch < 10:
                t_ = tm[ch % 2]
                for grp in range(4):
                    pt = psb[grp % 2]
                    for j in range(4):
                        tt_ = grp * 4 + j
                        P.tr(pt[:, j * 128:(j + 1) * 128], s_[:, tt_ * 128:(tt_ + 1) * 128], ident_bf[:])
                    evac(t_[:, grp * 4:(grp + 1) * 4, :], pt[:, 0:512].rearrange("p (j t) -> p j t", j=4))
                for hh in range(2):
                    P.dma("sp", xsb_v[:, hh * 8:(hh + 1) * 8, ch * 128:(ch + 1) * 128], t_[:, hh * 8:(hh + 1) * 8, :])
    P.sb_release(mark0)
    if stages <= 2:
        P.emit()
        return P

    def load_cast_block(dst, src_v, c0, ncols, nkc):
        step = 4
        for q4 in range(0, nkc, step):
            P.dma("pool", dst[:, q4:q4 + step, 0:ncols], src_v[:, q4:q4 + step, c0:c0 + ncols])

    dskip_d = din("dskip", [1, 16])
    ssdg_d = din("ssd_g", [1, 1024])
    m3 = P.sb_mark()
    yacc = P.sb("yacc", [128, NT_OWN, 1024], F32)
    Sin = P.sb("Sin", [128, 16, 64], F32)
    Sin_bf = P.sb("Sin_bf", [128, 16, 64], BF16)
    dskip_bc = P.sb("dskip_bc", [128, 16], F32)
    ssdg_bc = P.sb("ssdg_bc", [128, 1024], F32)
    P.dma("sp", dskip_bc[:], dskip_d.to_broadcast([128, 16]))
    P.dma("sp", ssdg_bc[:], ssdg_d.to_broadcast([128, 1024]))
    negm4 = [P.sb(f"negm4_{d_}", [128, 4, 128], F32) for d_ in range(2)]
    P.copy(negm4[0][:], consts[:, K_NMU:K_NMU + 128].unsqueeze(1).to_broadcast([128, 4, 128]))
    P.copy(negm4[1][:], consts[:, K_NML:K_NML + 128].unsqueeze(1).to_broadcast([128, 4, 128]))
    tri = [consts[:, K_TRIU:K_TRIU + 128], consts[:, K_TRIL:K_TRIL + 128]]
    ones_f = consts[:, K_ONES:K_ONES + 128]
    xsb_t = [P.sb(f"xsbt{i}", [128, 1280], BF16) for i in range(2)]
    bcm_t = [P.sb(f"bcm{i}", [128, 4, 128], BF16) for i in range(2)]
    cst = P.sb("cst", [128, 32], F32)
    wst = P.sb("wst", [128, 16], F32)
    dec = P.sb("dec", [128, 16], F32)
    ecs = P.sb("ecs", [128, 16], F32)
    xd = P.sb("xd", [128, 16, 64], BF16)
    xdw = P.sb("xdw", [128, 16, 64], BF16)
    cbT = P.sb("cbT", [128, 2, 128], F32)
    datri = P.sb("datri", [128, 16, 128], F32)
    arg = P.sb("arg", [128, 16, 128], F32)
    Mbf = P.sb("Mbf", [128, 16, 128], BF16)
    ytmp = P.sb("ytmp", [128, 16, 64], F32)
    bc_v = bc_d.rearrange("(j n) t -> n j t", n=128)
    P.memset(Sin[:], 0.0)
    P.memset(Sin_bf[:], 0.0)
    it3 = [0]

    def ssd_chunk(c, d_, out):
        it3[0] += 1
        xt_ = xsb_t[it3[0] % 2]
        bt = bcm_t[it3[0] % 2]
        P.dma("sp", xt_[:], xsb_d[c * 128:(c + 1) * 128, :])
        if out:
            P.dma("sp", bt[:], bc_v[:, :, c * 128:(c + 1) * 128])
        dt_ = dtda[:, c, d_ * 16:(d_ + 1) * 16]
        da = dtda[:, c, 32 + d_ * 16:32 + (d_ + 1) * 16]
        A = ps[4]
        P.mm(A[:, 0:16], tri[d_], da)
        P.mm(A[:, 16:32], ones_f, da)
        P.copy(cst[:], A[:, 0:32])
        P.tt(wst[:], cst[:, 16:32], cst[:, 0:16], ALU.subtract)
        P.act(wst[:], wst[:], AF.Exp)
        P.act(dec[:], cst[:, 16:32], AF.Exp)
        xs3 = xt_[:, 0:1024].rearrange("p (h e) -> p h e", e=64)
        P.tt(xd[:], xs3, dt_.unsqueeze(2).to_broadcast([128, 16, 64]), ALU.mult)
        P.tt(xdw[:], xd[:], wst[:].unsqueeze(2).to_broadcast([128, 16, 64]), ALU.mult)
        if out:
            for g in range(2):
                P.mm(A[:, 256 + g * 128:256 + (g + 1) * 128], bt[:, g, :], bt[:, 2 + g, :])
            P.copy(cbT[:], A[:, 256:512].rearrange("p (g l) -> p g l", g=2))
            P.tt(datri[:], tri[d_].unsqueeze(1).to_broadcast([128, 16, 128]),
                 da.unsqueeze(2).to_broadcast([128, 16, 128]), ALU.mult)
            for j in range(4):
                R = ps[j]
                P.mm(R[:], ones_f, datri[:, 4 * j:4 * j + 4, :].rearrange("p h l -> p (h l)"), start=True, stop=False)
                P.mm(R[:], ident_f, negm4[d_][:].rearrange("p h l -> p (h l)"), start=False, stop=True)
                P.tt(arg[:, 4 * j:4 * j + 4, :], R[:].rearrange("p (h l) -> p h l", h=4),
                     cst[:, 4 * j:4 * j + 4].unsqueeze(2).to_broadcast([128, 4, 128]), ALU.subtract)
            P.act(arg[:], arg[:], AF.Exp)
            for g in range(2):
                P.tt(Mbf[:, g * 8:(g + 1) * 8, :], arg[:, g * 8:(g + 1) * 8, :],
                     cbT[:, g, :].unsqueeze(1).to_broadcast([128, 8, 128]), ALU.mult)
            for h in range(16):
                Y = ps[5 + h // 8]
                P.mm(Y[:, (h % 8) * 64:(h % 8 + 1) * 64], Mbf[:, h, :], xd[:, h, :])
            for g in range(2):
                P.mm(ps[g][:], bt[:, 2 + g, :], Sin_bf[:, g * 8:(g + 1) * 8, :].rearrange("p h e -> p (h e)"))
            P.act(ecs[:], cst[:, 0:16], AF.Exp)
            for g in range(2):
                hs = slice(g * 8, (g + 1) * 8)
                ysl = yacc[:, c, g * 512:(g + 1) * 512].rearrange("p (h e) -> p h e", e=64)
                P.tt(ytmp[:, hs, :], ps[g][:].rearrange("p (h e) -> p h e", e=64),
                     ecs[:, hs].unsqueeze(2).to_broadcast([128, 8, 64]), ALU.mult)
                P.tt(ytmp[:, hs, :], ytmp[:, hs, :], ps[5 + g][:].rearrange("p (h e) -> p h e", e=64), ALU.add)
                if d_ == 0:
                    P.tt(ysl, xs3[:, hs, :], dskip_bc[:, hs].unsqueeze(2).to_broadcast([128, 8, 64]), ALU.mult)
                P.tt(ysl, ysl, ytmp[:, hs, :], ALU.add)
        for g in range(2):
            P.mm(ps[2 + g][:], xt_[:, 1024 + g * 128:1024 + (g + 1) * 128],
                 xdw[:, g * 8:(g + 1) * 8, :].rearrange("p h e -> p (h e)"))
        P.tt(Sin[:], Sin[:], dec[:].unsqueeze(2).to_broadcast([128, 16, 64]), ALU.mult)
        for g in range(2):
            sl = Sin[:, g * 8:(g + 1) * 8, :]
            P.tt(sl, sl, ps[2 + g][:].rearrange("p (h e) -> p h e", e=64), ALU.add)
        P.copy(Sin_bf[:], Sin[:], eng="act")

    for c in range(NT_OWN):
        ssd_chunk(c, 0, True)
    P.memset(Sin[:], 0.0)
    P.memset(Sin_bf[:], 0.0)
    for c in range(NT_ALL - 1, NT_OWN - 1, -1):
        ssd_chunk(c, 1, False)
    for c in range(NT_OWN - 1, -1, -1):
        ssd_chunk(c, 1, True)
    ztile = [P.sb(f"zt{i}", [128, 1024], F32) for i in range(2)]
    gy = P.sb("gy", [128, 1024], F32)
    for c in range(NT_OWN):
        zt = ztile[c % 2]
        P.dma("sp", zt[:], z_d[c * 128:(c + 1) * 128, :])
        P.act(zt[:], zt[:], AF.Silu)
        P.tt(gy[:], yacc[:, c, :], zt[:], ALU.mult)
        rmsnorm_tile(gy[:], u_sb[:, c, 0:1024], ssdg_bc[:], ss[0], 1024)
    P.sb_release(m3)
    if stages <= 3:
        if dbg:
            u_dbg = dscr("u_s", [OWN, D], BF16)
            P.dma("sp", u_dbg.rearrange("(t p) c -> p t c", p=128), u_sb[:])
            P.barrier()
        P.emit()
        return P

    LAM_INIT = 0.8 - 0.6 * float(np.exp(-0.3 * 0))
    lamv_d = din("lamv", [1, 256])
    subg_d = din("subln_g", [1, 128])
    lam_sb = P.sb("lam_sb", [128, 256], F32)
    ls = P.sb("ls", [128, 4], F32)
    nlam = P.sb("nlam", [128, 1], F32)
    gsub_bc = P.sb("gsub_bc", [128, 128], F32)
    P.dma("sp", lam_sb[:], lamv_d.to_broadcast([128, 256]))
    P.dma("sp", gsub_bc[:], subg_d.to_broadcast([128, 128]))
    P.ts(gsub_bc[:], gsub_bc[:], 1.0 - LAM_INIT, None, ALU.mult)
    P.stt(junk[:, 0:64], lam_sb[:, 0:64], 1.0, lam_sb[:, 64:128], ALU.mult, ALU.mult, accum_out=ls[:, 0:1])
    P.stt(junk[:, 0:64], lam_sb[:, 128:192], 1.0, lam_sb[:, 192:256], ALU.mult, ALU.mult, accum_out=ls[:, 1:2])
    P.act(ls[:, 0:2], ls[:, 0:2], AF.Exp)
    P.tt(ls[:, 2:3], ls[:, 0:1], ls[:, 1:2], ALU.subtract)
    P.ts(nlam[:], ls[:, 2:3], LAM_INIT, -1.0, ALU.add, ALU.mult)
    ktm = P.sb("ktm", [128, NT_ALL, 128], BF16)
    vaug = [P.sb(f"vaug{i}", [128, NT_ALL, 160], BF16) for i in range(2)]
    qtm = P.sb("qtm", [128, NT_OWN, 128], BF16)
    kT = [P.sb(f"kT{i}", [128, SEQ], BF16) for i in range(2)]
    qT = [P.sb(f"qT{i}", [128, OWN], BF16) for i in range(2)]
    ksq = P.sb("ksq", [128, SEQ], BF16)
    qsq = P.sb("qsq", [128, OWN], BF16)
    negm = [P.sb(f"negm{i}", [1, OWN], BF16) for i in range(2)]
    kmx = P.sb("kmx", [1, 16], F32)
    qn = P.sb("qn", [1, 512], F32)
    NPT = 6
    PT = [P.sb(f"PT{i}", [128, 512], BF16) for i in range(NPT)]
    ones_b = P.sb("ones_b", [128, 128], BF16)
    P.copy(ones_b[:], consts[:, K_ONES:K_ONES + 128])
    osub = P.sb("osub", [128, 128], F32)
    otmp = P.sb("otmp", [128, 128], F32)
    rs_ = P.sb("rs_", [128, 4], F32)
    for b_ in vaug:
        P.memset(b_[:], 1.0)
    k_v = k_d.rearrange("(t p) c -> p t c", p=128)
    v_v = v_d.rearrange("(t p) c -> p t c", p=128)
    q_v = q_d.rearrange("(t p) c -> p t c", p=128)

    def rmsnorm_ln(src, dst_bf, gb, ssb, width):
        P.stt(junk[:, 0:width], src, 1.0, src, ALU.mult, ALU.mult, accum_out=ssb[:, 0:1])
        P.act(ssb[:, 1:2], ssb[:, 0:1], AF.Ln, bias=eps_t[:, 0:1], scale=1.0 / width)
        P.act(ssb[:, 1:2], ssb[:, 1:2], AF.Exp, scale=-0.5)
        P.stt(dst_bf, src, ssb[:, 1:2], gb, ALU.mult, ALU.mult)

    ptc = [0]
    for h in range(8):
        cs_ = slice(h * 128, (h + 1) * 128)
        va = vaug[h % 2]
        P.dma("sp", ktm[:], k_v[:, :, cs_])
        P.dma("sp", va[:, :, 0:128], v_v[:, :, cs_])
        P.dma("sp", qtm[:], q_v[:, :, cs_])
        kT_ = kT[h % 2]
        qT_ = qT[h % 2]
        for grp in range(4):
            pt = psb[grp % 2]
            for j in range(4):
                P.tr(pt[:, j * 128:(j + 1) * 128], ktm[:, grp * 4 + j, :], ident_bf[:])
            evac(kT_[:, grp * 512:(grp + 1) * 512], pt[:, 0:512])
        for grp in range(2):
            pt = psb[grp % 2]
            for j in range(4):
                P.tr(pt[:, j * 128:(j + 1) * 128], qtm[:, grp * 4 + j, :], ident_bf[:])
            evac(qT_[:, grp * 512:(grp + 1) * 512], pt[:, 0:512])
        P.tt(ksq[:], kT_[:], kT_[:], ALU.mult)
        P.tt(qsq[:], qT_[:], qT_[:], ALU.mult, eng="pool")
        for c in range(2):
            pr = slice(c * 64, (c + 1) * 64)
            for kb in range(4):
                P.mm(ps[5][0:1, :], ones_b[pr, 0:1], ksq[pr, kb * 512:(kb + 1) * 512])
                P.reduce(kmx[:, c * 4 + kb:c * 4 + kb + 1], ps[5][0:1, :], ALU.max)
            P.reduce(kmx[:, 8 + c:9 + c], kmx[:, c * 4:c * 4 + 4], ALU.max)
            for qb in range(2):
                P.mm(ps[5][0:1, :], ones_b[pr, 0:1], qsq[pr, qb * 512:(qb + 1) * 512])
                P.ts(qn[:], ps[5][0:1, :], kmx[:, 8 + c:9 + c], 1e-20, ALU.mult, ALU.add)
                P.act(qn[:], qn[:], AF.Ln)
                P.act(qn[:], qn[:], AF.Exp, scale=0.5)
                P.ts(negm[c][:, qb * 512:(qb + 1) * 512], qn[:], -1.0, None, ALU.mult)
        for qb in range(2):
            qs_ = slice(qb * 512, (qb + 1) * 512)

            def acc(g):
                return ps[2 + g // 3][:, (g % 3) * 160:(g % 3) * 160 + 129]

            def s_mm(kt):
                for c in range(2):
                    pr = slice(c * 64, (c + 1) * 64)
                    Sb = ps[6 + c] if kt % 2 else ps[c]
                    P.mm(Sb[:], kT_[pr, kt * 128:(kt + 1) * 128], qT_[pr, qs_], start=True, stop=False)
                    P.mm(Sb[:], ones_b[0:1, 0:128], negm[c][0:1, qs_], start=False, stop=True)

            s_mm(0)
            for kt in range(NT_ALL):
                if kt + 1 < NT_ALL:
                    s_mm(kt + 1)
                for c in range(2):
                    Sb = ps[6 + c] if kt % 2 else ps[c]
                    ptc[0] += 1
                    pt_ = PT[ptc[0] % NPT]
                    P.act(pt_[:], Sb[:], AF.Exp, scale=0.125)
                    for qt in range(4):
                        g_ = qt * 2 + c
                        P.mm(acc(g_), pt_[:, qt * 128:(qt + 1) * 128], va[:, kt, 0:129],
                             start=(kt == 0 and g_ in (0, 4, 6)), stop=(kt == NT_ALL - 1),
                             skip_group_check=True)
            for qt in range(4):
                O1 = acc(qt * 2)
                O2 = acc(qt * 2 + 1)
                P.add("dve", lambda e, o=rs_[:, 0:1], i_=O1[:, 128:129]: e.reciprocal(o, i_), reads=[O1[:, 128:129]], writes=[rs_[:, 0:1]])
                P.add("dve", lambda e, o=rs_[:, 1:2], i_=O2[:, 128:129]: e.reciprocal(o, i_), reads=[O2[:, 128:129]], writes=[rs_[:, 1:2]])
                P.tt(rs_[:, 2:3], rs_[:, 1:2], nlam[:], ALU.mult)
                P.ts(otmp[:], O2[:, 0:128], rs_[:, 2:3], None, ALU.mult)
                P.stt(osub[:], O1[:, 0:128], rs_[:, 0:1], otmp[:], ALU.mult, ALU.add)
                rmsnorm_ln(osub[:], u_sb[:, qb * 4 + qt, 1024 + h * 128:1024 + (h + 1) * 128], gsub_bc[:], ss[1], 128)
    P.sb_release(m3)
    if stages <= 4:
        if dbg:
            u_dbg = dscr("u_s", [OWN, D], BF16)
            P.dma("sp", u_dbg.rearrange("(t p) c -> p t c", p=128), u_sb[:])
            P.barrier()
        P.emit()
        return P

    w_out_d = din("w_out", [D, D])
    h1 = P.sb_top("h1", [128, NT_OWN, D], F32)
    m5 = P.sb_mark()
    uT = P.sb("uT", [128, 16, OWN], BF16)
    wb2 = [P.sb(f"wb2_{i}", [128, 16, 512], BF16) for i in range(2)]
    xo = [P.sb(f"xo{i}", [128, 512], F32) for i in range(2)]
    for i in range(NT_OWN):
        for grp in range(4):
            pt = psb[grp % 2]
            for j in range(4):
                kc = grp * 4 + j
                P.tr(pt[:, j * 128:(j + 1) * 128], u_sb[:, i, kc * 128:(kc + 1) * 128], ident_bf[:])
            evac(uT[:, grp * 4:(grp + 1) * 4, i * 128:(i + 1) * 128], pt[:, 0:512].rearrange("p (j t) -> p j t", j=4))
    wo_v = w_out_d.rearrange("(kc p) n -> p kc n", p=128)
    for cb in range(4):
        w = wb2[cb % 2]
        load_cast_block(w, wo_v, cb * 512, 512, 16)
        for i in range(NT_OWN):
            cnt[0] += 1
            pst = ps[cnt[0] % 4]
            xo_ = xo[cnt[0] % 2]
            P.dma("sp", xo_[:], x_d[i * 128:(i + 1) * 128, cb * 512:(cb + 1) * 512])
            for kc in range(16):
                P.mm(pst[:], uT[:, kc, i * 128:(i + 1) * 128], w[:, kc, :], start=(kc == 0), stop=(kc == 15))
            P.tt(h1[:, i, cb * 512:(cb + 1) * 512], pst[:], xo_[:], ALU.add)
    P.sb_release(m5)
    if stages <= 5:
        if dbg:
            h_dbg = dscr("h1_s", [OWN, D], F32)
            P.dma("sp", h_dbg.rearrange("(t p) c -> p t c", p=128), h1[:])
            P.barrier()
        P.emit()
        return P

    NE = 64
    CAP = 128
    gffn_d = din("g_ffn", [1, D])
    wr_d = din("w_route", [D, 72])
    br_d = din("b_route", [1, 72])
    weg_d = din("w_exp_gate", [NE, D, 512])
    weu_d = din("w_exp_up", [NE, D, 512])
    wed_d = din("w_exp_down", [NE, 512, D])
    n2_d = dscr("n2_s", [OWN + 128, D], BF16)
    rowtok_d = dscr("rowtok_s", [NE * CAP, 1], I32)
    yrows_d = dscr("yrows_s", [NE * CAP, D], F32)

    m6 = P.sb_mark()
    gates = P.sb("gates", [128, NT_OWN, 2], F32)
    dest_f = P.sb("dest_f", [128, NT_OWN, 2], F32)
    dest_i = P.sb("dest_i", [128, NT_OWN, 2], I32)
    dst2_f = P.sb("dst2_f", [128, NT_OWN, 2], F32)
    dst2_i = P.sb("dst2_i", [128, NT_OWN, 2], I32)
    rowtok = P.sb("rowtok", [128, NE], I32)
    m6r = P.sb_mark()
    gffn_bc = P.sb("gffn_bc", [128, D], F32)
    P.dma("sp", gffn_bc[:], gffn_d.to_broadcast([128, D]))
    wr_sb = P.sb("wr_sb", [128, 16, 72], F32)
    P.dma("sp", wr_sb[:], wr_d.rearrange("(kc p) n -> p kc n", p=128))
    br_bc = P.sb("br_bc", [128, 72], F32)
    P.dma("sp", br_bc[:], br_d.to_broadcast([128, 72]))
    n2f = [P.sb(f"n2f{i}", [128, D], F32) for i in range(2)]
    n2b = [P.sb(f"n2b{i}", [128, D], BF16) for i in range(2)]
    n2T = P.sb("n2T", [128, 16, 128], F32)
    A1 = P.sb("A1", [128, NT_OWN, NE], F32)
    A2 = P.sb("A2", [128, NT_OWN, NE], F32)
    Aall = P.sb("Aall", [128, NT_OWN, NE], BF16)
    lg = P.sb("lg", [128, 72], F32)
    rt = P.sb("rt", [128, 16], F32)
    Gm = P.sb("Gm", [128, 8], F32)
    tmp88 = P.sb("tmp88", [128, 8, 8], F32)
    esel = P.sb("esel", [128, 8], F32)
    e2 = P.sb("e2", [128, 8], F32)
    mk1 = P.sb("mk1", [128, 8], F32)
    mk2 = P.sb("mk2", [128, 8], F32)
    zrow = P.sb("zrow", [128, D], BF16)
    P.memset(zrow[:], 0.0)
    P.dma("sp", n2_d[OWN:OWN + 128, :], zrow[:])
    rinit = P.sb("rinit", [128, NE], I32)
    P.memset(rinit[:], OWN)
    P.dma("sp", rowtok_d.rearrange("(r e) o -> r (e o)", e=NE), rinit[:])
    ones_bf = P.sb("ones_bf", [128, 128], BF16)
    slt_bf = P.sb("slt_bf", [128, 128], BF16)
    P.copy(ones_bf[:], consts[:, K_ONES:K_ONES + 128])
    P.copy(slt_bf[:], consts[:, K_SLT:K_SLT + 128])
    iota_e = consts[:, K_IOTA:K_IOTA + 64]

    for i in range(NT_OWN):
        nf = n2f[i % 2]
        nbb = n2b[i % 2]
        P.stt(junk[:], h1[:, i, :], 1.0, h1[:, i, :], ALU.mult, ALU.mult, accum_out=ss[0][:, 0:1])
        P.act(ss[0][:, 1:2], ss[0][:, 0:1], AF.Sqrt, bias=eps_t[:, 0:1], scale=1.0 / D)
        P.add("dve", lambda e, o=ss[0][:, 1:2]: e.reciprocal(o, o), reads=[ss[0][:, 1:2]], writes=[ss[0][:, 1:2]])
        P.stt(nf[:], h1[:, i, :], ss[0][:, 1:2], gffn_bc[:], ALU.mult, ALU.mult)
        P.copy(nbb[:], nf[:], eng="pool")
        P.dma("sp", n2_d[i * 128:(i + 1) * 128, :], nbb[:])
        for grp in range(4):
            pt = ps[grp % 2]
            for j in range(4):
                kc = grp * 4 + j
                P.tr(pt[:, j * 128:(j + 1) * 128], nf[:, kc * 128:(kc + 1) * 128], ident_f)
            evac(n2T[:, grp * 4:(grp + 1) * 4, :], pt[:].rearrange("p (j t) -> p j t", j=4))
        for kc in range(16):
            P.mm(ps[2][:, 0:72], n2T[:, kc, :], wr_sb[:, kc, :], start=(kc == 0), stop=(kc == 15))
        P.tt(lg[:], ps[2][:, 0:72], br_bc[:], ALU.add)
        P.reduce(rt[:, 0:1], lg[:, 0:8], ALU.max)
        P.ts(Gm[:], lg[:, 0:8], rt[:, 0:1], None, ALU.is_equal)
        P.ts(rt[:, 1:2], rt[:, 0:1], -1.0, None, ALU.mult)
        P.act(tmp88[:, 0, :], lg[:, 0:8], AF.Exp, bias=rt[:, 1:2], accum_out=rt[:, 2:3])
        P.add("dve", lambda e, o=rt[:, 2:3]: e.reciprocal(o, o), reads=[rt[:, 2:3]], writes=[rt[:, 2:3]])
        P.tt(tmp88[:], lg[:, 8:72].rearrange("p (g e) -> p g e", g=8), Gm[:].unsqueeze(2).to_broadcast([128, 8, 8]), ALU.mult)
        P.reduce(esel[:], tmp88[:].rearrange("p g e -> p e g"), ALU.add)
        P.reduce(rt[:, 3:4], esel[:], ALU.max)
        P.ts(mk1[:], esel[:], rt[:, 3:4], None, ALU.is_equal)
        P.stt(e2[:], mk1[:], NEG, esel[:], ALU.mult, ALU.add)
        P.reduce(rt[:, 4:5], e2[:], ALU.max)
        P.ts(mk2[:], e2[:], rt[:, 4:5], None, ALU.is_equal)
        P.tt(rt[:, 5:6], rt[:, 4:5], rt[:, 3:4], ALU.subtract)
        P.act(rt[:, 5:6], rt[:, 5:6], AF.Exp)
        P.ts(rt[:, 5:6], rt[:, 5:6], 1.0, None, ALU.add)
        P.add("dve", lambda e, o=rt[:, 5:6]: e.reciprocal(o, o), reads=[rt[:, 5:6]], writes=[rt[:, 5:6]])
        P.ts(rt[:, 6:7], rt[:, 5:6], -1.0, 1.0, ALU.mult, ALU.add)
        P.tt(gates[:, i, 0:1], rt[:, 5:6], rt[:, 2:3], ALU.mult)
        P.tt(gates[:, i, 1:2], rt[:, 6:7], rt[:, 2:3], ALU.mult)
        P.tt(A1[:, i, :].rearrange("p (g e) -> p g e", g=8), Gm[:].unsqueeze(2).to_broadcast([128, 8, 8]),
             mk1[:].unsqueeze(1).to_broadcast([128, 8, 8]), ALU.mult)
        P.tt(A2[:, i, :].rearrange("p (g e) -> p g e", g=8), Gm[:].unsqueeze(2).to_broadcast([128, 8, 8]),
             mk2[:].unsqueeze(1).to_broadcast([128, 8, 8]), ALU.mult)
        P.tt(Aall[:, i, :], A1[:, i, :], A2[:, i, :], ALU.add)

    tokidx = P.sb("tokidx", [128, NT_OWN], I32)
    tokf = P.sb("tokf", [128, NT_OWN], F32)
    rk = P.sb("rk", [128, NE], F32)
    ovf = P.sb("ovf", [128, NE], F32)
    for i in range(NT_OWN):
        P.ts(tokf[:, i:i + 1], consts[:, K_IOTA:K_IOTA + 1], 1.0, float(i * 128), ALU.mult, ALU.add)
    pidx = P.sb("pidx", [128, 1], F32)
    P.reduce(pidx[:], consts[:, K_SLT:K_SLT + 128].rearrange("p l -> p l"), ALU.add)
    P.ts(pidx[:], pidx[:], -1.0, 127.0, ALU.mult, ALU.add)
    for i in range(NT_OWN):
        P.tt(tokf[:, i:i + 1], tokf[:, i:i + 1], pidx[:], ALU.add)
    P.copy(tokidx[:], tokf[:])
    for i in range(NT_OWN):
        R = ps[3]
        for j in range(i):
            P.mm(R[:, 0:NE], ones_bf[:], Aall[:, j, :], start=(j == 0), stop=False)
        P.mm(R[:, 0:NE], slt_bf[:], Aall[:, i, :], start=(i == 0), stop=True)
        P.ts(ovf[:], R[:, 0:NE], float(CAP), 1.0e6, ALU.is_ge, ALU.mult)
        P.stt(rk[:], iota_e, float(CAP), R[:, 0:NE], ALU.mult, ALU.add)
        P.tt(rk[:], rk[:], ovf[:], ALU.add)
        P.tt(ovf[:], rk[:], A1[:, i, :], ALU.mult)
        P.reduce(dest_f[:, i, 0:1], ovf[:], ALU.add)
        P.tt(ovf[:], rk[:], A2[:, i, :], ALU.mult)
        P.reduce(dest_f[:, i, 1:2], ovf[:], ALU.add)
        P.ts(ovf[:], R[:, 0:NE], float(CAP), 1.0e6, ALU.is_ge, ALU.mult)
        P.stt(rk[:], R[:, 0:NE], float(NE), iota_e, ALU.mult, ALU.add)
        P.tt(rk[:], rk[:], ovf[:], ALU.add)
        P.tt(ovf[:], rk[:], A1[:, i, :], ALU.mult)
        P.reduce(dst2_f[:, i, 0:1], ovf[:], ALU.add)
        P.tt(ovf[:], rk[:], A2[:, i, :], ALU.mult)
        P.reduce(dst2_f[:, i, 1:2], ovf[:], ALU.add)
    P.copy(dest_i[:], dest_f[:])
    P.copy(dst2_i[:], dst2_f[:])
    for i in range(NT_OWN):
        for k_ in range(2):
            P.add("pool", lambda e, i=i, k_=k_: e.indirect_dma_start(
                out=rowtok_d, out_offset=bass.IndirectOffsetOnAxis(ap=dst2_i[:, i, k_:k_ + 1], axis=0),
                in_=tokidx[:, i:i + 1], in_offset=None, bounds_check=NE * CAP - 1, oob_is_err=False),
                reads=[dst2_i[:, i, k_:k_ + 1], tokidx[:, i:i + 1]], writes=[rowtok_d], dma=True)
    P.dma("sp", rowtok[:], rowtok_d.rearrange("(r e) o -> r (e o)", e=NE))
    P.sb_release(m6r)
    m6b = P.sb_mark()
    wg = [P.sb(f"wg{i}", [128, 16, 512], BF16) for i in range(2)]
    wu = [P.sb(f"wu{i}", [128, 16, 512], BF16) for i in range(2)]
    wd = [P.sb_at(f"wd{i}", [128, 4, D], BF16, U_OFF + i * 16384) for i in range(2)]
    xe = [P.sb(f"xe{i}", [128, D], BF16) for i in range(2)]
    xeT = P.sb("xeT", [128, 16, 128], BF16)
    hg = P.sb("hg", [128, 512], F32)
    hact = P.sb("hact", [128, 512], BF16)
    actT = P.sb("actT", [128, 4, 128], BF16)
    ye = [P.sb(f"ye{i}", [128, D], F32) for i in range(2)]
    for e_ in range(NE):
        b_ = e_ % 2
        load_cast_block(wg[b_], weg_d[e_].rearrange("(kc p) f -> p kc f", p=128), 0, 512, 16)
        load_cast_block(wu[b_], weu_d[e_].rearrange("(kc p) f -> p kc f", p=128), 0, 512, 16)
        wdv = wed_d[e_].rearrange("(kc p) n -> p kc n", p=128)
        for kc in range(4):
            P.dma("pool", wd[b_][:, kc:kc + 1, :], wdv[:, kc:kc + 1, :])
        P.add("pool", lambda e, e_=e_, b_=b_: e.indirect_dma_start(
            out=xe[b_][:], out_offset=None, in_=n2_d,
            in_offset=bass.IndirectOffsetOnAxis(ap=rowtok[:, e_:e_ + 1], axis=0)),
            reads=[n2_d, rowtok[:, e_:e_ + 1]], writes=[xe[b_][:]], dma=True)
        for grp in range(4):
            pt = psb[grp % 2]
            for j in range(4):
                kc = grp * 4 + j
                P.tr(pt[:, j * 128:(j + 1) * 128], xe[b_][:, kc * 128:(kc + 1) * 128], ident_bf[:])
            evac(xeT[:, grp * 4:(grp + 1) * 4, :], pt[:, 0:512].rearrange("p (j t) -> p j t", j=4))
        for kc in range(16):
            P.mm(ps[0][:], xeT[:, kc, :], wg[b_][:, kc, :], start=(kc == 0), stop=(kc == 15))
        for kc in range(16):
            P.mm(ps[1][:], xeT[:, kc, :], wu[b_][:, kc, :], start=(kc == 0), stop=(kc == 15))
        P.act(hg[:], ps[0][:], AF.Silu)
        P.tt(hact[:], hg[:], ps[1][:], ALU.mult)
        pt = psb[0]
        for kc in range(4):
            P.tr(pt[:, kc * 128:(kc + 1) * 128], hact[:, kc * 128:(kc + 1) * 128], ident_bf[:])
        evac(actT[:], pt[:, 0:512].rearrange("p (j t) -> p j t", j=4))
        for cb in range(4):
            Yb = ps[2 + cb]
            for kc in range(4):
                P.mm(Yb[:], actT[:, kc, :], wd[b_][:, kc, cb * 512:(cb + 1) * 512], start=(kc == 0), stop=(kc == 3))
            evac(ye[b_][:, cb * 512:(cb + 1) * 512], Yb[:])
        P.dma("sp", yrows_d[e_ * CAP:(e_ + 1) * CAP, :], ye[b_][:])
    P.sb_release(m6b)
    yg = [P.sb(f"yg{i}", [128, D], F32) for i in range(2)]
    for i in range(NT_OWN):
        for k_ in range(2):
            y_ = yg[k_]
            P.memset(y_[:], 0.0, eng="pool")
            P.add("pool", lambda e, i=i, k_=k_, y_=y_: e.indirect_dma_start(
                out=y_[:], out_offset=None, in_=yrows_d,
                in_offset=bass.IndirectOffsetOnAxis(ap=dest_i[:, i, k_:k_ + 1], axis=0),
                bounds_check=NE * CAP - 1, oob_is_err=False),
                reads=[yrows_d, dest_i[:, i, k_:k_ + 1]], writes=[y_[:]], dma=True)
            P.stt(h1[:, i, :], y_[:], gates[:, i, k_:k_ + 1], h1[:, i, :], ALU.mult, ALU.add)
    P.sb_release(m6)
    if stages <= 6:
        if dbg:
            h_dbg = dscr("h2_s", [OWN, D], F32)
            P.dma("sp", h_dbg.rearrange("(t p) c -> p t c", p=128), h1[:])
            P.barrier()
        P.emit()
        return P

    p_d = din("p", [OWN, 256])
    wpp_d = din("w_ple_proj", [256, D])
    pleg_d = din("ple_g", [1, D])
    wpg_d = din("w_ple_gate", [D, D])
    bpg_d = din("b_ple_gate", [1, D])
    fing_d = din("final_g", [1, D])
    wpg = P.sb("wpg", [128, 16, D], BF16)
    wpg_v = wpg_d.rearrange("(kc p) n -> p kc n", p=128)
    for cb in range(4):
        for q4 in range(0, 16, 4):
            P.dma("pool", wpg[:, q4:q4 + 4, cb * 512:(cb + 1) * 512], wpg_v[:, q4:q4 + 4, cb * 512:(cb + 1) * 512])
    wpp = P.sb("wpp", [128, 2, D], BF16)
    P.dma("pool", wpp[:], wpp_d.rearrange("(kc p) n -> p kc n", p=128))
    pleg_bc = P.sb_at("pleg_bc", [128, D], F32, U_OFF)
    bpg_bc = P.sb_at("bpg_bc", [128, D], F32, U_OFF + 8192)
    fing_bc = P.sb_at("fing_bc", [128, D], F32, U_OFF + 16384)
    P.dma("sp", pleg_bc[:], pleg_d.to_broadcast([128, D]))
    P.dma("sp", bpg_bc[:], bpg_d.to_broadcast([128, D]))
    P.dma("sp", fing_bc[:], fing_d.to_broadcast([128, D]))
    hb = P.sb("hb", [128, D], BF16)
    hT = P.sb("hT", [128, 16, 128], BF16)
    pt_f = P.sb("pt_f", [128, 256], F32)
    pt_b = P.sb("pt_b", [128, 256], BF16)
    pT = P.sb("pT", [128, 2, 128], BF16)
    gate_sb = P.sb("gate_sb", [128, D], F32)
    ple_raw = P.sb("ple_raw", [128, D], F32)
    ple_n = ple_raw
    osb = [gate_sb, gate_sb]
    for i in range(NT_OWN):
        hrow = h1[:, i, :]
        P.copy(hb[:], hrow, eng="pool")
        for grp in range(4):
            pt = psb[grp % 2]
            for j in range(4):
                kc = grp * 4 + j
                P.tr(pt[:, j * 128:(j + 1) * 128], hb[:, kc * 128:(kc + 1) * 128], ident_bf[:])
            evac(hT[:, grp * 4:(grp + 1) * 4, :], pt[:, 0:512].rearrange("p (j t) -> p j t", j=4))
        P.dma("sp", pt_f[:], p_d[i * 128:(i + 1) * 128, :])
        P.copy(pt_b[:], pt_f[:])
        ptp = psb[0]
        for kc in range(2):
            P.tr(ptp[:, kc * 128:(kc + 1) * 128], pt_b[:, kc * 128:(kc + 1) * 128], ident_bf[:])
        evac(pT[:], ptp[:, 0:256].rearrange("p (j t) -> p j t", j=2))
        for cb in range(4):
            cs_ = slice(cb * 512, (cb + 1) * 512)
            G = ps[cb % 2]
            for kc in range(16):
                P.mm(G[:], hT[:, kc, :], wpg[:, kc, cs_], start=(kc == 0), stop=(kc == 15))
            P.tt(gate_sb[:, cs_], G[:], bpg_bc[:, cs_], ALU.add)
            Lp = ps[2 + cb % 2]
            for kc in range(2):
                P.mm(Lp[:], pT[:, kc, :], wpp[:, kc, cs_], start=(kc == 0), stop=(kc == 1))
            evac(ple_raw[:, cs_], Lp[:])
        P.act(gate_sb[:], gate_sb[:], AF.Sigmoid)
        P.stt(junk[:], ple_raw[:], 1.0, ple_raw[:], ALU.mult, ALU.mult, accum_out=ss[0][:, 0:1])
        P.act(ss[0][:, 1:2], ss[0][:, 0:1], AF.Sqrt, bias=eps_t[:, 0:1], scale=1.0 / D)
        P.add("dve", lambda e, o=ss[0][:, 1:2]: e.reciprocal(o, o), reads=[ss[0][:, 1:2]], writes=[ss[0][:, 1:2]])
        P.stt(ple_n[:], ple_raw[:], ss[0][:, 1:2], pleg_bc[:], ALU.mult, ALU.mult)
        P.tt(ple_n[:], ple_n[:], gate_sb[:], ALU.mult)
        P.tt(hrow, hrow, ple_n[:], ALU.add)
        o_ = osb[i % 2]
        P.stt(junk[:], hrow, 1.0, hrow, ALU.mult, ALU.mult, accum_out=ss[1][:, 0:1])
        P.act(ss[1][:, 1:2], ss[1][:, 0:1], AF.Sqrt, bias=eps_t[:, 0:1], scale=1.0 / D)
        P.add("dve", lambda e, o=ss[1][:, 1:2]: e.reciprocal(o, o), reads=[ss[1][:, 1:2]], writes=[ss[1][:, 1:2]])
        P.stt(o_[:], hrow, ss[1][:, 1:2], fing_bc[:], ALU.mult, ALU.mult)
        P.dma("sp", out_d[i * 128:(i + 1) * 128, :], o_[:])
    P.barrier()
    P.emit()
    return P


def prep_core_inputs(inp, c, shared):
    b, half = c // 2, c % 2
    flip = half == 1
    m = {}
    xb = inp["x"][b]
    m["x"] = np.ascontiguousarray(xb[::-1] if flip else xb)
    pos = inp["positions"][b]
    pos = pos[::-1] if flip else pos
    m["pos"] = np.ascontiguousarray(pos.reshape(NT_ALL, 128).T).astype(np.int32)
    m["consts"] = shared["consts"]
    m["g_mix"] = np.ascontiguousarray(inp["norm_mix_g"][0][None, :])
    m["w_in"] = shared["w_in_flip"] if flip else shared["w_in"]
    cw = inp["conv_w"][0]
    if flip:
        cw = cw[::-1]
    m["convw"] = np.ascontiguousarray(cw.T.reshape(12, 128, 5).transpose(1, 0, 2))
    m["convb"] = np.ascontiguousarray(inp["conv_b"][0].reshape(12, 128).T)
    f, r = ("dt_bias_b", "dt_bias_f") if flip else ("dt_bias_f", "dt_bias_b")
    m["dtb"] = np.concatenate([inp[f][0], inp[r][0]])[None, :].astype(np.float32)
    f, r = ("a_log_b", "a_log_f") if flip else ("a_log_f", "a_log_b")
    m["alog"] = np.concatenate([inp[f][0], inp[r][0]])[None, :].astype(np.float32)
    return m


def prep_shared(inp):
    sh = {}
    sh["consts"] = host_consts()
    w = np.ascontiguousarray(inp["w_in"][0])
    sh["w_in"] = w
    wf = w.copy()
    wf[:, C_DT:C_DT + 16] = w[:, C_DT + 16:C_DT + 32]
    wf[:, C_DT + 16:C_DT + 32] = w[:, C_DT:C_DT + 16]
    sh["w_in_flip"] = wf
    return sh


def prep_core_inputs_full(inp, c, shared):
    m = prep_core_inputs(inp, c, shared)
    b, half = c // 2, c % 2
    flip = half == 1
    m["dskip"] = np.ascontiguousarray(inp["d_skip"][0][None, :])
    m["ssd_g"] = np.ascontiguousarray(inp["ssd_norm_g"][0][None, :])
    m["lamv"] = shared["lamv"]
    m["subln_g"] = np.ascontiguousarray(inp["subln_g"][0][None, :])
    m["w_out"] = shared["w_out"]
    m["g_ffn"] = np.ascontiguousarray(inp["norm_ffn_g"][0][None, :])
    m["w_route"] = shared["w_route"]
    m["b_route"] = shared["b_route"]
    m["w_exp_gate"] = shared["w_exp_gate"]
    m["w_exp_up"] = shared["w_exp_up"]
    m["w_exp_down"] = shared["w_exp_down"]
    pb = inp["p"][0, b]
    pb = pb[::-1] if flip else pb
    m["p"] = np.ascontiguousarray(pb[:OWN])
    m["w_ple_proj"] = shared["w_ple_proj"]
    m["ple_g"] = np.ascontiguousarray(inp["ple_norm_g"][0][None, :])
    m["w_ple_gate"] = shared["w_ple_gate"]
    m["b_ple_gate"] = np.ascontiguousarray(inp["b_ple_gate"][0][None, :])
    m["final_g"] = np.ascontiguousarray(inp["final_norm_g"][None, :])
    return m


def prep_shared_full(inp):
    sh = prep_shared(inp)
    sh["lamv"] = np.concatenate([inp["lam_q1"][0], inp["lam_k1"][0], inp["lam_q2"][0], inp["lam_k2"][0]])[None, :].astype(np.float32)
    sh["w_out"] = np.ascontiguousarray(inp["w_out"][0])
    sh["w_route"] = np.ascontiguousarray(np.concatenate([inp["w_route_group"][0], inp["w_route_expert"][0]], axis=1))
    sh["b_route"] = np.concatenate([inp["b_route_group"][0], inp["b_route_expert"][0]])[None, :].astype(np.float32)
    sh["w_exp_gate"] = np.ascontiguousarray(inp["w_exp_gate"][0])
    sh["w_exp_up"] = np.ascontiguousarray(inp["w_exp_up"][0])
    sh["w_exp_down"] = np.ascontiguousarray(inp["w_exp_down"][0])
    sh["w_ple_proj"] = np.ascontiguousarray(inp["w_ple_proj"][0])
    sh["w_ple_gate"] = np.ascontiguousarray(inp["w_ple_gate"][0])
    return sh


def kernel(**inputs):
    inp = {k: np.asarray(v) for k, v in inputs.items()}
    nc = bass.Bass("TRN2", target_bir_lowering=False)
    build_program(nc, stages=99, dbg=False)
    sh = prep_shared_full(inp)
    maps = [prep_core_inputs_full(inp, c, sh) for c in range(8)]
    res = run_bass_kernel_spmd(nc, maps, core_ids=list(range(8)))
    out = np.zeros((4, SEQ, D), np.float32)
    for c in range(8):
        b, half = c // 2, c % 2
        o = np.asarray(res.results[c]["out"])
        if half == 0:
            out[b, 0:OWN] = o
        else:
            out[b, OWN:SEQ] = o[::-1]
    return out
```

```python
import numpy as np
import concourse.bass as bass
import concourse.mybir as mybir
from concourse.bass_utils import run_bass_kernel_spmd

F32 = mybir.dt.float32
BF16 = mybir.dt.bfloat16
I32 = mybir.dt.int32
AF = mybir.ActivationFunctionType
ALU = mybir.AluOpType
AX = mybir.AxisListType

SBUF_LO = 16512
SBUF_HI = 229344


class _Op:
    __slots__ = ("eng", "fn", "dma", "deps", "flag", "ordinal", "sem", "semval", "idx", "extra_waits")

    def __init__(self, eng, fn, dma):
        self.eng = eng
        self.fn = fn
        self.dma = dma
        self.deps = set()
        self.flag = False
        self.ordinal = -1
        self.sem = None
        self.semval = 0
        self.idx = -1
        self.extra_waits = None


def _box(ap):
    t = ap.tensor
    name = t.name
    cls = type(t).__name__
    try:
        pairs = [(int(s), int(c)) for s, c in ap.ap]
        off = int(ap.offset)
    except Exception:
        return (name, 0, 1 << 30, 0, 1 << 60)
    if cls.startswith("DRam"):
        lo = off
        hi = off
        for s, c in pairs:
            if s >= 0:
                hi += s * (c - 1)
            else:
                lo += s * (c - 1)
        return (name, 0, 1, lo, hi)
    shape = [int(v) for v in t.shape]
    rowlen = 1
    for v in shape[1:]:
        rowlen *= v
    esz = 4 if t.dtype in (F32, I32) else 2
    p0 = off // rowlen
    lo = off % rowlen
    ps, pc = pairs[0]
    p1 = p0 + (pc - 1) * (ps // rowlen) + 1
    hi = lo
    for s, c in pairs[1:]:
        hi += abs(s) * (c - 1)
    return (name, p0, p1, lo * esz, (hi + 1) * esz - 1)


def _overlap(a, b):
    return a[1] < b[2] and b[1] < a[2] and a[3] <= b[4] and b[3] <= a[4]


def _covers(a, b):
    return a[1] <= b[1] and a[2] >= b[2] and a[3] <= b[3] and a[4] >= b[4]


class Prog:
    ENGS = ("pe", "act", "dve", "pool", "sp")
    NDMA = 8

    def __init__(self, nc):
        self.nc = nc
        self.ops = []
        self.live = {}
        self.per_eng = {e: [] for e in self.ENGS}
        self.dma_count = {e: 0 for e in self.ENGS}
        self.dma_hist = {e: [] for e in self.ENGS}
        self.sb_ptr = SBUF_LO
        self.sb_names = 0
        self.psum = []

    def sb(self, name, shape, dtype):
        esz = 4 if dtype in (F32, I32) else 2
        n = 1
        for v in shape[1:]:
            n *= v
        nbytes = (n * esz + 31) // 32 * 32
        off = self.sb_ptr
        if off + nbytes > getattr(self, "sb_hi", SBUF_HI):
            raise RuntimeError(f"SBUF overflow allocating {name}: {off}+{nbytes}")
        self.sb_ptr += nbytes
        self.sb_names += 1
        return self.nc.alloc_sbuf_tensor_at(f"{name}_{self.sb_names}", list(shape), dtype, offset=off)

    def sb_top(self, name, shape, dtype):
        esz = 4 if dtype in (F32, I32) else 2
        n = 1
        for v in shape[1:]:
            n *= v
        nbytes = (n * esz + 31) // 32 * 32
        self.sb_hi = getattr(self, "sb_hi", SBUF_HI) - nbytes
        self.sb_names += 1
        return self.nc.alloc_sbuf_tensor_at(f"{name}_{self.sb_names}", list(shape), dtype, offset=self.sb_hi)

    def sb_at(self, name, shape, dtype, offset):
        self.sb_names += 1
        return self.nc.alloc_sbuf_tensor_at(f"{name}_{self.sb_names}", list(shape), dtype, offset=offset)

    def sb_mark(self):
        return self.sb_ptr

    def sb_release(self, mark):
        self.barrier()
        self.sb_ptr = mark

    def add(self, eng, fn, reads=(), writes=(), dma=False):
        op = _Op(eng, fn, dma)
        op.idx = len(self.ops)
        self.ops.append(op)
        self.per_eng[eng].append(op)
        for ap in reads:
            b = _box(ap)
            lst = self.live.setdefault(b[0], [])
            for ent in lst:
                if ent[2] and _overlap(ent[0], b):
                    op.deps.add(ent[1])
            if not dma:
                for ent in lst:
                    if (not ent[2]) and ent[0] == b and ent[1].eng == eng and not ent[1].dma:
                        ent[1] = op
                        break
                else:
                    lst.append([b, op, False])
            else:
                lst.append([b, op, False])
        for ap in writes:
            b = _box(ap)
            lst = self.live.setdefault(b[0], [])
            keep = []
            for ent in lst:
                if _overlap(ent[0], b):
                    if ent[1] is not op:
                        op.deps.add(ent[1])
                    if _covers(b, ent[0]):
                        continue
                keep.append(ent)
            keep.append([b, op, True])
            self.live[b[0]] = keep
        if dma:
            j = self.dma_count[eng]
            self.dma_count[eng] = j + 1
            op.sem = ("dma", eng, j % self.NDMA)
            op.semval = 16 * (j // self.NDMA + 1)
            hist = self.dma_hist[eng]
            if j >= self.NDMA:
                op.deps.add(hist[j - self.NDMA])
            hist.append(op)
        return op

    def core_barrier(self):
        self.barrier()
        self.seg_marks = getattr(self, "seg_marks", [])
        self.seg_marks.append(len(self.ops))

    def barrier(self):
        last = {e: (self.per_eng[e][-1] if self.per_eng[e] else None) for e in self.ENGS}
        dmas = []
        for e in self.ENGS:
            dmas.extend(self.dma_hist[e][-self.NDMA:])
        for e in self.ENGS:
            op = _Op(e, None, False)
            op.idx = len(self.ops)
            self.ops.append(op)
            self.per_eng[e].append(op)
            for e2 in self.ENGS:
                if last[e2] is not None and not last[e2].dma:
                    op.deps.add(last[e2])
                elif last[e2] is not None:
                    for o in reversed(self.per_eng[e2][:-1]):
                        if not o.dma:
                            op.deps.add(o)
                            break
            for d in dmas:
                op.deps.add(d)
        self.live = {}

    def emit(self):
        nc = self.nc
        for op in self.ops:
            for d in op.deps:
                if d.dma:
                    continue
                if d.eng == "pe" and op.eng == "pe" and not op.dma and op.fn is not None:
                    continue
                d.flag = True
        counts = {e: 0 for e in self.ENGS}
        for e in self.ENGS:
            for op in self.per_eng[e]:
                if op.dma:
                    continue
                if op.flag:
                    if op.fn is None:
                        op.flag = False
                        continue
                    counts[e] += 1
                    op.ordinal = counts[e]
        import contextlib
        with contextlib.ExitStack() as st:
            sems = {}
            for e in self.ENGS:
                if counts[e] > 0:
                    sems[("eng", e)] = st.enter_context(nc.semaphore(f"c_{e}"))
                if self.dma_count[e] > 0:
                    for k in range(min(self.NDMA, self.dma_count[e])):
                        sems[("dma", e, k)] = st.enter_context(nc.semaphore(f"d_{e}{k}"))
            marks = list(getattr(self, "seg_marks", [])) + [len(self.ops) + 1]
            waited_all = {e: {} for e in self.ENGS}
            pos = {e: 0 for e in self.ENGS}

            def run_engine(e, eng, upto):
                waited = waited_all[e]
                lst = self.per_eng[e]
                while pos[e] < len(lst) and lst[pos[e]].idx < upto:
                    op = lst[pos[e]]
                    pos[e] += 1
                    need = {}
                    for d in op.deps:
                        if d.dma:
                            key, val = d.sem, d.semval
                        else:
                            if d.eng == "pe" and e == "pe" and not op.dma and op.fn is not None:
                                continue
                            if d.ordinal < 0:
                                continue
                            key, val = ("eng", d.eng), d.ordinal
                        if need.get(key, 0) < val:
                            need[key] = val
                    for key, val in need.items():
                        if waited.get(key, 0) >= val:
                            continue
                        waited[key] = val
                        eng.wait_ge(sems[key], val)
                    if op.fn is None:
                        continue
                    try:
                        ins = op.fn(eng)
                    except Exception:
                        cl = op.fn.__closure__
                        print("EMIT FAIL op", op.idx, e, [str(c.cell_contents)[:300] for c in (cl or [])])
                        raise
                    if op.dma:
                        ins.then_inc(sems[op.sem], 16)
                    elif op.flag:
                        ins.then_inc(sems[("eng", e)], 1)

            for si, upto in enumerate(marks):
                with nc.Block() as block:

                    @block.tensor
                    def _(eng):
                        run_engine("pe", eng, upto)

                    @block.scalar
                    def _(eng):
                        run_engine("act", eng, upto)

                    @block.vector
                    def _(eng):
                        run_engine("dve", eng, upto)

                    @block.gpsimd
                    def _(eng):
                        run_engine("pool", eng, upto)

                    @block.sync
                    def _(eng):
                        run_engine("sp", eng, upto)

                if si + 1 < len(marks):
                    nc.all_core_barrier()

    def dma(self, q, out, in_, **kw):
        return self.add(q, lambda eng: eng.dma_start(out=out, in_=in_, **kw), reads=[in_], writes=[out], dma=True)

    def mm(self, out, lhsT, rhs, start=True, stop=True, **kw):
        rd = [lhsT, rhs]
        return self.add("pe", lambda eng: eng.matmul(out, lhsT, rhs, start=start, stop=stop, **kw),
                        reads=rd, writes=[out])

    def tr(self, out, in_, ident):
        return self.add("pe", lambda eng: eng.transpose(out, in_, ident), reads=[in_, ident], writes=[out])

    def act(self, out, in_, func, bias=None, scale=None, accum_out=None, eng="act"):
        kw = {}
        rd = [in_]
        wr = [out]
        if bias is not None:
            kw["bias"] = bias
            if not isinstance(bias, (int, float)):
                rd.append(bias)
        if scale is not None:
            kw["scale"] = scale
            if not isinstance(scale, (int, float)):
                rd.append(scale)
        if accum_out is not None:
            kw["accum_out"] = accum_out
            wr.append(accum_out)
        return self.add(eng, lambda e: e.activation(out, in_, func, **kw), reads=rd, writes=wr)

    def tt(self, out, in0, in1, op, eng="dve"):
        return self.add(eng, lambda e: e.tensor_tensor(out, in0, in1, op), reads=[in0, in1], writes=[out])

    def ts(self, out, in0, s1, s2, op0, op1=None, eng="dve", accum_out=None):
        rd = [in0]
        if not isinstance(s1, (int, float)):
            rd.append(s1)
        if s2 is not None and not isinstance(s2, (int, float)):
            rd.append(s2)
        wr = [out]
        kw = {}
        if accum_out is not None:
            kw["accum_out"] = accum_out
            wr.append(accum_out)
        if op1 is None:
            return self.add(eng, lambda e: e.tensor_scalar(out, in0, s1, None, op0, **kw), reads=rd, writes=wr)
        return self.add(eng, lambda e: e.tensor_scalar(out, in0, s1, s2, op0, op1, **kw), reads=rd, writes=wr)

    def stt(self, out, in0, scalar, in1, op0, op1, eng="dve", accum_out=None):
        rd = [in0, in1]
        if not isinstance(scalar, (int, float)):
            rd.append(scalar)
        wr = [out]
        kw = {}
        if accum_out is not None:
            kw["accum_out"] = accum_out
            wr.append(accum_out)
        return self.add(eng, lambda e: e.scalar_tensor_tensor(out, in0, scalar, in1, op0, op1, **kw),
                        reads=rd, writes=wr)

    def copy(self, out, in_, eng="dve"):
        if eng == "act":
            return self.add("act", lambda e: e.copy(out, in_), reads=[in_], writes=[out])
        return self.add(eng, lambda e: e.tensor_copy(out, in_), reads=[in_], writes=[out])

    def reduce(self, out, in_, op, axis=AX.X, eng="dve"):
        return self.add(eng, lambda e: e.tensor_reduce(out, in_, axis, op), reads=[in_], writes=[out])

    def memset(self, ap, val, eng="dve"):
        return self.add(eng, lambda e: e.memset(ap, val), reads=[], writes=[ap])


D = 2048
SEQ = 2048
OWN = 1024
NT_ALL = 16
NT_OWN = 8
IN_W = 5664
EPS = 1e-6
C_Z = 0
C_XBC = 1024
C_DT = 2560
C_Q = 2592
C_K = 3616
C_V = 4640
NEG = -1.0e30

K_IDENT = 0
K_TRIU = 128
K_TRIL = 256
K_ONES = 384
K_NMU = 512
K_NML = 640
K_SLT = 768
K_DELTA = 896
K_INVF = 912
K_IOTA = 920
NCONST = 984


def host_consts():
    c = np.zeros((128, NCONST), np.float32)
    s = np.arange(128)[:, None]
    l = np.arange(128)[None, :]
    c[:, K_IDENT:K_IDENT + 128] = (s == l)
    c[:, K_TRIU:K_TRIU + 128] = (s <= l)
    c[:, K_TRIL:K_TRIL + 128] = (s >= l)
    c[:, K_ONES:K_ONES + 128] = 1.0
    c[:, K_NMU:K_NMU + 128] = np.where(s <= l, 0.0, NEG)
    c[:, K_NML:K_NML + 128] = np.where(s >= l, 0.0, NEG)
    c[:, K_SLT:K_SLT + 128] = (s < l)
    c[:16, K_DELTA:K_DELTA + 16] = np.eye(16)
    inv_freq = (500000.0 ** (-np.arange(0, 16, 2, dtype=np.float32) / 16)).astype(np.float32)
    c[:, K_INVF:K_INVF + 8] = inv_freq[None, :]
    c[:, K_IOTA:K_IOTA + 64] = np.arange(64, dtype=np.float32)[None, :]
    return c


class Ctx:
    pass


def build_program(nc, stages=99, dbg=False):
    P = Prog(nc)
    nc.allow_low_precision("bf16 matmul operands, fp32 accumulation")
    okind = "ExternalOutput" if dbg else "Internal"

    def din(name, shape, dt=F32):
        return nc.dram_tensor(name, list(shape), dt, kind="ExternalInput").ap()

    def dscr(name, shape, dt):
        return nc.dram_tensor(name, list(shape), dt, kind=okind).ap()

    x_d = din("x", [SEQ, D])
    pos_d = din("pos", [128, NT_ALL], I32)
    consts_d = din("consts", [128, NCONST])
    g_mix_d = din("g_mix", [1, D])
    w_in_d = din("w_in", [D, IN_W])
    convw_d = din("convw", [128, 12, 5])
    convb_d = din("convb", [128, 12])
    dtb_d = din("dtb", [1, 32])
    alog_d = din("alog", [1, 32])
    out_d = nc.dram_tensor("out", [OWN, D], F32, kind="ExternalOutput").ap()

    z_d = dscr("z_s", [OWN, 1024], F32)
    q_d = dscr("q_s", [OWN, 1024], BF16)
    k_d = dscr("k_s", [SEQ, 1024], BF16)
    v_d = dscr("v_s", [SEQ, 1024], BF16)
    xsb_d = dscr("xsb_s", [SEQ, 1280], BF16)
    bc_d = dscr("bc_s", [512, SEQ], BF16)
    dt_d = dscr("dt_s", [SEQ, 64], F32)

    consts = P.sb("consts", [128, NCONST], F32)
    ident_bf = P.sb("ident_bf", [128, 128], BF16)
    DTDA_OFF = P.sb_ptr
    dtda = P.sb("dtda", [128, NT_ALL, 64], F32)
    P._tok = P.sb("cb_tok", [128, 8], F32)
    P._tokd = nc.dram_tensor("cb_tokd", [128, 2], F32).ap()
    P.memset(P._tok[:], 0.0)
    JUNK_OFF = P.sb_ptr
    junk = P.sb("junk", [128, D], BF16)
    ss = [P.sb(f"ss{i}", [128, 2], F32) for i in range(2)]
    eps_t = P.sb("eps_t", [128, 1], F32)
    P.memset(eps_t[:], EPS)
    u_sb = P.sb_top("u_sb", [128, NT_OWN, D], BF16)
    U_OFF = P.sb_hi
    ps = [nc.alloc_psum_tensor(f"ps{i}", [128, 512], F32) for i in range(8)]
    psb = [ps[6][:].bitcast(BF16), ps[7][:].bitcast(BF16)]

    P.dma("sp", consts[:], consts_d)
    P.copy(ident_bf[:], consts[:, K_IDENT:K_IDENT + 128])
    ident_f = consts[:, K_IDENT:K_IDENT + 128]

    evac_rr = [0]

    def evac(out, in_):
        evac_rr[0] += 1
        if evac_rr[0] % 2:
            return P.copy(out, in_, eng="act")
        return P.copy(out, in_, eng="dve")

    mark0 = P.sb_mark()
    nT = P.sb("nT", [128, 16, SEQ], BF16)
    mark1 = P.sb_mark()
    g_bc = P.sb("g_bc", [128, D], F32)
    xt = [P.sb(f"xt{i}", [128, D], F32) for i in range(2)]
    nb = [P.sb(f"nb{i}", [128, D], BF16) for i in range(2)]

    P.dma("sp", g_bc[:], g_mix_d.to_broadcast([128, D]))


    def rmsnorm_tile(src, dst_bf, gb, ssb, width):
        P.stt(junk[:, 0:width], src, 1.0, src, ALU.mult, ALU.mult, accum_out=ssb[:, 0:1])
        P.act(ssb[:, 1:2], ssb[:, 0:1], AF.Sqrt, bias=eps_t[:, 0:1], scale=1.0 / width)
        P.add("dve", lambda e, o=ssb[:, 1:2]: e.reciprocal(o, o), reads=[ssb[:, 1:2]], writes=[ssb[:, 1:2]])
        P.stt(dst_bf, src, ssb[:, 1:2], gb, ALU.mult, ALU.mult)

    for i in range(NT_ALL):
        xti = xt[i % 2]
        P.dma("sp", xti[:], x_d[i * 128:(i + 1) * 128, :])
        rmsnorm_tile(xti[:], nb[i % 2][:], g_bc[:], ss[i % 2], D)
        for grp in range(4):
            pt = psb[grp % 2]
            for j in range(4):
                kc = grp * 4 + j
                P.tr(pt[:, j * 128:(j + 1) * 128], nb[i % 2][:, kc * 128:(kc + 1) * 128], ident_bf[:])
            evac(nT[:, grp * 4:(grp + 1) * 4, i * 128:(i + 1) * 128],
                 pt[:, 0:512].rearrange("p (j t) -> p j t", j=4))
    P.sb_release(mark1)
    if stages <= 1:
        P.emit()
        return P

    wb = [P.sb(f"wb{i}", [128, 16, 512], BF16) for i in range(2)]
    w_in_v = w_in_d.rearrange("(kc p) n -> p kc n", p=128)
    wb_i = [0]

    def load_wblock(c0, ncols):
        b = wb[wb_i[0] % 2]
        wb_i[0] += 1
        for q4 in range(4):
            P.dma("pool", b[:, q4 * 4:(q4 + 1) * 4, 0:ncols], w_in_v[:, q4 * 4:(q4 + 1) * 4, c0:c0 + ncols])
        return b

    cs_sin = P.sb("sin", [128, NT_ALL, 8], F32)
    cs_cos = P.sb("cos", [128, NT_ALL, 8], F32)
    pos_i = P.sb("pos_i", [128, NT_ALL], I32)
    pos_sb = P.sb("pos", [128, NT_ALL], F32)
    ang = P.sb("ang", [128, NT_ALL, 8], F32)
    P.dma("sp", pos_i[:], pos_d)
    P.copy(pos_sb[:], pos_i[:])
    TWO_PI = float(2 * np.pi)
    P.tt(ang[:], pos_sb[:].unsqueeze(2).to_broadcast([128, NT_ALL, 8]),
         consts[:, K_INVF:K_INVF + 8].unsqueeze(1).to_broadcast([128, NT_ALL, 8]), ALU.mult)
    angi = P.sb("angi", [128, NT_ALL, 8], I32)
    angk = P.sb("angk", [128, NT_ALL, 8], F32)
    angm = P.sb("angm", [128, NT_ALL, 8], F32)
    PI = float(np.pi)

    def sin_of(dst, shift):
        P.ts(angk[:], ang[:], shift, 1.0 / TWO_PI, ALU.add, ALU.mult)
        P.copy(angi[:], angk[:])
        P.copy(angk[:], angi[:])
        P.ts(angm[:], ang[:], shift, None, ALU.add)
        P.stt(angm[:], angk[:], -TWO_PI, angm[:], ALU.mult, ALU.add)
        P.ts(angk[:], angm[:], PI, None, ALU.is_gt)
        P.stt(angm[:], angk[:], -TWO_PI, angm[:], ALU.mult, ALU.add)
        P.ts(angm[:], angm[:], -PI, PI, ALU.max, ALU.min)
        P.act(dst, angm[:], AF.Sin)

    sin_of(cs_sin[:], 0.0)
    sin_of(cs_cos[:], PI / 2)

    dtb_bc = P.sb("dtb_bc", [128, 32], F32)
    a_bc = P.sb("a_bc", [128, 32], F32)
    P.dma("sp", dtb_bc[:], dtb_d.to_broadcast([128, 32]))
    P.dma("sp", a_bc[:], alog_d.to_broadcast([128, 32]))
    P.act(a_bc[:], a_bc[:], AF.Exp)
    P.ts(a_bc[:], a_bc[:], -1.0, None, ALU.mult)

    stf = [P.sb(f"stf{i}", [128, 512], F32) for i in range(2)]
    stb = [P.sb(f"stb{i}", [128, 512], BF16) for i in range(2)]
    rtmp = P.sb("rtmp", [128, 4, 4, 8], F32)
    cnt = [0]

    def tokmajor_segment(c0, ncols_total, ntiles, kind, dst):
        for b0 in range(0, ncols_total, 512):
            ncols = min(512, ncols_total - b0)
            w = load_wblock(c0 + b0, ncols)
            for i in range(ntiles):
                cnt[0] += 1
                pst = ps[cnt[0] % 4]
                for kc in range(16):
                    P.mm(pst[:, 0:ncols], nT[:, kc, i * 128:(i + 1) * 128], w[:, kc, 0:ncols],
                         start=(kc == 0), stop=(kc == 15))
                sf = stf[cnt[0] % 2]
                sbf = stb[cnt[0] % 2]
                rows = slice(i * 128, (i + 1) * 128)
                if kind == "z":
                    evac(sf[:, 0:ncols], pst[:, 0:ncols])
                    P.dma("sp", dst[rows, b0:b0 + ncols], sf[:, 0:ncols])
                elif kind == "v":
                    evac(sbf[:, 0:ncols], pst[:, 0:ncols])
                    P.dma("sp", dst[rows, b0:b0 + ncols], sbf[:, 0:ncols])
                elif kind == "qk":
                    evac(sf[:, 0:ncols], pst[:, 0:ncols])
                    v4 = sf[:, 0:512].rearrange("p (g d) -> p g d", d=64)
                    t1 = v4[:, :, 0:8]
                    t2 = v4[:, :, 8:16]
                    cb = cs_cos[:, i, :].unsqueeze(1).to_broadcast([128, 8, 8])
                    sb_ = cs_sin[:, i, :].unsqueeze(1).to_broadcast([128, 8, 8])
                    r = rtmp[:].rearrange("p a b d -> p (a b) d")
                    P.tt(r[:, 0:8, :], t1, cb, ALU.mult, eng="pool")
                    P.tt(r[:, 8:16, :], t2, sb_, ALU.mult, eng="pool")
                    r2 = rtmp2[:].rearrange("p a b d -> p (a b) d")
                    P.tt(r2[:, 0:8, :], t2, cb, ALU.mult, eng="pool")
                    P.tt(r2[:, 8:16, :], t1, sb_, ALU.mult, eng="pool")
                    P.tt(t1, r[:, 0:8, :], r[:, 8:16, :], ALU.subtract, eng="pool")
                    P.tt(t2, r2[:, 0:8, :], r2[:, 8:16, :], ALU.add, eng="pool")
                    P.copy(sbf[:, 0:ncols], sf[:, 0:ncols], eng="pool")
                    P.dma("sp", dst[rows, b0:b0 + ncols], sbf[:, 0:ncols])
                elif kind == "dt":
                    d = dtda[:, i, :]
                    P.tt(d[:, 0:32], pst[:, 0:32], dtb_bc[:], ALU.add)
                    P.act(d[:, 0:32], d[:, 0:32], AF.Exp)
                    P.act(d[:, 0:32], d[:, 0:32], AF.Ln, bias=1.0)
                    P.tt(d[:, 32:64], d[:, 0:32], a_bc[:], ALU.mult)
                    if dbg:
                        P.dma("sp", dt_d[rows, :], d)

    rtmp2 = P.sb("rtmp2", [128, 4, 4, 8], F32)

    tokmajor_segment(C_DT, 32, NT_ALL, "dt", None)
    tokmajor_segment(C_Z, 1024, NT_OWN, "z", z_d)
    tokmajor_segment(C_Q, 1024, NT_OWN, "qk", q_d)
    tokmajor_segment(C_K, 1024, NT_ALL, "qk", k_d)
    tokmajor_segment(C_V, 1024, NT_ALL, "v", v_d)

    cw = P.sb("cw", [128, 12, 5], F32)
    cbias = P.sb("cbias", [128, 12], F32)
    P.dma("sp", cw[:], convw_d)
    P.dma("sp", cbias[:], convb_d)
    xcm = [P.sb(f"xcm{i}", [128, SEQ + 4], F32) for i in range(2)]
    for b_ in xcm:
        P.memset(b_[:, 0:2], 0.0)
        P.memset(b_[:, SEQ + 2:SEQ + 4], 0.0)
    cacc = P.sb("cacc", [128, SEQ], F32)
    so = [P.sb(f"so{i}", [128, SEQ], BF16) for i in range(2)]
    tm = [P.sb(f"tm{i}", [128, NT_ALL, 128], BF16) for i in range(2)]
    xsb_v = xsb_d.rearrange("(tt p) c -> p tt c", p=128)
    for blk in range(3):
        w = load_wblock(C_XBC + blk * 512, 512)
        for cc in range(4):
            ch = blk * 4 + cc
            xb = xcm[ch % 2]
            for tb in range(4):
                cnt[0] += 1
                pst = ps[cnt[0] % 4]
                for kc in range(16):
                    P.mm(pst[:], w[:, kc, cc * 128:(cc + 1) * 128], nT[:, kc, tb * 512:(tb + 1) * 512],
                         start=(kc == 0), stop=(kc == 15))
                evac(xb[:, 2 + tb * 512:2 + (tb + 1) * 512], pst[:])
            P.ts(cacc[:], xb[:, 0:SEQ], cw[:, ch, 0:1], None, ALU.mult)
            for k in range(1, 5):
                P.stt(cacc[:], xb[:, k:k + SEQ], cw[:, ch, k:k + 1], cacc[:], ALU.mult, ALU.add)
            s_ = so[ch % 2]
            P.act(s_[:], cacc[:], AF.Silu, bias=cbias[:, ch:ch + 1])
            if ch >= 8:
                P.dma("sp", bc_d[(ch - 8) * 128:(ch - 7) * 128, :], s_[:])
            if ch < 10:
                t_ = tm[ch % 2]
                for grp in range(4):
                    pt = psb[grp % 2]
                    for j in range(4):
                        tt_ = grp * 4 + j
                        P.tr(pt[:, j * 128:(j + 1) * 128], s_[:, tt_ * 128:(tt_ + 1) * 128], ident_bf[:])
                    evac(t_[:, grp * 4:(grp + 1) * 4, :], pt[:, 0:512].rearrange("p (j t) -> p j t", j=4))
                for hh in range(2):
                    P.dma("sp", xsb_v[:, hh * 8:(hh + 1) * 8, ch * 128:(ch + 1) * 128], t_[:, hh * 8:(hh + 1) * 8, :])
    P.sb_release(mark0)
    if stages <= 2:
        P.emit()
        return P

    def load_cast_block(dst, src_v, c0, ncols, nkc):
        step = 4
        for q4 in range(0, nkc, step):
            P.dma("pool", dst[:, q4:q4 + step, 0:ncols], src_v[:, q4:q4 + step, c0:c0 + ncols])

    dskip_d = din("dskip", [1, 16])
    ssdg_d = din("ssd_g", [1, 1024])
    m3 = P.sb_mark()
    yacc = P.sb("yacc", [128, NT_OWN, 1024], F32)
    Sin = P.sb("Sin", [128, 16, 64], F32)
    Sin_bf = P.sb("Sin_bf", [128, 16, 64], BF16)
    dskip_bc = P.sb("dskip_bc", [128, 16], F32)
    ssdg_bc = P.sb("ssdg_bc", [128, 1024], F32)
    P.dma("sp", dskip_bc[:], dskip_d.to_broadcast([128, 16]))
    P.dma("sp", ssdg_bc[:], ssdg_d.to_broadcast([128, 1024]))
    negm4 = [P.sb(f"negm4_{d_}", [128, 4, 128], F32) for d_ in range(2)]
    P.copy(negm4[0][:], consts[:, K_NMU:K_NMU + 128].unsqueeze(1).to_broadcast([128, 4, 128]))
    P.copy(negm4[1][:], consts[:, K_NML:K_NML + 128].unsqueeze(1).to_broadcast([128, 4, 128]))
    tri = [consts[:, K_TRIU:K_TRIU + 128], consts[:, K_TRIL:K_TRIL + 128]]
    ones_f = consts[:, K_ONES:K_ONES + 128]
    xsb_t = [P.sb(f"xsbt{i}", [128, 1280], BF16) for i in range(2)]
    bcm_t = [P.sb(f"bcm{i}", [128, 4, 128], BF16) for i in range(2)]
    cst = P.sb("cst", [128, 32], F32)
    wst = P.sb("wst", [128, 16], F32)
    dec = P.sb("dec", [128, 16], F32)
    ecs = P.sb("ecs", [128, 16], F32)
    xd = P.sb("xd", [128, 16, 64], BF16)
    xdw = P.sb("xdw", [128, 16, 64], BF16)
    cbT = P.sb("cbT", [128, 2, 128], F32)
    datri = P.sb("datri", [128, 16, 128], F32)
    arg = P.sb("arg", [128, 16, 128], F32)
    Mbf = P.sb("Mbf", [128, 16, 128], BF16)
    ytmp = P.sb("ytmp", [128, 16, 64], F32)
    bc_v = bc_d.rearrange("(j n) t -> n j t", n=128)
    P.memset(Sin[:], 0.0)
    P.memset(Sin_bf[:], 0.0)
    it3 = [0]

    def ssd_chunk(c, d_, out):
        it3[0] += 1
        xt_ = xsb_t[it3[0] % 2]
        bt = bcm_t[it3[0] % 2]
        P.dma("sp", xt_[:], xsb_d[c * 128:(c + 1) * 128, :])
        if out:
            P.dma("sp", bt[:], bc_v[:, :, c * 128:(c + 1) * 128])
        dt_ = dtda[:, c, d_ * 16:(d_ + 1) * 16]
        da = dtda[:, c, 32 + d_ * 16:32 + (d_ + 1) * 16]
        A = ps[4]
        P.mm(A[:, 0:16], tri[d_], da)
        P.mm(A[:, 16:32], ones_f, da)
        P.copy(cst[:], A[:, 0:32])
        P.tt(wst[:], cst[:, 16:32], cst[:, 0:16], ALU.subtract)
        P.act(wst[:], wst[:], AF.Exp)
        P.act(dec[:], cst[:, 16:32], AF.Exp)
        xs3 = xt_[:, 0:1024].rearrange("p (h e) -> p h e", e=64)
        P.tt(xd[:], xs3, dt_.unsqueeze(2).to_broadcast([128, 16, 64]), ALU.mult)
        P.tt(xdw[:], xd[:], wst[:].unsqueeze(2).to_broadcast([128, 16, 64]), ALU.mult)
        if out:
            for g in range(2):
                P.mm(A[:, 256 + g * 128:256 + (g + 1) * 128], bt[:, g, :], bt[:, 2 + g, :])
            P.copy(cbT[:], A[:, 256:512].rearrange("p (g l) -> p g l", g=2))
            P.tt(datri[:], tri[d_].unsqueeze(1).to_broadcast([128, 16, 128]),
                 da.unsqueeze(2).to_broadcast([128, 16, 128]), ALU.mult)
            for j in range(4):
                R = ps[j]
                P.mm(R[:], ones_f, datri[:, 4 * j:4 * j + 4, :].rearrange("p h l -> p (h l)"), start=True, stop=False)
                P.mm(R[:], ident_f, negm4[d_][:].rearrange("p h l -> p (h l)"), start=False, stop=True)
                P.tt(arg[:, 4 * j:4 * j + 4, :], R[:].rearrange("p (h l) -> p h l", h=4),
                     cst[:, 4 * j:4 * j + 4].unsqueeze(2).to_broadcast([128, 4, 128]), ALU.subtract)
            P.act(arg[:], arg[:], AF.Exp)
            for g in range(2):
                P.tt(Mbf[:, g * 8:(g + 1) * 8, :], arg[:, g * 8:(g + 1) * 8, :],
                     cbT[:, g, :].unsqueeze(1).to_broadcast([128, 8, 128]), ALU.mult)
            for h in range(16):
                Y = ps[5 + h // 8]
                P.mm(Y[:, (h % 8) * 64:(h % 8 + 1) * 64], Mbf[:, h, :], xd[:, h, :])
            for g in range(2):
                P.mm(ps[g][:], bt[:, 2 + g, :], Sin_bf[:, g * 8:(g + 1) * 8, :].rearrange("p h e -> p (h e)"))
            P.act(ecs[:], cst[:, 0:16], AF.Exp)
            for g in range(2):
                hs = slice(g * 8, (g + 1) * 8)
                ysl = yacc[:, c, g * 512:(g + 1) * 512].rearrange("p (h e) -> p h e", e=64)
                P.tt(ytmp[:, hs, :], ps[g][:].rearrange("p (h e) -> p h e", e=64),
                     ecs[:, hs].unsqueeze(2).to_broadcast([128, 8, 64]), ALU.mult)
                P.tt(ytmp[:, hs, :], ytmp[:, hs, :], ps[5 + g][:].rearrange("p (h e) -> p h e", e=64), ALU.add)
                if d_ == 0:
                    P.tt(ysl, xs3[:, hs, :], dskip_bc[:, hs].unsqueeze(2).to_broadcast([128, 8, 64]), ALU.mult)
                P.tt(ysl, ysl, ytmp[:, hs, :], ALU.add)
        for g in range(2):
            P.mm(ps[2 + g][:], xt_[:, 1024 + g * 128:1024 + (g + 1) * 128],
                 xdw[:, g * 8:(g + 1) * 8, :].rearrange("p h e -> p (h e)"))
        P.tt(Sin[:], Sin[:], dec[:].unsqueeze(2).to_broadcast([128, 16, 64]), ALU.mult)
        for g in range(2):
            sl = Sin[:, g * 8:(g + 1) * 8, :]
            P.tt(sl, sl, ps[2 + g][:].rearrange("p (h e) -> p h e", e=64), ALU.add)
        P.copy(Sin_bf[:], Sin[:], eng="act")

    for c in range(NT_OWN):
        ssd_chunk(c, 0, True)
    P.memset(Sin[:], 0.0)
    P.memset(Sin_bf[:], 0.0)
    for c in range(NT_ALL - 1, NT_OWN - 1, -1):
        ssd_chunk(c, 1, False)
    for c in range(NT_OWN - 1, -1, -1):
        ssd_chunk(c, 1, True)
    ztile = [P.sb(f"zt{i}", [128, 1024], F32) for i in range(2)]
    gy = P.sb("gy", [128, 1024], F32)
    for c in range(NT_OWN):
        zt = ztile[c % 2]
        P.dma("sp", zt[:], z_d[c * 128:(c + 1) * 128, :])
        P.act(zt[:], zt[:], AF.Silu)
        P.tt(gy[:], yacc[:, c, :], zt[:], ALU.mult)
        rmsnorm_tile(gy[:], u_sb[:, c, 0:1024], ssdg_bc[:], ss[0], 1024)
    P.sb_release(m3)
    if stages <= 3:
        if dbg:
            u_dbg = dscr("u_s", [OWN, D], BF16)
            P.dma("sp", u_dbg.rearrange("(t p) c -> p t c", p=128), u_sb[:])
            P.barrier()
        P.emit()
        return P

    LAM_INIT = 0.8 - 0.6 * float(np.exp(-0.3 * 0))
    lamv_d = din("lamv", [1, 256])
    subg_d = din("subln_g", [1, 128])
    lam_sb = P.sb("lam_sb", [128, 256], F32)
    ls = P.sb("ls", [128, 4], F32)
    nlam = P.sb("nlam", [128, 1], F32)
    gsub_bc = P.sb("gsub_bc", [128, 128], F32)
    P.dma("sp", lam_sb[:], lamv_d.to_broadcast([128, 256]))
    P.dma("sp", gsub_bc[:], subg_d.to_broadcast([128, 128]))
    P.ts(gsub_bc[:], gsub_bc[:], 1.0 - LAM_INIT, None, ALU.mult)
    P.stt(junk[:, 0:64], lam_sb[:, 0:64], 1.0, lam_sb[:, 64:128], ALU.mult, ALU.mult, accum_out=ls[:, 0:1])
    P.stt(junk[:, 0:64], lam_sb[:, 128:192], 1.0, lam_sb[:, 192:256], ALU.mult, ALU.mult, accum_out=ls[:, 1:2])
    P.act(ls[:, 0:2], ls[:, 0:2], AF.Exp)
    P.tt(ls[:, 2:3], ls[:, 0:1], ls[:, 1:2], ALU.subtract)
    P.ts(nlam[:], ls[:, 2:3], LAM_INIT, -1.0, ALU.add, ALU.mult)
    ktm = P.sb("ktm", [128, NT_ALL, 128], BF16)
    vaug = [P.sb(f"vaug{i}", [128, NT_ALL, 160], BF16) for i in range(2)]
    qtm = P.sb("qtm", [128, NT_OWN, 128], BF16)
    kT = [P.sb(f"kT{i}", [128, SEQ], BF16) for i in range(2)]
    qT = [P.sb(f"qT{i}", [128, OWN], BF16) for i in range(2)]
    qTz = [[P.sb(f"qTz{i}_{c}", [128, OWN], BF16) for c in range(2)] for i in range(2)]
    for i_ in range(2):
        for c_ in range(2):
            P.memset(qTz[i_][c_][:], 0.0)
    ksq = P.sb("ksq", [128, SEQ], BF16)
    qsq = P.sb("qsq", [128, OWN], BF16)
    negm = [P.sb(f"negm{i}", [1, OWN], BF16) for i in range(2)]
    kmx = P.sb("kmx", [1, 16], F32)
    qn = P.sb("qn", [1, 512], F32)
    nbias = P.sb("nbias", [128, 2], F32)
    NPT = 8
    PT = [P.sb(f"PT{i}", [128, 512], BF16) for i in range(NPT)]
    ones_b = P.sb("ones_b", [128, 128], BF16)
    P.copy(ones_b[:], consts[:, K_ONES:K_ONES + 128])
    osub = P.sb("osub", [128, 128], F32)
    otmp = P.sb("otmp", [128, 128], F32)
    rs_ = P.sb("rs_", [128, 4], F32)
    for b_ in vaug:
        P.memset(b_[:], 1.0)
    k_v = k_d.rearrange("(t p) c -> p t c", p=128)
    v_v = v_d.rearrange("(t p) c -> p t c", p=128)
    q_v = q_d.rearrange("(t p) c -> p t c", p=128)

    def rmsnorm_ln(src, dst_bf, gb, ssb, width):
        P.stt(junk[:, 0:width], src, 1.0, src, ALU.mult, ALU.mult, accum_out=ssb[:, 0:1])
        P.act(ssb[:, 1:2], ssb[:, 0:1], AF.Ln, bias=eps_t[:, 0:1], scale=1.0 / width)
        P.act(ssb[:, 1:2], ssb[:, 1:2], AF.Exp, scale=-0.5)
        P.stt(dst_bf, src, ssb[:, 1:2], gb, ALU.mult, ALU.mult)

    ptc = [0]
    for h in range(8):
        cs_ = slice(h * 128, (h + 1) * 128)
        va = vaug[h % 2]
        P.dma("sp", ktm[:], k_v[:, :, cs_])
        P.dma("sp", va[:, :, 0:128], v_v[:, :, cs_])
        P.dma("sp", qtm[:], q_v[:, :, cs_])
        kT_ = kT[h % 2]
        qT_ = qT[h % 2]
        for grp in range(4):
            pt = psb[grp % 2]
            for j in range(4):
                P.tr(pt[:, j * 128:(j + 1) * 128], ktm[:, grp * 4 + j, :], ident_bf[:])
            evac(kT_[:, grp * 512:(grp + 1) * 512], pt[:, 0:512])
        for grp in range(2):
            pt = psb[grp % 2]
            for j in range(4):
                P.tr(pt[:, j * 128:(j + 1) * 128], qtm[:, grp * 4 + j, :], ident_bf[:])
            evac(qTz[h % 2][0][0:64, grp * 512:(grp + 1) * 512], pt[0:64, 0:512])
            evac(qTz[h % 2][1][64:128, grp * 512:(grp + 1) * 512], pt[64:128, 0:512])
        P.tt(ksq[:], kT_[:], kT_[:], ALU.mult)
        P.tt(qsq[0:64, :], qTz[h % 2][0][0:64, :], qTz[h % 2][0][0:64, :], ALU.mult, eng="pool")
        P.tt(qsq[64:128, :], qTz[h % 2][1][64:128, :], qTz[h % 2][1][64:128, :], ALU.mult, eng="pool")
        for c in range(2):
            pr = slice(c * 64, (c + 1) * 64)
            for kb in range(4):
                P.mm(ps[5][0:1, :], ones_b[pr, 0:1], ksq[pr, kb * 512:(kb + 1) * 512])
                P.reduce(kmx[:, c * 4 + kb:c * 4 + kb + 1], ps[5][0:1, :], ALU.max)
            P.reduce(kmx[:, 8 + c:9 + c], kmx[:, c * 4:c * 4 + 4], ALU.max)
            for qb in range(2):
                P.mm(ps[5][0:1, :], ones_b[pr, 0:1], qsq[pr, qb * 512:(qb + 1) * 512])
                P.reduce(kmx[:, 10 + c * 2 + qb:11 + c * 2 + qb], ps[5][0:1, :], ALU.max)
            P.reduce(kmx[:, 14 + c:15 + c], kmx[:, 10 + c * 2:12 + c * 2], ALU.max)
        P.tt(qn[:, 0:2], kmx[:, 8:10], kmx[:, 14:16], ALU.mult)
        P.ts(qn[:, 0:2], qn[:, 0:2], 1e-20, None, ALU.add)
        P.act(qn[:, 0:2], qn[:, 0:2], AF.Ln)
        P.act(qn[:, 0:2], qn[:, 0:2], AF.Exp, scale=0.5)
        P.ts(qn[:, 2:4], qn[:, 0:2], -0.125, None, ALU.mult)
        P.mm(ps[5][:, 0:2], consts[0:1, K_ONES:K_ONES + 128], qn[0:1, 2:4])
        P.copy(nbias[:], ps[5][:, 0:2])
        for qb in range(2):
            qs_ = slice(qb * 512, (qb + 1) * 512)

            def acc(g):
                return ps[2 + g // 3][:, (g % 3) * 160:(g % 3) * 160 + 129]

            SB = (0, 1, 5, 6, 7)
            LA = 4

            def s_tile(T):
                kt, c = T // 2, T % 2
                pr = slice(c * 64, (c + 1) * 64)
                Sb = ps[SB[T % 5]]
                P.mm(Sb[:], kT_[:, kt * 128:(kt + 1) * 128], qTz[h % 2][c][:, qs_])

            NTILE = 2 * NT_ALL
            for T in range(min(LA, NTILE)):
                s_tile(T)
            for T in range(NTILE):
                if T + LA < NTILE:
                    s_tile(T + LA)
                kt, c = T // 2, T % 2
                Sb = ps[SB[T % 5]]
                ptc[0] += 1
                pt_ = PT[ptc[0] % NPT]
                P.act(pt_[:], Sb[:], AF.Exp, bias=nbias[:, c:c + 1], scale=0.125)
                for qt in range(4):
                    g_ = qt * 2 + c
                    P.mm(acc(g_), pt_[:, qt * 128:(qt + 1) * 128], va[:, kt, 0:129],
                         start=(kt == 0 and g_ in (0, 4, 6)), stop=(kt == NT_ALL - 1),
                         skip_group_check=True)
            for qt in range(4):
                O1 = acc(qt * 2)
                O2 = acc(qt * 2 + 1)
                P.add("dve", lambda e, o=rs_[:, 0:1], i_=O1[:, 128:129]: e.reciprocal(o, i_), reads=[O1[:, 128:129]], writes=[rs_[:, 0:1]])
                P.add("dve", lambda e, o=rs_[:, 1:2], i_=O2[:, 128:129]: e.reciprocal(o, i_), reads=[O2[:, 128:129]], writes=[rs_[:, 1:2]])
                P.tt(rs_[:, 2:3], rs_[:, 1:2], nlam[:], ALU.mult)
                P.ts(otmp[:], O2[:, 0:128], rs_[:, 2:3], None, ALU.mult)
                P.stt(osub[:], O1[:, 0:128], rs_[:, 0:1], otmp[:], ALU.mult, ALU.add)
                rmsnorm_ln(osub[:], u_sb[:, qb * 4 + qt, 1024 + h * 128:1024 + (h + 1) * 128], gsub_bc[:], ss[1], 128)
    P.sb_release(m3)
    if stages <= 4:
        if dbg:
            u_dbg = dscr("u_s", [OWN, D], BF16)
            P.dma("sp", u_dbg.rearrange("(t p) c -> p t c", p=128), u_sb[:])
            P.barrier()
        P.emit()
        return P

    w_out_d = din("w_out", [D, D])
    h1 = P.sb_top("h1", [128, NT_OWN, D], F32)
    m5 = P.sb_mark()
    uT = P.sb("uT", [128, 16, OWN], BF16)
    wb2 = [P.sb(f"wb2_{i}", [128, 16, 512], BF16) for i in range(2)]
    xo = [P.sb(f"xo{i}", [128, 512], F32) for i in range(2)]
    for i in range(NT_OWN):
        for grp in range(4):
            pt = psb[grp % 2]
            for j in range(4):
                kc = grp * 4 + j
                P.tr(pt[:, j * 128:(j + 1) * 128], u_sb[:, i, kc * 128:(kc + 1) * 128], ident_bf[:])
            evac(uT[:, grp * 4:(grp + 1) * 4, i * 128:(i + 1) * 128], pt[:, 0:512].rearrange("p (j t) -> p j t", j=4))
    wo_v = w_out_d.rearrange("(kc p) n -> p kc n", p=128)
    for cb in range(4):
        w = wb2[cb % 2]
        load_cast_block(w, wo_v, cb * 512, 512, 16)
        for i in range(NT_OWN):
            cnt[0] += 1
            pst = ps[cnt[0] % 4]
            xo_ = xo[cnt[0] % 2]
            P.dma("sp", xo_[:], x_d[i * 128:(i + 1) * 128, cb * 512:(cb + 1) * 512])
            for kc in range(16):
                P.mm(pst[:], uT[:, kc, i * 128:(i + 1) * 128], w[:, kc, :], start=(kc == 0), stop=(kc == 15))
            P.tt(h1[:, i, cb * 512:(cb + 1) * 512], pst[:], xo_[:], ALU.add)
    P.sb_release(m5)
    if stages <= 5:
        if dbg:
            h_dbg = dscr("h1_s", [OWN, D], F32)
            P.dma("sp", h_dbg.rearrange("(t p) c -> p t c", p=128), h1[:])
            P.barrier()
        P.emit()
        return P

    NE = 64
    NEL = 32
    NB = 2
    CAP = 128 * NB
    NTG = 16
    gffn_d = din("g_ffn", [1, D])
    wr_d = din("w_route", [D, 72])
    br_d = din("b_route", [1, 72])
    weg_d = din("w_exp_gate", [NEL, D, 512])
    weu_d = din("w_exp_up", [NEL, D, 512])
    wed_d = din("w_exp_down", [NEL, 512, D])
    yidx_d = din("yrow_idx", [128, NB * NEL], I32)

    def dshared(name, shape, dt):
        return nc.dram_tensor(name, list(shape), dt, addr_space="Shared").ap()

    n2_sh = dshared("n2_sh", [3, OWN, D], BF16)
    a_sh = dshared("a_sh", [2, 2, 128, NT_OWN, NE], BF16)
    y_sh = dshared("y_sh", [NE * CAP + 128, D], BF16)
    rowtok_d = dscr("rowtok_s", [CAP * NE + 64, 1], I32)
    dsty_d = dscr("dsty_s", [NTG, 128, 2], I32)
    n2_flat = n2_sh.rearrange("h t d -> (h t) d")
    y_flat = y_sh
    half_sp = bass.ds(nc.sync.partition_id() % 2, 1)

    m6 = P.sb_mark()
    gates = P.sb("gates", [128, NT_OWN, 2], F32)
    dest_own = P.sb("dest_own", [128, NT_OWN, 2], I32)
    rowtok = P.sb("rowtok", [128, NB, NEL], I32)
    yrow_idx = P.sb("yrow_idx", [128, NB, NEL], I32)
    P.dma("sp", yrow_idx[:], yidx_d.rearrange("p (n e) -> p n e", n=NB))
    m6r = P.sb_mark()
    gffn_bc = P.sb("gffn_bc", [128, D], F32)
    P.dma("sp", gffn_bc[:], gffn_d.to_broadcast([128, D]))
    wr_sb = P.sb("wr_sb", [128, 16, 72], F32)
    P.dma("sp", wr_sb[:], wr_d.rearrange("(kc p) n -> p kc n", p=128))
    br_bc = P.sb("br_bc", [128, 72], F32)
    P.dma("sp", br_bc[:], br_d.to_broadcast([128, 72]))
    n2f = [P.sb(f"n2f{i}", [128, D], F32) for i in range(2)]
    n2b = [P.sb(f"n2b{i}", [128, D], BF16) for i in range(2)]
    n2T = P.sb("n2T", [128, 16, 128], F32)
    A12 = P.sb("A12", [128, 2, NT_OWN, NE], BF16)
    lg = P.sb("lg", [128, 72], F32)
    rt = P.sb("rt", [128, 16], F32)
    Gm = P.sb("Gm", [128, 8], F32)
    tmp88 = P.sb("tmp88", [128, 8, 8], F32)
    esel = P.sb("esel", [128, 8], F32)
    e2 = P.sb("e2", [128, 8], F32)
    mk1 = P.sb("mk1", [128, 8], F32)
    mk2 = P.sb("mk2", [128, 8], F32)
    zrow = P.sb("zrow", [128, D], BF16)
    P.memset(zrow[:], 0.0)
    P.dma("sp", n2_sh[2, 0:128, :], zrow[:])
    rinit = P.sb("rinit", [128, NB * NE], I32)
    P.memset(rinit[:], 2 * OWN)
    P.dma("sp", rowtok_d[0:CAP * NE, :].rearrange("(r x) o -> r (x o)", r=128), rinit[:])
    P.dma("sp", y_sh[NE * CAP:NE * CAP + 128, :], zrow[:])
    ones_bf = P.sb("ones_bf", [128, 128], BF16)
    slt_bf = P.sb("slt_bf", [128, 128], BF16)
    P.copy(ones_bf[:], consts[:, K_ONES:K_ONES + 128])
    P.copy(slt_bf[:], consts[:, K_SLT:K_SLT + 128])
    iota_e = consts[:, K_IOTA:K_IOTA + 64]

    for i in range(NT_OWN):
        nf = n2f[i % 2]
        nbb = n2b[i % 2]
        P.stt(junk[:], h1[:, i, :], 1.0, h1[:, i, :], ALU.mult, ALU.mult, accum_out=ss[0][:, 0:1])
        P.act(ss[0][:, 1:2], ss[0][:, 0:1], AF.Sqrt, bias=eps_t[:, 0:1], scale=1.0 / D)
        P.add("dve", lambda e, o=ss[0][:, 1:2]: e.reciprocal(o, o), reads=[ss[0][:, 1:2]], writes=[ss[0][:, 1:2]])
        P.stt(nf[:], h1[:, i, :], ss[0][:, 1:2], gffn_bc[:], ALU.mult, ALU.mult)
        P.copy(nbb[:], nf[:], eng="pool")
        P.dma("sp", n2_sh.rearrange("h (t p) d -> p h t d", p=128)[:, half_sp, i, :], nbb[:].unsqueeze(1))
        for grp in range(4):
            pt = ps[grp % 2]
            for j in range(4):
                kc = grp * 4 + j
                P.tr(pt[:, j * 128:(j + 1) * 128], nf[:, kc * 128:(kc + 1) * 128], ident_f)
            evac(n2T[:, grp * 4:(grp + 1) * 4, :], pt[:].rearrange("p (j t) -> p j t", j=4))
        for kc in range(16):
            P.mm(ps[2][:, 0:72], n2T[:, kc, :], wr_sb[:, kc, :], start=(kc == 0), stop=(kc == 15))
        P.tt(lg[:], ps[2][:, 0:72], br_bc[:], ALU.add)
        P.reduce(rt[:, 0:1], lg[:, 0:8], ALU.max)
        P.ts(Gm[:], lg[:, 0:8], rt[:, 0:1], None, ALU.is_equal)
        P.ts(rt[:, 1:2], rt[:, 0:1], -1.0, None, ALU.mult)
        P.act(tmp88[:, 0, :], lg[:, 0:8], AF.Exp, bias=rt[:, 1:2], accum_out=rt[:, 2:3])
        P.add("dve", lambda e, o=rt[:, 2:3]: e.reciprocal(o, o), reads=[rt[:, 2:3]], writes=[rt[:, 2:3]])
        P.tt(tmp88[:], lg[:, 8:72].rearrange("p (g e) -> p g e", g=8), Gm[:].unsqueeze(2).to_broadcast([128, 8, 8]), ALU.mult)
        P.reduce(esel[:], tmp88[:].rearrange("p g e -> p e g"), ALU.add)
        P.reduce(rt[:, 3:4], esel[:], ALU.max)
        P.ts(mk1[:], esel[:], rt[:, 3:4], None, ALU.is_equal)
        P.stt(e2[:], mk1[:], NEG, esel[:], ALU.mult, ALU.add)
        P.reduce(rt[:, 4:5], e2[:], ALU.max)
        P.ts(mk2[:], e2[:], rt[:, 4:5], None, ALU.is_equal)
        P.tt(rt[:, 5:6], rt[:, 4:5], rt[:, 3:4], ALU.subtract)
        P.act(rt[:, 5:6], rt[:, 5:6], AF.Exp)
        P.ts(rt[:, 5:6], rt[:, 5:6], 1.0, None, ALU.add)
        P.add("dve", lambda e, o=rt[:, 5:6]: e.reciprocal(o, o), reads=[rt[:, 5:6]], writes=[rt[:, 5:6]])
        P.ts(rt[:, 6:7], rt[:, 5:6], -1.0, 1.0, ALU.mult, ALU.add)
        P.tt(gates[:, i, 0:1], rt[:, 5:6], rt[:, 2:3], ALU.mult)
        P.tt(gates[:, i, 1:2], rt[:, 6:7], rt[:, 2:3], ALU.mult)
        P.tt(A12[:, 0, i, :].rearrange("p (g e) -> p g e", g=8), Gm[:].unsqueeze(2).to_broadcast([128, 8, 8]),
             mk1[:].unsqueeze(1).to_broadcast([128, 8, 8]), ALU.mult)
        P.tt(A12[:, 1, i, :].rearrange("p (g e) -> p g e", g=8), Gm[:].unsqueeze(2).to_broadcast([128, 8, 8]),
             mk2[:].unsqueeze(1).to_broadcast([128, 8, 8]), ALU.mult)
    for k_ in range(2):
        P.dma("sp", a_sh.rearrange("h k p t e -> p h k t e")[:, half_sp, k_, :, :], A12[:, k_, :, :].unsqueeze(1))
    P.core_barrier()

    Ag = P.sb("Ag", [128, 2, NTG, NE], BF16)
    for k_ in range(2):
        for hh in range(2):
            P.dma("sp", Ag[:, k_, hh * NT_OWN:(hh + 1) * NT_OWN, :], a_sh[hh, k_, :, :, :])
    Aall = P.sb("Aall", [128, NTG, NE], BF16)
    P.tt(Aall[:], Ag[:, 0, :, :], Ag[:, 1, :, :], ALU.add)
    tokidx = P.sb("tokidx", [128, NTG], I32)
    tokf = P.sb("tokf", [128, NTG], F32)
    rk = P.sb("rk", [128, NE], F32)
    ovf = P.sb("ovf", [128, NE], F32)
    dsty_f = P.sb("dsty_f", [128, NTG, 2], F32)
    dsty_i = P.sb("dsty_i", [128, NTG, 2], I32)
    dst2_f = P.sb("dst2_f", [128, NTG, 2], F32)
    dst2_i = P.sb("dst2_i", [128, NTG, 2], I32)
    pidx = P.sb("pidx", [128, 1], F32)
    P.reduce(pidx[:], consts[:, K_SLT:K_SLT + 128], ALU.add)
    P.ts(pidx[:], pidx[:], -1.0, 127.0, ALU.mult, ALU.add)
    for i in range(NTG):
        P.ts(tokf[:, i:i + 1], pidx[:], 1.0, float(i * 128), ALU.mult, ALU.add)
    P.copy(tokidx[:], tokf[:])
    for i in range(NTG):
        R = ps[3]
        for j in range(i):
            P.mm(R[:, 0:NE], ones_bf[:], Aall[:, j, :], start=(j == 0), stop=False)
        P.mm(R[:, 0:NE], slt_bf[:], Aall[:, i, :], start=(i == 0), stop=True)
        for k_ in range(2):
            pass
        P.ts(ovf[:], R[:, 0:NE], float(CAP), 1.0e6, ALU.is_ge, ALU.mult)
        P.stt(rk[:], iota_e, float(CAP), R[:, 0:NE], ALU.mult, ALU.add)
        P.tt(rk[:], rk[:], ovf[:], ALU.add)
        for k_ in range(2):
            P.tt(ovf[:], rk[:], Ag[:, k_, i, :], ALU.mult)
            P.reduce(dsty_f[:, i, k_:k_ + 1], ovf[:], ALU.add)
        P.ts(dsty_f[:, i, :], dsty_f[:, i, :], float(NE * CAP), None, ALU.min)
        P.ts(ovf[:], R[:, 0:NE], float(CAP), 1.0e6, ALU.is_ge, ALU.mult)
        P.stt(rk[:], R[:, 0:NE], float(NE), iota_e, ALU.mult, ALU.add)
        P.tt(rk[:], rk[:], ovf[:], ALU.add)
        for k_ in range(2):
            P.tt(ovf[:], rk[:], Ag[:, k_, i, :], ALU.mult)
            P.reduce(dst2_f[:, i, k_:k_ + 1], ovf[:], ALU.add)
        P.ts(dst2_f[:, i, :], dst2_f[:, i, :], float(NE * CAP), None, ALU.min)
    P.copy(dsty_i[:], dsty_f[:])
    P.copy(dst2_i[:], dst2_f[:])
    for i in range(NTG):
        for k_ in range(2):
            P.add("pool", lambda e, i=i, k_=k_: e.indirect_dma_start(
                out=rowtok_d, out_offset=bass.IndirectOffsetOnAxis(ap=dst2_i[:, i, k_:k_ + 1], axis=0),
                in_=tokidx[:, i:i + 1], in_offset=None),
                reads=[dst2_i[:, i, k_:k_ + 1], tokidx[:, i:i + 1]], writes=[rowtok_d], dma=True)
    P.dma("sp", dsty_d.rearrange("t p k -> p t k"), dsty_i[:])
    for nb_ in range(NB):
        P.dma("sp", rowtok[:, nb_, :].unsqueeze(1),
              rowtok_d[nb_ * 128 * NE:(nb_ + 1) * 128 * NE, :].rearrange("(r h e) o -> r h (e o)", h=2, e=NEL)[:, half_sp, :])
    P.dma("sp", dest_own[:].unsqueeze(1),
          dsty_d.rearrange("(h t) p k -> p h t k", h=2)[:, half_sp, :, :])
    P.sb_release(m6r)
    m6b = P.sb_mark()
    wg = [P.sb(f"wg{i}", [128, 16, 512], BF16) for i in range(2)]
    wu = [P.sb(f"wu{i}", [128, 16, 512], BF16) for i in range(2)]
    wd = [P.sb_at(f"wd{i}", [128, 4, D], BF16, U_OFF + i * 16384) for i in range(2)]
    xe = [P.sb(f"xe{i}", [128, D], BF16) for i in range(2 * NB)]
    xeT = [P.sb(f"xeT{i}", [128, 16, 128], BF16) for i in range(2)]
    hg = [P.sb(f"hg{i}", [128, 512], F32) for i in range(2)]
    hact = [P.sb(f"hact{i}", [128, 512], BF16) for i in range(2)]
    actT = [P.sb(f"actT{i}", [128, 4, 128], BF16) for i in range(2)]
    ye = [P.sb_at("ye0", [128, D], BF16, DTDA_OFF), P.sb_at("ye1", [128, D], BF16, JUNK_OFF)]

    def W_(e_):
        b_ = e_ % 2
        load_cast_block(wg[b_], weg_d[e_].rearrange("(kc p) f -> p kc f", p=128), 0, 512, 16)
        load_cast_block(wu[b_], weu_d[e_].rearrange("(kc p) f -> p kc f", p=128), 0, 512, 16)
        wdv = wed_d[e_].rearrange("(kc p) n -> p kc n", p=128)
        for kc in range(4):
            P.dma("pool", wd[b_][:, kc:kc + 1, :], wdv[:, kc:kc + 1, :])

    def G_(e_, nb_):
        xb = xe[(e_ % 2) * NB + nb_]
        P.add("pool", lambda e, e_=e_, xb=xb, nb_=nb_: e.indirect_dma_start(
            out=xb[:], out_offset=None, in_=n2_flat,
            in_offset=bass.IndirectOffsetOnAxis(ap=rowtok[:, nb_, e_:e_ + 1], axis=0)),
            reads=[n2_flat, rowtok[:, nb_, e_:e_ + 1]], writes=[xb[:]], dma=True)

    def S_(e_, nb_, bi):
        yb = ye[bi % 2]
        P.add("pool", lambda e, e_=e_, yb=yb, nb_=nb_: e.indirect_dma_start(
            out=y_flat, out_offset=bass.IndirectOffsetOnAxis(ap=yrow_idx[:, nb_, e_:e_ + 1], axis=0),
            in_=yb[:], in_offset=None),
            reads=[yb[:], yrow_idx[:, nb_, e_:e_ + 1]], writes=[y_flat], dma=True)

    def TGU_(e_, nb_, bi):
        b_ = e_ % 2
        xb = xe[(e_ % 2) * NB + nb_]
        xt_ = xeT[bi % 2]
        for grp in range(4):
            pt = psb[grp % 2]
            for j in range(4):
                kc = grp * 4 + j
                P.tr(pt[:, j * 128:(j + 1) * 128], xb[:, kc * 128:(kc + 1) * 128], ident_bf[:])
            evac(xt_[:, grp * 4:(grp + 1) * 4, :], pt[:, 0:512].rearrange("p (j t) -> p j t", j=4))
        pg_ = ps[(bi % 2) * 2]
        pu_ = ps[(bi % 2) * 2 + 1]
        for kc in range(16):
            P.mm(pg_[:], xt_[:, kc, :], wg[b_][:, kc, :], start=(kc == 0), stop=(kc == 15))
        for kc in range(16):
            P.mm(pu_[:], xt_[:, kc, :], wu[b_][:, kc, :], start=(kc == 0), stop=(kc == 15))

    def AD_(e_, nb_, bi):
        b_ = e_ % 2
        pg_ = ps[(bi % 2) * 2]
        pu_ = ps[(bi % 2) * 2 + 1]
        P.act(hg[bi % 2][:], pg_[:], AF.Silu)
        P.tt(hact[bi % 2][:], hg[bi % 2][:], pu_[:], ALU.mult)
        pt = psb[bi % 2]
        for kc in range(4):
            P.tr(pt[:, kc * 128:(kc + 1) * 128], hact[bi % 2][:, kc * 128:(kc + 1) * 128], ident_bf[:])
        evac(actT[bi % 2][:], pt[:, 0:512].rearrange("p (j t) -> p j t", j=4))
        for cb in range(4):
            Yb = ps[4 + cb % 2]
            for kc in range(4):
                P.mm(Yb[:], actT[bi % 2][:, kc, :], wd[b_][:, kc, cb * 512:(cb + 1) * 512], start=(kc == 0), stop=(kc == 3))
            evac(ye[bi % 2][:, cb * 512:(cb + 1) * 512], Yb[:])

    blocks = [(e_, nb_) for e_ in range(NEL) for nb_ in range(NB)]
    W_(0)
    for nb_ in range(NB):
        G_(0, nb_)
    W_(1)
    TGU_(0, 0, 0)
    for bi, (e_, nb_) in enumerate(blocks):
        if nb_ == 0 and e_ + 1 < NEL:
            for n2_ in range(NB):
                G_(e_ + 1, n2_)
        if bi + 1 < len(blocks):
            TGU_(blocks[bi + 1][0], blocks[bi + 1][1], bi + 1)
        AD_(e_, nb_, bi)
        S_(e_, nb_, bi)
        if nb_ == NB - 1 and e_ + 2 < NEL:
            W_(e_ + 2)
    P.sb_release(m6b)
    P.core_barrier()
    yg = [P.sb(f"yg{i}", [128, D], BF16) for i in range(2)]
    for i in range(NT_OWN):
        for k_ in range(2):
            y_ = yg[k_]
            P.add("pool", lambda e, i=i, k_=k_, y_=y_: e.indirect_dma_start(
                out=y_[:], out_offset=None, in_=y_flat,
                in_offset=bass.IndirectOffsetOnAxis(ap=dest_own[:, i, k_:k_ + 1], axis=0)),
                reads=[y_flat, dest_own[:, i, k_:k_ + 1]], writes=[y_[:]], dma=True)
            P.stt(h1[:, i, :], y_[:], gates[:, i, k_:k_ + 1], h1[:, i, :], ALU.mult, ALU.add)
    P.sb_release(m6)
    if stages <= 6:
        P.emit()
        return P

    p_d = din("p", [OWN, 256])
    wpp_d = din("w_ple_proj", [256, D])
    pleg_d = din("ple_g", [1, D])
    wpg_d = din("w_ple_gate", [D, D])
    bpg_d = din("b_ple_gate", [1, D])
    fing_d = din("final_g", [1, D])
    wpg = P.sb("wpg", [128, 16, D], BF16)
    wpg_v = wpg_d.rearrange("(kc p) n -> p kc n", p=128)
    for cb in range(4):
        for q4 in range(0, 16, 4):
            P.dma("pool", wpg[:, q4:q4 + 4, cb * 512:(cb + 1) * 512], wpg_v[:, q4:q4 + 4, cb * 512:(cb + 1) * 512])
    wpp = P.sb("wpp", [128, 2, D], BF16)
    P.dma("pool", wpp[:], wpp_d.rearrange("(kc p) n -> p kc n", p=128))
    pleg_bc = P.sb_at("pleg_bc", [128, D], F32, U_OFF)
    bpg_bc = P.sb_at("bpg_bc", [128, D], F32, U_OFF + 8192)
    fing_bc = P.sb_at("fing_bc", [128, D], F32, U_OFF + 16384)
    P.dma("sp", pleg_bc[:], pleg_d.to_broadcast([128, D]))
    P.dma("sp", bpg_bc[:], bpg_d.to_broadcast([128, D]))
    P.dma("sp", fing_bc[:], fing_d.to_broadcast([128, D]))
    hb = P.sb("hb", [128, D], BF16)
    hT = P.sb("hT", [128, 16, 128], BF16)
    pt_f = P.sb("pt_f", [128, 256], F32)
    pt_b = P.sb("pt_b", [128, 256], BF16)
    pT = P.sb("pT", [128, 2, 128], BF16)
    gate_sb = P.sb("gate_sb", [128, D], F32)
    ple_raw = P.sb("ple_raw", [128, D], F32)
    ple_n = ple_raw
    osb = [gate_sb, gate_sb]
    for i in range(NT_OWN):
        hrow = h1[:, i, :]
        P.copy(hb[:], hrow, eng="pool")
        for grp in range(4):
            pt = psb[grp % 2]
            for j in range(4):
                kc = grp * 4 + j
                P.tr(pt[:, j * 128:(j + 1) * 128], hb[:, kc * 128:(kc + 1) * 128], ident_bf[:])
            evac(hT[:, grp * 4:(grp + 1) * 4, :], pt[:, 0:512].rearrange("p (j t) -> p j t", j=4))
        P.dma("sp", pt_f[:], p_d[i * 128:(i + 1) * 128, :])
        P.copy(pt_b[:], pt_f[:])
        ptp = psb[0]
        for kc in range(2):
            P.tr(ptp[:, kc * 128:(kc + 1) * 128], pt_b[:, kc * 128:(kc + 1) * 128], ident_bf[:])
        evac(pT[:], ptp[:, 0:256].rearrange("p (j t) -> p j t", j=2))
        for cb in range(4):
            cs_ = slice(cb * 512, (cb + 1) * 512)
            G = ps[cb % 2]
            for kc in range(16):
                P.mm(G[:], hT[:, kc, :], wpg[:, kc, cs_], start=(kc == 0), stop=(kc == 15))
            P.tt(gate_sb[:, cs_], G[:], bpg_bc[:, cs_], ALU.add)
            Lp = ps[2 + cb % 2]
            for kc in range(2):
                P.mm(Lp[:], pT[:, kc, :], wpp[:, kc, cs_], start=(kc == 0), stop=(kc == 1))
            evac(ple_raw[:, cs_], Lp[:])
        P.act(gate_sb[:], gate_sb[:], AF.Sigmoid)
        P.stt(junk[:], ple_raw[:], 1.0, ple_raw[:], ALU.mult, ALU.mult, accum_out=ss[0][:, 0:1])
        P.act(ss[0][:, 1:2], ss[0][:, 0:1], AF.Sqrt, bias=eps_t[:, 0:1], scale=1.0 / D)
        P.add("dve", lambda e, o=ss[0][:, 1:2]: e.reciprocal(o, o), reads=[ss[0][:, 1:2]], writes=[ss[0][:, 1:2]])
        P.stt(ple_n[:], ple_raw[:], ss[0][:, 1:2], pleg_bc[:], ALU.mult, ALU.mult)
        P.tt(ple_n[:], ple_n[:], gate_sb[:], ALU.mult)
        P.tt(hrow, hrow, ple_n[:], ALU.add)
        o_ = osb[i % 2]
        P.stt(junk[:], hrow, 1.0, hrow, ALU.mult, ALU.mult, accum_out=ss[1][:, 0:1])
        P.act(ss[1][:, 1:2], ss[1][:, 0:1], AF.Sqrt, bias=eps_t[:, 0:1], scale=1.0 / D)
        P.add("dve", lambda e, o=ss[1][:, 1:2]: e.reciprocal(o, o), reads=[ss[1][:, 1:2]], writes=[ss[1][:, 1:2]])
        P.stt(o_[:], hrow, ss[1][:, 1:2], fing_bc[:], ALU.mult, ALU.mult)
        P.dma("sp", out_d[i * 128:(i + 1) * 128, :], o_[:])
    P.barrier()
    P.emit()
    return P


def prep_core_inputs(inp, c, shared):
    b, half = c // 2, c % 2
    flip = half == 1
    m = {}
    xb = inp["x"][b]
    m["x"] = np.ascontiguousarray(xb[::-1] if flip else xb)
    pos = inp["positions"][b]
    pos = pos[::-1] if flip else pos
    m["pos"] = np.ascontiguousarray(pos.reshape(NT_ALL, 128).T).astype(np.int32)
    m["consts"] = shared["consts"]
    m["g_mix"] = np.ascontiguousarray(inp["norm_mix_g"][0][None, :])
    m["w_in"] = shared["w_in_flip"] if flip else shared["w_in"]
    cw = inp["conv_w"][0]
    if flip:
        cw = cw[::-1]
    m["convw"] = np.ascontiguousarray(cw.T.reshape(12, 128, 5).transpose(1, 0, 2))
    m["convb"] = np.ascontiguousarray(inp["conv_b"][0].reshape(12, 128).T)
    f, r = ("dt_bias_b", "dt_bias_f") if flip else ("dt_bias_f", "dt_bias_b")
    m["dtb"] = np.concatenate([inp[f][0], inp[r][0]])[None, :].astype(np.float32)
    f, r = ("a_log_b", "a_log_f") if flip else ("a_log_f", "a_log_b")
    m["alog"] = np.concatenate([inp[f][0], inp[r][0]])[None, :].astype(np.float32)
    return m


def prep_shared(inp):
    sh = {}
    sh["consts"] = host_consts()
    w = np.ascontiguousarray(inp["w_in"][0])
    sh["w_in"] = w
    wf = w.copy()
    wf[:, C_DT:C_DT + 16] = w[:, C_DT + 16:C_DT + 32]
    wf[:, C_DT + 16:C_DT + 32] = w[:, C_DT:C_DT + 16]
    sh["w_in_flip"] = wf
    return sh


def prep_core_inputs_full(inp, c, shared):
    m = prep_core_inputs(inp, c, shared)
    b, half = c // 2, c % 2
    flip = half == 1
    m["dskip"] = np.ascontiguousarray(inp["d_skip"][0][None, :])
    m["ssd_g"] = np.ascontiguousarray(inp["ssd_norm_g"][0][None, :])
    m["lamv"] = shared["lamv"]
    m["subln_g"] = np.ascontiguousarray(inp["subln_g"][0][None, :])
    m["w_out"] = shared["w_out"]
    m["g_ffn"] = np.ascontiguousarray(inp["norm_ffn_g"][0][None, :])
    m["w_route"] = shared["w_route"]
    m["b_route"] = shared["b_route"]
    m["w_exp_gate"] = shared["w_exp_gate"][half * 32:(half + 1) * 32]
    m["w_exp_up"] = shared["w_exp_up"][half * 32:(half + 1) * 32]
    m["w_exp_down"] = shared["w_exp_down"][half * 32:(half + 1) * 32]
    NB_ = 2
    yi = ((half * 32 + np.arange(32)[None, None, :]) * (128 * NB_) + np.arange(NB_)[None, :, None] * 128
          + np.arange(128)[:, None, None])
    m["yrow_idx"] = np.ascontiguousarray(yi.reshape(128, NB_ * 32)).astype(np.int32)
    pb = inp["p"][0, b]
    pb = pb[::-1] if flip else pb
    m["p"] = np.ascontiguousarray(pb[:OWN])
    m["w_ple_proj"] = shared["w_ple_proj"]
    m["ple_g"] = np.ascontiguousarray(inp["ple_norm_g"][0][None, :])
    m["w_ple_gate"] = shared["w_ple_gate"]
    m["b_ple_gate"] = np.ascontiguousarray(inp["b_ple_gate"][0][None, :])
    m["final_g"] = np.ascontiguousarray(inp["final_norm_g"][None, :])
    return m


def prep_shared_full(inp):
    sh = prep_shared(inp)
    sh["lamv"] = np.concatenate([inp["lam_q1"][0], inp["lam_k1"][0], inp["lam_q2"][0], inp["lam_k2"][0]])[None, :].astype(np.float32)
    sh["w_out"] = np.ascontiguousarray(inp["w_out"][0])
    sh["w_route"] = np.ascontiguousarray(np.concatenate([inp["w_route_group"][0], inp["w_route_expert"][0]], axis=1))
    sh["b_route"] = np.concatenate([inp["b_route_group"][0], inp["b_route_expert"][0]])[None, :].astype(np.float32)
    sh["w_exp_gate"] = np.ascontiguousarray(inp["w_exp_gate"][0])
    sh["w_exp_up"] = np.ascontiguousarray(inp["w_exp_up"][0])
    sh["w_exp_down"] = np.ascontiguousarray(inp["w_exp_down"][0])
    sh["w_ple_proj"] = np.ascontiguousarray(inp["w_ple_proj"][0])
    sh["w_ple_gate"] = np.ascontiguousarray(inp["w_ple_gate"][0])
    return sh


def kernel(**inputs):
    inp = {k: np.asarray(v) for k, v in inputs.items()}
    nc = bass.Bass("TRN2", target_bir_lowering=False, num_devices=8)
    build_program(nc, stages=99, dbg=False)
    sh = prep_shared_full(inp)
    maps = [prep_core_inputs_full(inp, c, sh) for c in range(8)]
    res = run_bass_kernel_spmd(nc, maps, core_ids=list(range(8)))
    out = np.zeros((4, SEQ, D), np.float32)
    for c in range(8):
        b, half = c // 2, c % 2
        o = np.asarray(res.results[c]["out"])
        if half == 0:
            out[b, 0:OWN] = o
        else:
            out[b, OWN:SEQ] = o[::-1]
    return out
```
